# Optimizing a Trainium2 kernel written in Bass

```python
import math
import jax, jax.numpy as jnp
from jax import lax
import numpy as np

D_MODEL = 1024
BATCH = 8
SEQ = 2048
DEPTH = 2

HEAD_DIM = 64
HEADS_PER_GROUP = 4
DILATION_PATTERNS = ((128, 1), (512, 4), (2048, 16))
N_DIL_GROUPS = len(DILATION_PATTERNS)
N_ATTN_HEADS = N_DIL_GROUPS * HEADS_PER_GROUP
ATTN_WIDTH = N_ATTN_HEADS * HEAD_DIM
ATTN_OUT_WIDTH = HEADS_PER_GROUP * HEAD_DIM
POOL_WINDOWS = (2, 4, 8, 16)
POOL_GROUP_WIDTH = 128
POOL_WIDTH = len(POOL_WINDOWS) * POOL_GROUP_WIDTH
N_BRANCHES = 2
IN_WIDTH = 3 * ATTN_WIDTH + POOL_WIDTH + N_BRANCHES * D_MODEL
N_BUCKETS = 32
MAX_DISTANCE = 1024
N_EXPERT_GROUPS = 8
EXPERTS_PER_GROUP = 8
N_EXPERTS = N_EXPERT_GROUPS * EXPERTS_PER_GROUP
TOP_K_IN_GROUP = 2
D_EXPERT = 512
MOE_BLOCK = 128
EPS = 1e-6
NEG_INF = -1e30

kernel_name = "hybrid_dilated_pool_hiermoe_encoder"


def rmsnorm(x, g):
    xf = x.astype(jnp.float32)
    r = lax.rsqrt(jnp.mean(xf * xf, axis=-1, keepdims=True) + EPS)
    return (xf * r).astype(x.dtype) * g


def t5_bucket(rel):
    nb = N_BUCKETS // 2
    ret = jnp.where(rel > 0, nb, 0)
    n = jnp.abs(rel)
    max_exact = nb // 2
    nf = jnp.maximum(n, max_exact).astype(jnp.float32)
    large = max_exact + (jnp.log(nf / max_exact) / math.log(MAX_DISTANCE / max_exact)
                         * (nb - max_exact)).astype(jnp.int32)
    large = jnp.minimum(large, nb - 1)
    return ret + jnp.where(n < max_exact, n, large)


def dilated_band_attention(q, k, v, bias_tab, dilation, n_side):
    B, S, H, Dh = q.shape
    L = S // dilation
    blk = n_side
    nb = -(-L // blk)
    Lp = nb * blk

    def to_sub(t):
        t = t.reshape(B, L, dilation, H, Dh).transpose(0, 2, 1, 3, 4)
        return jnp.pad(t, ((0, 0), (0, 0), (0, Lp - L), (0, 0), (0, 0)))

    def key_blocks(t):
        tp = jnp.pad(to_sub(t), ((0, 0), (0, 0), (blk, blk), (0, 0), (0, 0)))
        tp = tp.reshape(B, dilation, nb + 2, blk, H, Dh)
        return jnp.concatenate([tp[:, :, :-2], tp[:, :, 1:-1], tp[:, :, 2:]], axis=3)

    qs = to_sub(q).reshape(B, dilation, nb, blk, H, Dh)
    kb = key_blocks(k)
    vb = key_blocks(v)

    a = jnp.arange(blk)[:, None]
    c = jnp.arange(3 * blk)[None, :]
    rel = c - blk - a
    key_idx = jnp.arange(nb)[:, None, None] * blk - blk + c[None]
    valid = (jnp.abs(rel) <= n_side)[None] & (key_idx >= 0) & (key_idx < L)
    pos_bias = bias_tab[t5_bucket(rel * dilation)].transpose(2, 0, 1).astype(jnp.float32)

    scale = HEAD_DIM ** -0.5
    logits = jnp.einsum('bgnqhd,bgnkhd->bgnhqk', qs, kb,
                        preferred_element_type=jnp.float32) * scale + pos_bias
    logits = jnp.where(valid[:, None], logits, NEG_INF)
    m = jnp.max(logits, axis=-1, keepdims=True)
    p = jnp.exp(logits - m)
    den = jnp.sum(p, axis=-1, keepdims=True)
    o = jnp.einsum('bgnhqk,bgnkhd->bgnqhd', (p / den).astype(v.dtype), vb)
    lse = (m + jnp.log(den))[..., 0]

    o = o.reshape(B, dilation, Lp, H, Dh)[:, :, :L].transpose(0, 2, 1, 3, 4).reshape(B, S, H, Dh)
    lse = lse.transpose(0, 1, 2, 4, 3).reshape(B, dilation, Lp, H)[:, :, :L]
    lse = lse.transpose(0, 2, 1, 3).reshape(B, S, H)
    return o, lse


def dilated_attention_mixture(q, k, v, rel_bias):
    B, S = q.shape[:2]
    outs, lses = [], []
    for gi, (window, dilation) in enumerate(DILATION_PATTERNS):
        hs = slice(gi * HEADS_PER_GROUP, (gi + 1) * HEADS_PER_GROUP)
        n_side = window // (2 * dilation)
        o, lse = dilated_band_attention(q[:, :, hs], k[:, :, hs], v[:, :, hs],
                                        rel_bias[:, hs], dilation, n_side)
        outs.append(o)
        lses.append(lse)
    w = jax.nn.softmax(jnp.stack(lses, axis=0), axis=0)
    o = jnp.sum(w[..., None].astype(q.dtype) * jnp.stack(outs, axis=0), axis=0)
    return o.reshape(B, S, ATTN_OUT_WIDTH)


def multiscale_pool(u, pool_w, pool_scale):
    B, S, _ = u.shape
    uf = u.astype(jnp.float32)
    cs = jnp.concatenate([jnp.zeros((B, 1, POOL_WIDTH), jnp.float32),
                          jnp.cumsum(uf, axis=1)], axis=1)
    pos = jnp.arange(S)
    outs = []
    for gi, w in enumerate(POOL_WINDOWS):
        sl = slice(gi * POOL_GROUP_WIDTH, (gi + 1) * POOL_GROUP_WIDTH)
        lo = jnp.maximum(pos - w // 2, 0)
        hi = jnp.minimum(pos + w - w // 2, S)
        csg = cs[..., sl]
        mean = (csg[:, hi] - csg[:, lo]) / (hi - lo).astype(jnp.float32)[None, :, None]
        outs.append(mean - uf[..., sl])
    pooled = jnp.stack(outs, axis=2).astype(u.dtype)
    mixed = jnp.einsum('bsgc,gce->bsge', pooled, pool_w).reshape(B, S, POOL_WIDTH)
    return mixed * pool_scale


def hybrid_mixer(xn, w_in, rel_bias, pool_w, pool_scale, w_proj_attn, w_proj_pool, w_out):
    B, S, D = xn.shape
    proj = xn @ w_in
    q, k, v, u, gates = jnp.split(
        proj, [ATTN_WIDTH, 2 * ATTN_WIDTH, 3 * ATTN_WIDTH, 3 * ATTN_WIDTH + POOL_WIDTH], axis=-1)
    q = q.reshape(B, S, N_ATTN_HEADS, HEAD_DIM)
    k = k.reshape(B, S, N_ATTN_HEADS, HEAD_DIM)
    v = v.reshape(B, S, N_ATTN_HEADS, HEAD_DIM)
    y_attn = dilated_attention_mixture(q, k, v, rel_bias) @ w_proj_attn
    y_pool = multiscale_pool(u, pool_w, pool_scale) @ w_proj_pool
    g = jax.nn.sigmoid(gates.astype(jnp.float32)).astype(xn.dtype).reshape(B, S, N_BRANCHES, D)
    y = g[:, :, 0] * y_attn + g[:, :, 1] * y_pool
    return y @ w_out


def hierarchical_moe(xn, w_router_group, w_router_expert, w_gate_e, w_up_e, w_down_e):
    B, S, D = xn.shape
    T = B * S
    xt = xn.reshape(T, D)
    g_logits = (xt @ w_router_group).astype(jnp.float32)
    g_prob = jax.nn.softmax(g_logits, axis=-1)
    g_idx = jnp.argmax(g_logits, axis=-1)
    g_p = jnp.take_along_axis(g_prob, g_idx[:, None], axis=1)[:, 0]
    e_all = jnp.einsum('td,gde->tge', xt, w_router_expert).astype(jnp.float32)
    e_logits = jnp.take_along_axis(e_all, g_idx[:, None, None], axis=1)[:, 0]
    top_val, top_idx = lax.top_k(e_logits, TOP_K_IN_GROUP)
    comb_w = g_p[:, None] * jax.nn.softmax(top_val, axis=-1)
    expert_id = g_idx[:, None].astype(jnp.int32) * EXPERTS_PER_GROUP + top_idx.astype(jnp.int32)

    A = T * TOP_K_IN_GROUP
    flat_e = expert_id.reshape(A)
    flat_t = jnp.repeat(jnp.arange(T, dtype=jnp.int32), TOP_K_IN_GROUP)
    flat_w = comb_w.reshape(A)
    se, st, sw = lax.sort((flat_e, flat_t, flat_w), num_keys=1, is_stable=True)
    counts = jnp.bincount(flat_e, length=N_EXPERTS)
    offsets = jnp.cumsum(counts) - counts
    padded = (counts + MOE_BLOCK - 1) // MOE_BLOCK * MOE_BLOCK
    padded_end = jnp.cumsum(padded)
    padded_off = padded_end - padded
    dest = padded_off[se] + (jnp.arange(A, dtype=jnp.int32) - offsets[se])
    P = A + N_EXPERTS * MOE_BLOCK
    n_blk = P // MOE_BLOCK
    x_pad = jnp.zeros((P, D), xt.dtype).at[dest].set(xt[st])
    blk_start = jnp.arange(n_blk, dtype=jnp.int32) * MOE_BLOCK
    blk_expert = jnp.minimum(jnp.searchsorted(padded_end, blk_start, side='right'),
                             N_EXPERTS - 1).astype(jnp.int32)

    def expert_block(args):
        xb, e = args
        h = jax.nn.silu(xb @ w_gate_e[e]) * (xb @ w_up_e[e])
        return h @ w_down_e[e]

    y_pad = lax.map(expert_block, (x_pad.reshape(n_blk, MOE_BLOCK, D), blk_expert)).reshape(P, D)
    y = y_pad[dest] * sw[:, None].astype(xt.dtype)
    out = jax.ops.segment_sum(y, st, num_segments=T)
    return out.reshape(B, S, D)


def setup_inputs(seed: int = 0) -> dict:
    key = jax.random.key(seed)
    ks = jax.random.split(key, 16)
    f32 = jnp.float32
    nrm = lambda k, shape, fan_in: jax.random.normal(k, shape, f32) * (fan_in ** -0.5)
    return {
        "x": jax.random.normal(ks[0], (BATCH, SEQ, D_MODEL), f32),
        "rel_bias": jax.random.normal(ks[1], (N_BUCKETS, N_ATTN_HEADS), f32) * 0.3,
        "norm_mix_g": 1.0 + 0.02 * jax.random.normal(ks[2], (DEPTH, D_MODEL), f32),
        "w_in": nrm(ks[3], (DEPTH, D_MODEL, IN_WIDTH), D_MODEL),
        "pool_w": nrm(ks[4], (DEPTH, len(POOL_WINDOWS), POOL_GROUP_WIDTH, POOL_GROUP_WIDTH), POOL_GROUP_WIDTH),
        "pool_scale": 1.0 + 0.1 * jax.random.normal(ks[5], (DEPTH, POOL_WIDTH), f32),
        "w_proj_attn": nrm(ks[6], (DEPTH, ATTN_OUT_WIDTH, D_MODEL), ATTN_OUT_WIDTH),
        "w_proj_pool": nrm(ks[7], (DEPTH, POOL_WIDTH, D_MODEL), POOL_WIDTH),
        "w_out": nrm(ks[8], (DEPTH, D_MODEL, D_MODEL), D_MODEL),
        "norm_ffn_g": 1.0 + 0.02 * jax.random.normal(ks[9], (DEPTH, D_MODEL), f32),
        "w_router_group": nrm(ks[10], (DEPTH, D_MODEL, N_EXPERT_GROUPS), D_MODEL),
        "w_router_expert": nrm(ks[11], (DEPTH, N_EXPERT_GROUPS, D_MODEL, EXPERTS_PER_GROUP), D_MODEL),
        "w_gate_e": nrm(ks[12], (DEPTH, N_EXPERTS, D_MODEL, D_EXPERT), D_MODEL),
        "w_up_e": nrm(ks[13], (DEPTH, N_EXPERTS, D_MODEL, D_EXPERT), D_MODEL),
        "w_down_e": nrm(ks[14], (DEPTH, N_EXPERTS, D_EXPERT, D_MODEL), D_EXPERT),
        "norm_final_g": 1.0 + 0.02 * jax.random.normal(ks[15], (D_MODEL,), f32),
    }


def reference(x, rel_bias, norm_mix_g, w_in, pool_w, pool_scale, w_proj_attn, w_proj_pool,
              w_out, norm_ffn_g, w_router_group, w_router_expert, w_gate_e, w_up_e, w_down_e,
              norm_final_g):
    h = x
    for l in range(DEPTH):
        h = h + hybrid_mixer(rmsnorm(h, norm_mix_g[l]), w_in[l], rel_bias, pool_w[l],
                             pool_scale[l], w_proj_attn[l], w_proj_pool[l], w_out[l])
        h = h + hierarchical_moe(rmsnorm(h, norm_ffn_g[l]), w_router_group[l],
                                 w_router_expert[l], w_gate_e[l], w_up_e[l], w_down_e[l])
    return rmsnorm(h, norm_final_g)
```

```python
import math
from contextlib import ExitStack

import numpy as np
import concourse.bass as bass
import concourse.mybir as mybir
from concourse.bass_utils import run_bass_kernel_spmd

F32 = mybir.dt.float32
BF16 = mybir.dt.bfloat16
I32 = mybir.dt.int32
ALU = mybir.AluOpType
AF = mybir.ActivationFunctionType
AX = mybir.AxisListType

S_ = 2048
D = 1024
NT = 16
INW = 4864
CAPG = 384
CAPE = 128
NEGM = -30000.0
GROUPS = ((1, 0), (4, 1), (16, 2))
POOLW = (2, 4, 8, 16)
C_ID, C_TRI, C_ONE, C_IOTA, C_EDGE, C_SEL = 0, 128, 256, 384, 768, 832
C_N = 896


class Sched:
    ENG = ["pe", "act", "dve", "pool", "sp"]
    RING = 12

    def __init__(self, nc, es):
        self.nc = nc
        self.obj = {"pe": nc.tensor, "act": nc.scalar, "dve": nc.vector, "pool": nc.gpsimd, "sp": nc.sync}
        self.sem = {e: es.enter_context(nc.semaphore("s_" + e)) for e in self.ENG}
        self.cnt = {e: 0 for e in self.ENG}
        self.seen = {x: {} for x in self.ENG}
        self.lastw = {}
        self.reads = {}
        self.ring = {}
        self.ring_i = {}
        for q in ("sp", "pool"):
            self.ring[q] = []
            self.ring_i[q] = 0
            for i in range(self.RING):
                name = "q_%s_%d" % (q, i)
                self.sem[name] = es.enter_context(nc.semaphore(name))
                self.cnt[name] = 0
                self.ring[q].append(name)

    def _dep(self, x, e, ts, raw=False):
        if e == x and x in ("pe", "sp"):
            return
        if self.seen[x].get(e, 0) < ts:
            self.seen[x][e] = ts
            self.obj[x].wait_ge(self.sem[e], ts)

    def _deps(self, x, reads, writes):
        for r in reads:
            w = self.lastw.get(r)
            if w is not None:
                self._dep(x, w[0], w[1], raw=True)
        for r in writes:
            w = self.lastw.get(r)
            if w is not None:
                self._dep(x, *w)
            rd = self.reads.get(r)
            if rd:
                for e, ts in rd.items():
                    self._dep(x, e, ts)

    def _mark(self, who, ts, reads, writes):
        for r in reads:
            self.reads.setdefault(r, {})[who] = ts
        for r in writes:
            self.lastw[r] = (who, ts)
            self.reads[r] = {}

    def op(self, x, fn, reads=(), writes=()):
        self._deps(x, reads, writes)
        self.cnt[x] += 1
        ts = self.cnt[x]
        fn(self.obj[x]).then_inc(self.sem[x], 1)
        self._mark(x, ts, reads, writes)

    def dma(self, q, fn, reads=(), writes=()):
        self._deps(q, reads, writes)
        i = self.ring_i[q]
        self.ring_i[q] = (i + 1) % self.RING
        name = self.ring[q][i]
        if self.cnt[name] > 0:
            self._dep(q, name, self.cnt[name])
        self.cnt[name] += 16
        ts = self.cnt[name]
        fn(self.obj[q]).then_inc(self.sem[name], 16)
        self._mark(name, ts, reads, writes)

    def barrier(self):
        for x in self.ENG:
            for e in self.cnt:
                if self.cnt[e] > 0:
                    self._dep(x, e, self.cnt[e])

    def finish(self):
        for name in self.cnt:
            if name.startswith("q_") and self.cnt[name] > 0:
                self._dep("sp", name, self.cnt[name])
        for e in self.ENG:
            if self.cnt[e] > 0:
                self._dep("sp", e, self.cnt[e])


def bias_index(h, ty):
    return h * 3 + ty if h < 8 else 24 + (h - 8)


def tok_slice(dil, sub, blk):
    st = sub + dil * 128 * blk
    return slice(st, st + dil * 127 + 1, dil)


def build(dbg=0):
    nc = bass.Bass("TRN2", target_bir_lowering=False)
    dt = lambda name, shape, d=F32, kind="ExternalInput": nc.dram_tensor(name, shape, d, kind=kind).ap()
    x_d = dt("x", [S_, D])
    w_in_d = dt("w_in", [2, D, INW])
    pool_w_d = dt("pool_w", [2, 4, 128, 128])
    w_pa_d = dt("w_proj_attn", [2, 256, D])
    w_pp_d = dt("w_proj_pool", [2, 512, D])
    w_out_d = dt("w_out", [2, D, D])
    wr_d = dt("wr", [2, D, 72])
    wg_d = dt("w_gate_e", [2, 64, D, 512])
    wu_d = dt("w_up_e", [2, 64, D, 512])
    wd_d = dt("w_down_e", [2, 64, 512, D])
    gvec_d = dt("gvec", [128, 5, D])
    pscale_d = dt("pscale", [128, 8])
    bias_d = dt("biasT", [128, 28, 128])
    cf_d = dt("cf32", [128, C_N])
    xg_d = dt("xg_scr", [64 * CAPE, D], BF16, kind="Internal")
    yg_d = dt("yg_scr", [64 * CAPE, D], BF16, kind="Internal")
    out_d = dt("out", [S_, D], kind="ExternalOutput")
    dbg_d = dt("dbg", [S_, D], kind="ExternalOutput") if dbg else None

    with ExitStack() as es:
        uid = [0]

        def sb(name, shape, d=F32, st=es):
            uid[0] += 1
            return st.enter_context(nc.sbuf_tensor("sb%d_%s" % (uid[0], name), shape, d))

        psum = lambda name, shape, d=F32: es.enter_context(nc.psum_tensor("ps_" + name, shape, d))
        S = Sched(nc, es)
        h = sb("h", [128, NT, D])
        xnT = sb("xnT", [128, 8, S_], BF16)
        biasT = sb("biasT", [128, 28, 128], BF16)
        cf = sb("cf", [128, C_N])
        gv = sb("gv", [128, D])
        pscale = sb("pscale", [128, 8])
        identb = sb("identb", [128, 128], BF16)
        ssq = sb("ssq", [128, NT])
        rstd = sb("rstd", [128, NT])
        junk = sb("junk", [128, D], BF16)
        B = [psum("B%d" % i, [128, 512]) for i in range(6)]
        tgen = [0]
        tstack = [ExitStack()]
        Tb = [None, None]

        def alloc_T():
            tgen[0] += 1
            tstack[0] = ExitStack()
            for i in range(2):
                Tb[i] = tstack[0].enter_context(nc.psum_tensor("ps_T%d_%d" % (i, tgen[0]), [128, 8, 128], BF16))

        alloc_T()
        es.callback(lambda: tstack[0].close())
        ident = cf[:, C_ID:C_ID + 128]
        tri = cf[:, C_TRI:C_TRI + 128]
        ones = cf[:, C_ONE:C_ONE + 128]
        iota = cf[:, C_IOTA:C_IOTA + CAPG]
        sel65 = cf[:, C_SEL:C_SEL + 64]

        S.dma("sp", lambda e: e.dma_start(out=cf[:], in_=cf_d), writes=["cf"])
        S.dma("pool", lambda e: e.dma_start(out=biasT[:], in_=bias_d), writes=["biasT"])
        S.dma("sp", lambda e: e.dma_start(out=pscale[:], in_=pscale_d), writes=["pscale"])
        for tt in range(NT):
            S.dma("sp", lambda e, tt=tt: e.dma_start(out=h[:, tt, :], in_=x_d[tt * 128:(tt + 1) * 128, :]),
                  writes=["h%d" % tt])
        S.op("dve", lambda e: e.tensor_copy(out=identb[:], in_=ident), reads=["cf"], writes=["identb"])

        pp_i = [0]
        pp_banks = [[0, 1, 2, 3, 4, 5]]

        def next_pp():
            lst = pp_banks[0]
            pp_i[0] = (pp_i[0] + 1) % len(lst)
            i = lst[pp_i[0]]
            return B[i], "B%d" % i

        bc_reg = nc.gpsimd.alloc_register("bc_reg")
        nc.gpsimd.reg_mov(bc_reg, 64 * CAPE - 1)
        pq_i = [0]
        pq_list = [(B[i], "B%d" % i) for i in range(6)]

        def next_pq():
            i = pq_i[0]
            pq_i[0] = (i + 1) % 6
            return pq_list[i]

        def rms_stats(gidx):
            S.dma("sp", lambda e: e.dma_start(out=gv[:], in_=gvec_d[:, gidx, :]), writes=["gv"])
            for tt in range(NT):
                S.op("act", lambda e, tt=tt: e.activation(out=junk[:], in_=h[:, tt, :], func=AF.Square,
                                                          accum_out=ssq[:, tt:tt + 1]),
                     reads=["h%d" % tt], writes=["junk", "ssq"])
            S.op("dve", lambda e: e.tensor_scalar(out=rstd[:], in0=ssq[:], scalar1=1.0 / D, scalar2=1e-6,
                                                  op0=ALU.mult, op1=ALU.add), reads=["ssq"], writes=["rstd"])
            S.op("act", lambda e: e.activation(out=rstd[:], in_=rstd[:], func=AF.Sqrt), reads=["rstd"], writes=["rstd"])
            S.op("dve", lambda e: e.reciprocal(out=rstd[:], in_=rstd[:]), reads=["rstd"], writes=["rstd"])

        def norm_tile(tt, out_ap, out_res, rres="rstd"):
            S.op("dve", lambda e: e.scalar_tensor_tensor(out=out_ap, in0=h[:, tt, :], scalar=rstd[:, tt:tt + 1],
                                                         in1=gv[:], op0=ALU.mult, op1=ALU.mult),
                 reads=["h%d" % tt, rres, "gv"], writes=[out_res])

        def rms_tile(tt):
            rr = "rstd_t%d" % tt
            S.op("act", lambda e: e.activation(out=junk[:], in_=h[:, tt, :], func=AF.Square, accum_out=ssq[:, tt:tt + 1]),
                 reads=["h%d" % tt], writes=["junk", "ssq_t%d" % tt])
            S.op("dve", lambda e: e.tensor_scalar(out=rstd[:, tt:tt + 1], in0=ssq[:, tt:tt + 1], scalar1=1.0 / D, scalar2=1e-6,
                                                  op0=ALU.mult, op1=ALU.add), reads=["ssq_t%d" % tt], writes=[rr])
            S.op("act", lambda e: e.activation(out=rstd[:, tt:tt + 1], in_=rstd[:, tt:tt + 1], func=AF.Ln), reads=[rr], writes=[rr])
            S.op("act", lambda e: e.activation(out=rstd[:, tt:tt + 1], in_=rstd[:, tt:tt + 1], func=AF.Exp, scale=-0.5),
                 reads=[rr], writes=[rr])
            return rr

        def mixer(l):
            m0 = ExitStack()
            if l == 0:
                rms_stats(2 * l)
                xnb = [sb("xnb%d" % i, [128, D], BF16, m0) for i in range(2)]
            for tt in (range(NT) if l == 0 else ()):
                xb = xnb[tt % 2]
                xr = "xnb%d" % (tt % 2)
                norm_tile(tt, xb[:], xr)
                Tt, Tr = Tb[tt % 2], "T%d" % (tt % 2)
                for c in range(8):
                    S.op("pe", lambda e, c=c, xb=xb, Tt=Tt: e.transpose(out=Tt[:, c, :], in_=xb[:, c * 128:(c + 1) * 128],
                                                                        identity=identb[:]),
                         reads=[xr, "identb"], writes=[Tr])
                if tt % 2 == 0:
                    S.op("act", lambda e, tt=tt, Tt=Tt: e.copy(out=xnT[:, :, tt * 128:(tt + 1) * 128], in_=Tt[:]),
                         reads=[Tr], writes=["xnT%d" % tt])
                else:
                    S.op("dve", lambda e, tt=tt, Tt=Tt: e.tensor_copy(out=xnT[:, :, tt * 128:(tt + 1) * 128], in_=Tt[:]),
                         reads=[Tr], writes=["xnT%d" % tt])
            xall = ["xnT%d" % t for t in range(NT)]
            S.barrier()
            m0.close()
            if l == 0:
                S.op("pool", lambda e: e.memset(junk[:], 0.0), writes=["junk"])
                for a_ in range(64 * CAPE // 128):
                    S.dma("sp", lambda e, a_=a_: e.dma_start(out=xg_d[a_ * 128:(a_ + 1) * 128, :], in_=junk[:]),
                          reads=["junk"], writes=["xg_init"])
            with ExitStack() as mx:
                oT = sb("oT", [128, 4, S_], BF16, mx)
                with ExitStack() as ph:
                    accO = sb("accO", [128, 4, S_], F32, ph)
                    S.op("pool", lambda e: e.memset(accO[:], 0.0), writes=["accO%d" % i for i in range(4)])
                    ph2 = ExitStack()
                    wh = [sb("wh%d" % i, [128, 8, 3, 128], BF16, ph2) for i in range(2)]
                    qA0 = [sb("qA0_%d" % i, [128, S_], BF16, ph2) for i in range(2)]
                    q0B = [sb("q0B_%d" % i, [128, S_], BF16, ph2) for i in range(2)]
                    kAB = [sb("kAB%d" % i, [128, S_], BF16, ph2) for i in range(2)]
                    vhp = [sb("vhp%d" % i, [128, NT, 2, 65], BF16, ph2) for i in range(2)]
                    pt = [sb("pt%d" % i, [128, 384], BF16, ph2) for i in range(3)]
                    tstack[0].close()
                    Bx = [ph2.enter_context(nc.psum_tensor("ps_Bx%d_%d" % (i, tgen[0]), [128, 512], F32)) for i in range(2)]
                    sc_banks = [(B[2], "B2"), (B[3], "B3"), (Bx[0], "Bx0")]
                    pv_banks = [(B[4], "B4"), (B[5], "B5"), (Bx[1], "Bx1")]
                    for i in range(2):
                        S.op("pool", lambda e, i=i: e.memset(vhp[i][:], 1.0), writes=["vhp%d" % i])
                        S.op("pool", lambda e, i=i: e.memset(qA0[i][:], 0.0), writes=["qA0_%d" % i])
                        S.op("pool", lambda e, i=i: e.memset(q0B[i][:], 0.0), writes=["q0B_%d" % i])
                    wqkv = w_in_d[l][:, 0:2304].rearrange("(c p) (t n) -> p c t n", p=128, t=3)
                    rot = [0]

                    def proj_gen(p):
                        gi = p // 2
                        dil = GROUPS[gi][0]
                        nsub = dil
                        nblk = 16 // dil
                        b_ = p % 2
                        w_ = wh[b_]
                        wr_ = "wh%d" % b_
                        for t3 in range(3):
                            S.dma("pool", lambda e, w_=w_, p=p, t3=t3: e.dma_start(
                                out=w_[:, :, t3, :], in_=wqkv[:, :, t3, p * 128:(p + 1) * 128]), writes=[wr_])
                        for tb in range(4):
                            tsl = slice(tb * 512, (tb + 1) * 512)
                            pp, pr = next_pp()
                            for c in range(8):
                                S.op("pe", lambda e, pp=pp, c=c, tsl=tsl, w_=w_: e.matmul(
                                    out=pp[:], lhsT=w_[:, c, 0, :], rhs=xnT[:, c, tsl], start=(c == 0), stop=(c == 7)),
                                    reads=[wr_] + xall[4 * tb:4 * tb + 4], writes=[pr])
                            S.op("act", lambda e, pp=pp, tsl=tsl: e.activation(
                                out=qA0[b_][0:64, tsl], in_=pp[0:64, :], func=AF.Copy, scale=0.125), reads=[pr], writes=["qA0_%d" % b_])
                            S.op("act", lambda e, pp=pp, tsl=tsl: e.activation(
                                out=q0B[b_][64:128, tsl], in_=pp[64:128, :], func=AF.Copy, scale=0.125), reads=[pr], writes=["q0B_%d" % b_])
                            yield
                        for tb in range(4):
                            tsl = slice(tb * 512, (tb + 1) * 512)
                            pp, pr = next_pp()
                            for c in range(8):
                                S.op("pe", lambda e, pp=pp, c=c, tsl=tsl, w_=w_: e.matmul(
                                    out=pp[:], lhsT=w_[:, c, 1, :], rhs=xnT[:, c, tsl], start=(c == 0), stop=(c == 7)),
                                    reads=[wr_] + xall[4 * tb:4 * tb + 4], writes=[pr])
                            S.op("dve", lambda e, pp=pp, tsl=tsl: e.tensor_copy(out=kAB[b_][:, tsl], in_=pp[:]),
                                 reads=[pr], writes=["kAB%d" % b_])
                            yield
                        for sub in range(nsub):
                            for blk in range(nblk):
                                ti = sub * nblk + blk
                                tsl = tok_slice(dil, sub, blk)
                                pp, pr = next_pp()
                                for c in range(8):
                                    S.op("pe", lambda e, pp=pp, c=c, tsl=tsl, w_=w_: e.matmul(
                                        out=pp[:, 0:128], lhsT=xnT[:, c, tsl], rhs=w_[:, c, 2, :],
                                        start=(c == 0), stop=(c == 7)), reads=[wr_] + xall, writes=[pr])
                                S.op("dve", lambda e, pp=pp, ti=ti: e.tensor_copy(
                                    out=vhp[b_][:, ti, :, 0:64], in_=pp[:, 0:128].rearrange("p (a b) -> p a b", a=2)),
                                    reads=[pr], writes=["vhp%d" % b_])
                                yield

                    def attn_gen(hd):
                        gi = hd // 4
                        hs = hd % 4
                        dil = GROUPS[gi][0]
                        nsub = dil
                        nblk = 16 // dil
                        b_ = (hd // 2) % 2
                        ab = hd % 2
                        qT = (qA0, q0B)[ab][b_]
                        qTr = ("qA0_%d", "q0B_%d")[ab] % b_
                        kT, kTr = kAB[b_], "kAB%d" % b_
                        vh, vhr = vhp[b_], "vhp%d" % b_
                        for sub in range(nsub):
                            for blk in range(nblk):
                                qsl = tok_slice(dil, sub, blk)
                                kts = [(blk + dk, dk + 1) for dk in (-1, 0, 1) if 0 <= blk + dk < nblk]
                                n = len(kts)
                                b0 = bias_index(hd, kts[0][1])
                                ri = rot[0] % 3
                                rot[0] += 1
                                psc, pscr = sc_banks[ri]
                                pob, pobr = pv_banks[ri]
                                ptb, ptr = pt[ri], "pt%d" % ri
                                S.op("pe", lambda e, psc=psc, n=n, b0=b0: e.matmul(
                                    out=psc[:, 0:n * 128], lhsT=identb[:], rhs=biasT[:, b0:b0 + n, :].rearrange("p a b -> p (a b)"),
                                    start=True, stop=False), reads=["identb", "biasT"], writes=[pscr])
                                for j, (kb, ty) in enumerate(kts):
                                    ksl = tok_slice(dil, sub, kb)
                                    S.op("pe", lambda e, psc=psc, j=j, n=n, ksl=ksl, qsl=qsl, qT=qT, kT=kT: e.matmul(
                                        out=psc[:, j * 128:(j + 1) * 128], lhsT=kT[:, ksl], rhs=qT[:, qsl],
                                        start=False, stop=(j == n - 1)), reads=[qTr, kTr], writes=[pscr])
                                S.op("act", lambda e, psc=psc, ptb=ptb, n=n: e.activation(
                                    out=ptb[:, 0:n * 128], in_=psc[:, 0:n * 128], func=AF.Exp), reads=[pscr], writes=[ptr])
                                for j, (kb, ty) in enumerate(kts):
                                    S.op("pe", lambda e, pob=pob, ptb=ptb, j=j, n=n, ti=sub * nblk + kb, vh=vh: e.matmul(
                                        out=pob[0:65, 0:128], lhsT=vh[:, ti, ab, :], rhs=ptb[:, j * 128:(j + 1) * 128],
                                        start=(j == 0), stop=(j == n - 1)), reads=[vhr, ptr], writes=[pobr])
                                S.op("dve", lambda e, pob=pob, hs=hs, qsl=qsl: e.tensor_tensor(
                                    out=accO[0:65, hs, qsl], in0=accO[0:65, hs, qsl], in1=pob[0:65, 0:128], op=ALU.add),
                                    reads=[pobr, "accO%d" % hs], writes=["accO%d" % hs])
                                yield

                    pp_banks[0] = [0, 1]
                    for _ in proj_gen(0):
                        pass
                    NPROJ, NATT = 24, 32
                    for p in range(6):
                        pg_ = proj_gen(p + 1) if p + 1 < 6 else None
                        done, step = 0, 0
                        for hd in (2 * p, 2 * p + 1):
                            for _ in attn_gen(hd):
                                step += 1
                                if pg_ is not None:
                                    want = (step * NPROJ + NATT - 1) // NATT
                                    while done < want:
                                        if next(pg_, "end") == "end":
                                            pg_ = None
                                            break
                                        done += 1
                        if pg_ is not None:
                            for _ in pg_:
                                pass
                    S.barrier()
                    ph2.close()
                    alloc_T()
                    pp_banks[0] = [0, 1, 2, 3, 4, 5]
                    rec = sb("rec", [128, 512], F32, ph)
                    for hs in range(4):
                        for tb in range(4):
                            tsl = slice(tb * 512, (tb + 1) * 512)
                            pp, pr = next_pp()
                            S.op("pe", lambda e, pp=pp, hs=hs, tsl=tsl: e.matmul(
                                out=pp[0:64, :], lhsT=sel65[0:65, :], rhs=accO[0:65, hs, tsl], start=True, stop=True),
                                reads=["cf", "accO%d" % hs], writes=[pr])
                            S.op("act", lambda e, pp=pp: e.activation(out=rec[0:64, :], in_=pp[0:64, :], func=AF.Ln),
                                 reads=[pr], writes=["rec"])
                            S.op("act", lambda e: e.activation(out=rec[0:64, :], in_=rec[0:64, :], func=AF.Exp, scale=-1.0),
                                 reads=["rec"], writes=["rec"])
                            S.op("dve", lambda e, hs=hs, tsl=tsl: e.tensor_tensor(
                                out=oT[0:64, hs, tsl], in0=accO[0:64, hs, tsl], in1=rec[0:64, :], op=ALU.mult),
                                reads=["rec", "accO%d" % hs], writes=["oT"])
                    S.barrier()
                mixedT = sb("mixedT", [128, 4, S_], BF16, mx)
                with ExitStack() as ph:
                    wu = sb("wu", [128, 8, 512], BF16, ph)
                    pw = sb("pw", [128, 4, 128], BF16, ph)
                    U2 = [sb("U%d" % i, [128, S_ + 32], F32, ph) for i in range(2)]
                    T1 = sb("T1", [128, S_ + 32], F32, ph)
                    T2 = sb("T2", [128, S_ + 32], F32, ph)
                    pooled = sb("pooled", [128, S_], BF16, ph)
                    S.dma("pool", lambda e: e.dma_start(
                        out=wu[:], in_=w_in_d[l][:, 2304:2816].rearrange("(c p) n -> p c n", p=128)), writes=["wu"])
                    S.dma("pool", lambda e: e.dma_start(
                        out=pw[:], in_=pool_w_d[l].rearrange("g c e -> c g e")), writes=["pw"])
                    for i in range(2):
                        S.op("pool", lambda e, i=i: e.memset(U2[i][:], 0.0), writes=["U%d" % i])

                    def uproj(g):
                        Ug, Ur = U2[g % 2], "U%d" % (g % 2)
                        for tb in range(4):
                            pp, pr = next_pp()
                            for c in range(8):
                                S.op("pe", lambda e, pp=pp, c=c, tb=tb, g=g: e.matmul(
                                    out=pp[:], lhsT=wu[:, c, g * 128:(g + 1) * 128], rhs=xnT[:, c, tb * 512:(tb + 1) * 512],
                                    start=(c == 0), stop=(c == 7)), reads=["wu"] + xall[4 * tb:4 * tb + 4], writes=[pr])
                            S.op("act", lambda e, pp=pp, tb=tb, Ug=Ug: e.copy(out=Ug[:, 16 + tb * 512:16 + (tb + 1) * 512], in_=pp[:]),
                                 reads=[pr], writes=[Ur])

                    uproj(0)
                    for g in range(4):
                        w = POOLW[g]
                        U, Ur = U2[g % 2], "U%d" % (g % 2)
                        if g + 1 < 4:
                            uproj(g + 1)
                        S.op("dve", lambda e, U=U: e.tensor_tensor(out=T1[:, 8:2072], in0=U[:, 7:2071], in1=U[:, 8:2072], op=ALU.add),
                             reads=[Ur], writes=["T1"])
                        cur, curr, oth, othr = T1, "T1", T2, "T2"
                        lo, hi, s = 8, 2072, 1
                        for lvl in range(g):
                            lo += 2 * s if lvl > 0 else 2
                            nlo, nhi = (10, 2070) if lvl == 0 else ((12, 2068) if lvl == 1 else (16, 2064))
                            S.op("dve", lambda e, cur=cur, oth=oth, s=s, nlo=nlo, nhi=nhi: e.tensor_tensor(
                                out=oth[:, nlo:nhi], in0=cur[:, nlo - s:nhi - s], in1=cur[:, nlo + s:nhi + s], op=ALU.add),
                                reads=[curr], writes=[othr])
                            cur, curr, oth, othr = oth, othr, cur, curr
                            s *= 2
                        S.op("dve", lambda e, cur=cur, oth=oth, w=w: e.tensor_scalar(
                            out=oth[:, 16:2064], in0=cur[:, 16:2064], scalar1=1.0 / w, scalar2=None, op0=ALU.mult),
                            reads=[curr], writes=[othr])
                        S.op("dve", lambda e, oth=oth, g=g: e.tensor_tensor(
                            out=oth[:, 16:24], in0=oth[:, 16:24], in1=cf[:, C_EDGE + g * 16:C_EDGE + g * 16 + 8], op=ALU.mult),
                            reads=["cf", othr], writes=[othr])
                        S.op("dve", lambda e, oth=oth, g=g: e.tensor_tensor(
                            out=oth[:, 2056:2064], in0=oth[:, 2056:2064], in1=cf[:, C_EDGE + g * 16 + 8:C_EDGE + g * 16 + 16],
                            op=ALU.mult), reads=["cf", othr], writes=[othr])
                        S.op("dve", lambda e, oth=oth, U=U: e.tensor_tensor(
                            out=pooled[:], in0=oth[:, 16:2064], in1=U[:, 16:2064], op=ALU.subtract),
                            reads=[othr, Ur], writes=["pooled"])
                        for tb in range(4):
                            pp, pr = next_pp()
                            S.op("pe", lambda e, pp=pp, g=g, tb=tb: e.matmul(
                                out=pp[:], lhsT=pw[:, g, :], rhs=pooled[:, tb * 512:(tb + 1) * 512], start=True, stop=True),
                                reads=["pw", "pooled"], writes=[pr])
                            S.op("act", lambda e, pp=pp, g=g, tb=tb: e.activation(
                                out=mixedT[:, g, tb * 512:(tb + 1) * 512], in_=pp[:], func=AF.Copy,
                                scale=pscale[:, l * 4 + g:l * 4 + g + 1]), reads=[pr, "pscale"], writes=["mixedT"])
                    S.barrier()
                with ExitStack() as ph:
                    wpa = sb("wpa", [128, 4, D], BF16, ph)
                    wpp = sb("wpp", [128, 4, D], BF16, ph)
                    wo = sb("wo", [128, 8, D], BF16, ph)
                    wgt = [sb("wgt%d" % i, [128, 8, 2, 128], BF16, ph) for i in range(2)]
                    g0 = sb("g0", [128, 512], F32, ph)
                    g1 = sb("g1", [128, 512], F32, ph)
                    t1 = sb("t1", [128, 512], F32, ph)
                    t2 = sb("t2", [128, 512], F32, ph)
                    yT = sb("yT", [128, 8, 512], BF16, ph)
                    S.dma("pool", lambda e: e.dma_start(
                        out=wpa[0:64, :, :], in_=w_pa_d[l].rearrange("(h p) n -> p h n", p=64)), writes=["wpa"])
                    S.dma("pool", lambda e: e.dma_start(
                        out=wpp[:], in_=w_pp_d[l].rearrange("(g p) n -> p g n", p=128)), writes=["wpp"])
                    S.dma("pool", lambda e: e.dma_start(
                        out=wo[:], in_=w_out_d[l].rearrange("(c p) n -> p c n", p=128)), writes=["wo"])
                    wgates = w_in_d[l][:, 2816:4864].rearrange("(c p) (t n) -> p c t n", p=128, t=2)
                    it = 0
                    for tb in range(4):
                        tsl = slice(tb * 512, (tb + 1) * 512)
                        xr = xall[4 * tb:4 * tb + 4]
                        for dc in range(8):
                            wg_ = wgt[it % 2]
                            wgr = "wgt%d" % (it % 2)
                            it += 1
                            for t2_ in range(2):
                                S.dma("pool", lambda e, wg_=wg_, dc=dc, t2_=t2_: e.dma_start(
                                    out=wg_[:, :, t2_, :], in_=wgates[:, :, t2_, dc * 128:(dc + 1) * 128]), writes=[wgr])
                            pc, pcr = next_pp()
                            for c in range(8):
                                S.op("pe", lambda e, pc=pc, c=c, wg_=wg_: e.matmul(
                                    out=pc[:], lhsT=wg_[:, c, 0, :], rhs=xnT[:, c, tsl], start=(c == 0), stop=(c == 7)),
                                    reads=[wgr] + xr, writes=[pcr])
                            pd, pdr = next_pp()
                            for c in range(8):
                                S.op("pe", lambda e, pd=pd, c=c, wg_=wg_: e.matmul(
                                    out=pd[:], lhsT=wg_[:, c, 1, :], rhs=xnT[:, c, tsl], start=(c == 0), stop=(c == 7)),
                                    reads=[wgr] + xr, writes=[pdr])
                            pa, par = next_pp()
                            for hs in range(4):
                                S.op("pe", lambda e, pa=pa, hs=hs, dc=dc: e.matmul(
                                    out=pa[:], lhsT=wpa[0:64, hs, dc * 128:(dc + 1) * 128], rhs=oT[0:64, hs, tsl],
                                    start=(hs == 0), stop=(hs == 3)), reads=["wpa", "oT"], writes=[par])
                            pb, pbr = next_pp()
                            for g in range(4):
                                S.op("pe", lambda e, pb=pb, g=g, dc=dc: e.matmul(
                                    out=pb[:], lhsT=wpp[:, g, dc * 128:(dc + 1) * 128], rhs=mixedT[:, g, tsl],
                                    start=(g == 0), stop=(g == 3)), reads=["wpp", "mixedT"], writes=[pbr])
                            S.op("act", lambda e, pc=pc: e.activation(out=g0[:], in_=pc[:], func=AF.Sigmoid),
                                 reads=[pcr], writes=["g0"])
                            S.op("act", lambda e, pd=pd: e.activation(out=g1[:], in_=pd[:], func=AF.Sigmoid),
                                 reads=[pdr], writes=["g1"])
                            S.op("dve", lambda e, pa=pa: e.tensor_tensor(out=t1[:], in0=g0[:], in1=pa[:], op=ALU.mult),
                                 reads=["g0", par], writes=["t1"])
                            S.op("dve", lambda e, pb=pb: e.tensor_tensor(out=t2[:], in0=g1[:], in1=pb[:], op=ALU.mult),
                                 reads=["g1", pbr], writes=["t2"])
                            S.op("dve", lambda e, dc=dc: e.tensor_tensor(out=yT[:, dc, :], in0=t1[:], in1=t2[:], op=ALU.add),
                                 reads=["t1", "t2"], writes=["yT"])
                        for ti in range(4):
                            tt = tb * 4 + ti
                            for nh in range(2):
                                pp, pr = next_pp()
                                for c in range(8):
                                    S.op("pe", lambda e, pp=pp, c=c, ti=ti, nh=nh: e.matmul(
                                        out=pp[:], lhsT=yT[:, c, ti * 128:(ti + 1) * 128], rhs=wo[:, c, nh * 512:(nh + 1) * 512],
                                        start=(c == 0), stop=(c == 7)), reads=["yT", "wo"], writes=[pr])
                                S.op("dve", lambda e, pp=pp, tt=tt, nh=nh: e.tensor_tensor(
                                    out=h[:, tt, nh * 512:(nh + 1) * 512], in0=h[:, tt, nh * 512:(nh + 1) * 512], in1=pp[:],
                                    op=ALU.add), reads=[pr, "h%d" % tt], writes=["h%d" % tt])
                    S.barrier()

        def moe(l):
            rms_stats(2 * l + 1)
            with ExitStack() as ph:
                desti = sb("desti", [128, NT, 2], I32, ph)
                cwt = sb("cwt", [128, NT, 2], F32, ph)
                NSLOT, NPRE = 8, 4
                wslot = [sb("ws%d" % i, [128, 4096], BF16, ph) for i in range(NPRE)]

                def wview(m):
                    t = wslot[m % NSLOT]
                    if m % 3 == 2:
                        return t[:].rearrange("p (c n) -> p c n", c=4)
                    return t[:].rearrange("p (c n) -> p c n", c=8)

                def issue_w(m):
                    if m >= 64 * 3:
                        return
                    ex_, j_ = m // 3, m % 3
                    if j_ == 2:
                        src = wd_d[l, ex_].rearrange("(c p) n -> p c n", p=128)
                        dst = wview(m)
                    else:
                        src = (wg_d, wu_d)[j_][l, ex_].rearrange("(c p r) n -> p c (r n)", p=128, r=2)
                        dst = wslot[m % NSLOT][:].rearrange("p (c n) -> p c n", c=4)
                    S.dma("pool", lambda e, src=src, dst=dst: e.dma_start(out=dst, in_=src), writes=["ws%d" % (m % NSLOT)])

                for m_ in range(NPRE):
                    issue_w(m_)
                rp = ExitStack()
                T = NT
                destf = sb("destf", [128, T, 2], F32, rp)
                wr = sb("wr", [128, 8, 72], F32, rp)
                xf2 = [sb("xf%d" % i, [128, D], F32, rp) for i in range(2)]
                xfT2 = [sb("xfT%d" % i, [128, 8, 128], F32, rp) for i in range(2)]
                lgA = sb("lgA", [128, T, 72], F32, rp)
                ohgA = sb("ohgA", [128, T, 8], F32, rp)
                t8A = sb("t8A", [128, T, 8], F32, rp)
                emA = sb("emA", [128, T, 64], F32, rp)
                em2A = sb("em2A", [128, T, 64], F32, rp)
                oh1A = sb("oh1A", [128, T, 64], F32, rp)
                oh2A = sb("oh2A", [128, T, 64], F32, rp)
                ohEA = sb("ohEA", [128, T, 64], F32, rp)
                t64A = sb("t64A", [128, T, 64], F32, rp)
                csA = sb("csA", [128, T, 64], F32, rp)
                rankA = sb("rankA", [128, T, 64], F32, rp)
                smA = sb("smA", [128, 16, T], F32, rp)
                iota64 = cf[:, C_IOTA:C_IOTA + 64]
                xtk = lambda tt, c0=0, n=1024: xnT[:, tt // 2, (tt % 2) * 1024 + c0:(tt % 2) * 1024 + c0 + n]
                S.dma("sp", lambda e: e.dma_start(out=wr[:], in_=wr_d[l].rearrange("(c p) n -> p c n", p=128)), writes=["wr"])
                S.op("pool", lambda e: e.memset(csA[:, 0, :], 0.0), writes=["csA"])
                def r_stage_a(tt):
                    xf = xf2[tt % 2]
                    xfr = "xf%d" % (tt % 2)
                    norm_tile(tt, xf[:], xfr)
                    S.op("pool", lambda e, tt=tt, xf=xf: e.tensor_copy(out=xtk(tt), in_=xf[:]), reads=[xfr], writes=["xtok%d" % tt])
                    xfT = xfT2[tt % 2]
                    for half in range(2):
                        bk = 2 + 2 * (tt % 2) + half
                        bname = "B%d" % bk
                        for c4 in range(4):
                            c = half * 4 + c4
                            S.op("pe", lambda e, c=c, c4=c4, bk=bk, xf=xf: e.transpose(
                                out=B[bk][:, c4 * 128:(c4 + 1) * 128], in_=xf[:, c * 128:(c + 1) * 128], identity=ident),
                                reads=[xfr, "cf"], writes=[bname])
                        xres = "xfT%d_%d" % (tt % 2, half)
                        if half == 0:
                            S.op("act", lambda e, bk=bk, xfT=xfT: e.copy(
                                out=xfT[:, 0:4, :], in_=B[bk][:].rearrange("p (a b) -> p a b", a=4)), reads=[bname], writes=[xres])
                        else:
                            S.op("dve", lambda e, bk=bk, xfT=xfT: e.tensor_copy(
                                out=xfT[:, 4:8, :], in_=B[bk][:].rearrange("p (a b) -> p a b", a=4)), reads=[bname], writes=[xres])

                def r_stage_b(tt):
                    xfT = xfT2[tt % 2]
                    pp, pr = next_pp()
                    for c in range(8):
                        S.op("pe", lambda e, c=c, pp=pp, xfT=xfT: e.matmul(out=pp[:, 0:72], lhsT=xfT[:, c, :], rhs=wr[:, c, :],
                                                                           start=(c == 0), stop=(c == 7)),
                             reads=["xfT%d_0" % (tt % 2), "xfT%d_1" % (tt % 2), "wr"], writes=[pr])
                    S.op("dve", lambda e, tt=tt, pp=pp: e.tensor_copy(out=lgA[:, tt, :], in_=pp[:, 0:72]), reads=[pr], writes=["lgA"])

                pp_banks[0] = [0, 1]
                for tt in range(NT):
                    r_stage_a(tt)
                    if tt > 0:
                        r_stage_b(tt - 1)
                r_stage_b(NT - 1)
                pp_banks[0] = [0, 1, 2, 3, 4, 5]
                sA = lambda j: smA[:, j, :]
                bc = lambda ap2, n: ap2.unsqueeze(2).to_broadcast([128, T, n])
                V = lambda fn, reads, writes: S.op("dve", fn, reads=reads, writes=writes)
                V(lambda e: e.tensor_reduce(out=sA(0), in_=lgA[:, :, 0:8], axis=AX.X, op=ALU.max), ["lgA"], ["smA"])
                V(lambda e: e.tensor_tensor(out=ohgA[:], in0=lgA[:, :, 0:8], in1=bc(sA(0), 8), op=ALU.is_equal),
                  ["lgA", "smA"], ["ohgA"])
                V(lambda e: e.tensor_tensor(out=t8A[:], in0=lgA[:, :, 0:8], in1=bc(sA(0), 8), op=ALU.subtract),
                  ["lgA", "smA"], ["t8A"])
                S.op("act", lambda e: e.activation(out=t8A[:], in_=t8A[:], func=AF.Exp), reads=["t8A"], writes=["t8A"])
                V(lambda e: e.tensor_reduce(out=sA(2), in_=t8A[:], axis=AX.X, op=ALU.add), ["t8A"], ["smA"])
                V(lambda e: e.reciprocal(out=sA(3), in_=sA(2)), ["smA"], ["smA"])
                V(lambda e: e.tensor_scalar(out=t8A[:], in0=ohgA[:], scalar1=-1.0, scalar2=1e9, op0=ALU.add, op1=ALU.mult),
                  ["ohgA", "t8A"], ["t8A"])
                V(lambda e: e.tensor_tensor(out=emA[:].rearrange("p t (g e) -> p t g e", g=8),
                                            in0=lgA[:, :, 8:72].rearrange("p t (g e) -> p t g e", g=8),
                                            in1=t8A[:].unsqueeze(3).to_broadcast([128, T, 8, 8]), op=ALU.add),
                  ["lgA", "t8A"], ["emA"])
                V(lambda e: e.tensor_reduce(out=sA(4), in_=emA[:], axis=AX.X, op=ALU.max), ["emA"], ["smA"])
                V(lambda e: e.tensor_tensor(out=oh1A[:], in0=emA[:], in1=bc(sA(4), 64), op=ALU.is_equal), ["emA", "smA"], ["oh1A"])
                V(lambda e: e.scalar_tensor_tensor(out=em2A[:], in0=oh1A[:], scalar=-1e9, in1=emA[:], op0=ALU.mult, op1=ALU.add),
                  ["oh1A", "emA"], ["em2A"])
                V(lambda e: e.tensor_reduce(out=sA(5), in_=em2A[:], axis=AX.X, op=ALU.max), ["em2A"], ["smA"])
                V(lambda e: e.tensor_tensor(out=oh2A[:], in0=em2A[:], in1=bc(sA(5), 64), op=ALU.is_equal), ["em2A", "smA"], ["oh2A"])
                V(lambda e: e.tensor_tensor(out=sA(6), in0=sA(5), in1=sA(4), op=ALU.subtract), ["smA"], ["smA"])
                S.op("act", lambda e: e.activation(out=sA(7), in_=sA(6), func=AF.Exp), reads=["smA"], writes=["smA"])
                V(lambda e: e.tensor_scalar(out=sA(8), in0=sA(7), scalar1=1.0, scalar2=None, op0=ALU.add), ["smA"], ["smA"])
                V(lambda e: e.reciprocal(out=sA(9), in_=sA(8)), ["smA"], ["smA"])
                V(lambda e: e.tensor_tensor(out=sA(10), in0=sA(9), in1=sA(3), op=ALU.mult), ["smA"], ["smA"])
                V(lambda e: e.tensor_tensor(out=sA(11), in0=sA(10), in1=sA(7), op=ALU.mult), ["smA"], ["smA"])
                V(lambda e: e.tensor_tensor(out=ohEA[:], in0=oh1A[:], in1=oh2A[:], op=ALU.add), ["oh1A", "oh2A"], ["ohEA"])
                for tt in range(1, NT):
                    V(lambda e, tt=tt: e.tensor_tensor(out=csA[:, tt, :], in0=csA[:, tt - 1, :], in1=ohEA[:, tt - 1, :], op=ALU.add),
                      ["csA", "ohEA"], ["csA"])
                for half in range(2):
                    pk, pkr = next_pp()
                    for t8 in range(8):
                        tt = half * 8 + t8
                        S.op("pe", lambda e, tt=tt, t8=t8, pk=pk: e.matmul(out=pk[:, t8 * 64:(t8 + 1) * 64], lhsT=tri,
                                                                            rhs=ohEA[:, tt, :], start=True, stop=False),
                             reads=["cf", "ohEA"], writes=[pkr])
                        S.op("pe", lambda e, tt=tt, t8=t8, pk=pk: e.matmul(out=pk[:, t8 * 64:(t8 + 1) * 64], lhsT=ones,
                                                                            rhs=csA[:, tt, :], start=False, stop=True),
                             reads=["cf", "csA"], writes=[pkr])
                    S.op("act", lambda e, half=half, pk=pk: e.copy(out=rankA[:, half * 8:(half + 1) * 8, :],
                                                                   in_=pk[:].rearrange("p (a c) -> p a c", c=64)),
                         reads=[pkr], writes=["rankA"])
                for k, ohk, ohr in ((0, oh1A, "oh1A"), (1, oh2A, "oh2A")):
                    V(lambda e, ohk=ohk: e.tensor_tensor(out=t64A[:], in0=ohk[:], in1=rankA[:], op=ALU.mult),
                      ["rankA", ohr, "t64A"], ["t64A"])
                    V(lambda e, k=k: e.tensor_reduce(out=sA(12 + k), in_=t64A[:], axis=AX.X, op=ALU.add), ["t64A"], ["smA"])
                    V(lambda e, ohk=ohk: e.tensor_tensor(out=t64A[:], in0=ohk[:], in1=iota64.unsqueeze(1).to_broadcast([128, T, 64]),
                                                         op=ALU.mult), ["cf", ohr, "t64A"], ["t64A"])
                    V(lambda e, k=k: e.tensor_reduce(out=sA(14 + k), in_=t64A[:], axis=AX.X, op=ALU.add), ["t64A"], ["smA"])
                    V(lambda e, k=k: e.scalar_tensor_tensor(out=destf[:, :, k], in0=sA(14 + k), scalar=float(CAPE), in1=sA(12 + k),
                                                            op0=ALU.mult, op1=ALU.add), ["smA"], ["destf"])
                    V(lambda e, k=k: e.tensor_scalar(out=sA(1), in0=sA(12 + k), scalar1=float(CAPE), scalar2=1e6,
                                                     op0=ALU.is_ge, op1=ALU.mult), ["smA"], ["smA"])
                    V(lambda e, k=k: e.tensor_tensor(out=destf[:, :, k], in0=destf[:, :, k], in1=sA(1), op=ALU.add),
                      ["smA", "destf"], ["destf"])
                    V(lambda e, k=k: e.tensor_scalar(out=sA(1), in0=sA(12 + k), scalar1=float(CAPE), scalar2=None, op0=ALU.is_lt),
                      ["smA"], ["smA"])
                    V(lambda e, k=k: e.tensor_tensor(out=cwt[:, :, k], in0=sA(10 + k), in1=sA(1), op=ALU.mult),
                      ["smA"], ["cwt"])
                S.op("dve", lambda e: e.tensor_copy(out=desti[:], in_=destf[:]), reads=["destf"], writes=["desti"])
                S.barrier()
                rp.close()
                xgs = []
                for tt in range(NT):
                    for k in range(2):
                        nm = "xgs%d" % (tt * 2 + k)
                        xgs.append(nm)
                        S.dma("pool", lambda e, tt=tt, k=k: e.indirect_dma_start(
                            out=xg_d, out_offset=bass.IndirectOffsetOnAxis(ap=desti[:, tt, k:k + 1], axis=0),
                            in_=xtk(tt), in_offset=None, bounds_check=bc_reg, oob_is_err=False),
                            reads=["xtok%d" % tt, "desti", "xg_init"], writes=[nm])
                xe = [sb("xe%d" % i, [128, D], BF16, ph) for i in range(2)]
                xeT = [sb("xeT%d" % i, [128, 8, 128], BF16, ph) for i in range(2)]
                wslot += [sb("ws%d" % i, [128, 4096], BF16, ph) for i in range(NPRE, NSLOT)]
                for m_ in range(NPRE, NSLOT):
                    issue_w(m_)
                sil = [sb("sil%d" % i, [128, 512], F32, ph) for i in range(2)]
                hw = [sb("hw%d" % i, [128, 512], BF16, ph) for i in range(2)]
                ye = [sb("ye%d" % i, [128, D], BF16, ph) for i in range(2)]
                gy = [sb("gy%d" % i, [128, D], BF16, ph) for i in range(4)]
                xnb2 = [sb("xnbm%d" % i, [128, D], BF16, ph) for i in range(2)] if l == 0 else None
                for i in range(4):
                    S.op("pool", lambda e, i=i: e.memset(gy[i][:], 0.0), writes=["gy%d" % i])
                ygs = []
                def stage_a(ex):
                    k = ex % 2
                    wg_v, wu_v = wview(3 * ex), wview(3 * ex + 1)
                    wg_r, wu_r = ["ws%d" % ((3 * ex + j_) % NSLOT) for j_ in range(2)]
                    S.dma("sp", lambda e, k=k, ex=ex: e.dma_start(out=xe[k][:], in_=xg_d[ex * CAPE:(ex + 1) * CAPE, :]),
                          reads=xgs, writes=["xe%d" % k])
                    for c in range(8):
                        S.op("pe", lambda e, c=c, k=k: e.transpose(out=Tb[k][:, c, :], in_=xe[k][:, (c // 2) * 256 + (c % 2):(c // 2) * 256 + 256:2],
                                                                   identity=identb[:]),
                             reads=["xe%d" % k, "identb"], writes=["T%d" % k])
                    S.op("act", lambda e, k=k: e.copy(out=xeT[k][:], in_=Tb[k][:]), reads=["T%d" % k], writes=["xeT%d" % k])
                    pg, pgr = next_pq()
                    for fc in range(4):
                        for c in range(8):
                            S.op("pe", lambda e, pg=pg, c=c, fc=fc, k=k: e.matmul(
                                out=pg[:, fc * 128:(fc + 1) * 128], lhsT=wg_v[:, c, fc * 128:(fc + 1) * 128], rhs=xeT[k][:, c, :],
                                start=(c == 0), stop=(c == 7)), reads=[wg_r, "xeT%d" % k], writes=[pgr])
                    issue_w(3 * ex + NSLOT)
                    pu, pur = next_pq()
                    for fc in range(4):
                        for c in range(8):
                            S.op("pe", lambda e, pu=pu, c=c, fc=fc, k=k: e.matmul(
                                out=pu[:, fc * 128:(fc + 1) * 128], lhsT=wu_v[:, c, fc * 128:(fc + 1) * 128], rhs=xeT[k][:, c, :],
                                start=(c == 0), stop=(c == 7)), reads=[wu_r, "xeT%d" % k], writes=[pur])
                    issue_w(3 * ex + 1 + NSLOT)
                    S.op("act", lambda e, pg=pg, k=k: e.activation(out=sil[k][:], in_=pg[:], func=AF.Silu),
                         reads=[pgr], writes=["sil%d" % k])
                    S.op("dve", lambda e, pu=pu, k=k: e.tensor_tensor(out=hw[k][:], in0=sil[k][:], in1=pu[:], op=ALU.mult),
                         reads=["sil%d" % k, pur], writes=["hw%d" % k])

                def stage_b(ex):
                    k = ex % 2
                    wd_v = wview(3 * ex + 2)
                    wd_r = "ws%d" % ((3 * ex + 2) % NSLOT)
                    for nh in range(2):
                        pp, pr = next_pq()
                        for fc in range(4):
                            S.op("pe", lambda e, pp=pp, fc=fc, nh=nh, k=k: e.matmul(
                                out=pp[:], lhsT=hw[k][:, fc * 128:(fc + 1) * 128], rhs=wd_v[:, fc, nh * 512:(nh + 1) * 512],
                                start=(fc == 0), stop=(fc == 3)), reads=["hw%d" % k, wd_r], writes=[pr])
                        if nh == 0:
                            S.op("act", lambda e, pp=pp, k=k: e.copy(out=ye[k][:, 0:512], in_=pp[:]), reads=[pr], writes=["ye%d" % k])
                        else:
                            S.op("dve", lambda e, pp=pp, k=k: e.tensor_copy(out=ye[k][:, 512:1024], in_=pp[:]),
                                 reads=[pr], writes=["ye%d" % k])
                    issue_w(3 * ex + 2 + NSLOT)
                    nm = "ygs%d" % ex
                    ygs.append(nm)
                    S.dma("sp", lambda e, k=k, ex=ex: e.dma_start(out=yg_d[ex * CAPE:(ex + 1) * CAPE, :], in_=ye[k][:]),
                          reads=["ye%d" % k], writes=[nm])

                for ex in range(64):
                    stage_a(ex)
                    if ex > 0:
                        stage_b(ex - 1)
                stage_b(63)
                S.dma("sp", lambda e: e.dma_start(out=gv[:], in_=gvec_d[:, 2 if l == 0 else 4, :]), writes=["gv"])
                gi_ = 0
                for tt in range(NT):
                    for k in range(2):
                        j = gi_ % 4
                        gi_ += 1
                        S.dma("pool", lambda e, tt=tt, k=k, j=j: e.indirect_dma_start(
                            out=gy[j][:, :], out_offset=None, in_=yg_d,
                            in_offset=bass.IndirectOffsetOnAxis(ap=desti[:, tt, k:k + 1], axis=0),
                            bounds_check=bc_reg, oob_is_err=False),
                            reads=ygs + ["desti"], writes=["gy%d" % j])
                        S.op("dve", lambda e, tt=tt, k=k, j=j: e.scalar_tensor_tensor(
                            out=h[:, tt, :], in0=gy[j][:], scalar=cwt[:, tt, k:k + 1], in1=h[:, tt, :],
                            op0=ALU.mult, op1=ALU.add), reads=["gy%d" % j, "cwt", "h%d" % tt], writes=["h%d" % tt])
                    rr = rms_tile(tt)
                    if l == 0:
                        xb, xr = xnb2[tt % 2], "xnbm%d" % (tt % 2)
                        norm_tile(tt, xb[:], xr, rres=rr)
                        Tt, Tr = Tb[tt % 2], "T%d" % (tt % 2)
                        for c in range(8):
                            S.op("pe", lambda e, c=c, xb=xb, Tt=Tt: e.transpose(out=Tt[:, c, :], in_=xb[:, c * 128:(c + 1) * 128],
                                                                                identity=identb[:]),
                                 reads=[xr, "identb"], writes=[Tr])
                        if tt % 2 == 0:
                            S.op("act", lambda e, tt=tt, Tt=Tt: e.copy(out=xnT[:, :, tt * 128:(tt + 1) * 128], in_=Tt[:]),
                                 reads=[Tr], writes=["xnT%d" % tt])
                        else:
                            S.op("dve", lambda e, tt=tt, Tt=Tt: e.tensor_copy(out=xnT[:, :, tt * 128:(tt + 1) * 128], in_=Tt[:]),
                                 reads=[Tr], writes=["xnT%d" % tt])
                    else:
                        for hf in range(2):
                            csl = slice(hf * 512, (hf + 1) * 512)
                            S.op("dve", lambda e, tt=tt, hf=hf, csl=csl: e.scalar_tensor_tensor(
                                out=sil[hf][:], in0=h[:, tt, csl], scalar=rstd[:, tt:tt + 1], in1=gv[:, csl],
                                op0=ALU.mult, op1=ALU.mult), reads=["h%d" % tt, rr, "gv"], writes=["sil%d" % hf])
                            S.dma("sp", lambda e, tt=tt, hf=hf, csl=csl: e.dma_start(out=out_d[tt * 128:(tt + 1) * 128, csl], in_=sil[hf][:]),
                                  reads=["sil%d" % hf])
                S.barrier()

        def dump_h(dst):
            for tt in range(NT):
                S.dma("sp", lambda e, tt=tt: e.dma_start(out=dst[tt * 128:(tt + 1) * 128, :], in_=h[:, tt, :]),
                      reads=["h%d" % tt])

        stages = [(0, "mix"), (0, "moe"), (1, "mix"), (1, "moe")]
        for si, (l, kind) in enumerate(stages):
            if kind == "mix":
                mixer(l)
            else:
                moe(l)
            if dbg and dbg == si + 1:
                dump_h(dbg_d)
        S.finish()
    return nc


def _t5_bucket(rel):
    nb = 16
    ret = np.where(rel > 0, nb, 0)
    n = np.abs(rel)
    max_exact = 8
    nf = np.maximum(n, max_exact).astype(np.float32)
    large = max_exact + (np.log(nf / np.float32(max_exact)) / np.float32(math.log(1024 / max_exact))
                         * np.float32(nb - max_exact)).astype(np.int32)
    large = np.minimum(large, nb - 1)
    return ret + np.where(n < max_exact, n, large)


def _host_consts(rel_bias):
    kk = np.arange(128)[:, None]
    qq = np.arange(128)[None, :]
    biasT = np.zeros((128, 28, 128), np.float32)
    for hd in range(12):
        dil = GROUPS[hd // 4][0]
        for ty in range(3):
            if hd >= 8 and ty != 1:
                continue
            rel = kk - qq + (ty - 1) * 128
            b = rel_bias[_t5_bucket(rel * dil), hd]
            biasT[:, bias_index(hd, ty), :] = np.where(np.abs(rel) <= 64, b, np.float32(NEGM))
    cf = np.zeros((128, C_N), np.float32)
    cf[:, C_ID:C_ID + 128] = np.eye(128, dtype=np.float32)
    cf[:, C_TRI:C_TRI + 128] = (kk < qq).astype(np.float32)
    cf[:, C_ONE:C_ONE + 128] = 1.0
    cf[:, C_IOTA:C_IOTA + CAPG] = np.arange(CAPG, dtype=np.float32)[None, :]
    for g, w in enumerate(POOLW):
        e = np.ones(16, np.float32)
        for i in range(8):
            cnt = min(i + w - w // 2, S_) - max(i - w // 2, 0)
            e[i] = w / cnt
            ii = S_ - 8 + i
            cnt = min(ii + w - w // 2, S_) - max(ii - w // 2, 0)
            e[8 + i] = w / cnt
        cf[:, C_EDGE + g * 16:C_EDGE + g * 16 + 16] = e[None, :]
    cf[64, C_SEL:C_SEL + 64] = 1.0
    return biasT, cf


_NC_CACHE = {}


def kernel(x, rel_bias, norm_mix_g, w_in, pool_w, pool_scale, w_proj_attn, w_proj_pool, w_out, norm_ffn_g,
           w_router_group, w_router_expert, w_gate_e, w_up_e, w_down_e, norm_final_g, _dbg=0, _cores=8):
    f = lambda a: np.ascontiguousarray(np.asarray(a, dtype=np.float32))
    x = f(x)
    rel_bias = f(rel_bias)
    biasT, cf = _host_consts(rel_bias)
    gvec = np.stack([f(norm_mix_g)[0], f(norm_ffn_g)[0], f(norm_mix_g)[1], f(norm_ffn_g)[1], f(norm_final_g)], 0)
    gvec = np.ascontiguousarray(np.broadcast_to(gvec[None], (128, 5, D)))
    pscale = np.ascontiguousarray(f(pool_scale).reshape(2, 4, 128).transpose(2, 0, 1).reshape(128, 8))
    wre = f(w_router_expert).transpose(0, 2, 1, 3).reshape(2, D, 64)
    wr = np.ascontiguousarray(np.concatenate([f(w_router_group), wre], axis=2))
    shared = {
        "w_in": f(w_in), "pool_w": f(pool_w), "w_proj_attn": f(w_proj_attn), "w_proj_pool": f(w_proj_pool),
        "w_out": f(w_out), "wr": wr, "w_gate_e": f(w_gate_e), "w_up_e": f(w_up_e), "w_down_e": f(w_down_e),
        "gvec": gvec, "pscale": pscale, "biasT": biasT, "cf32": cf,
    }
    key = _dbg
    if key not in _NC_CACHE:
        _NC_CACHE[key] = build(_dbg)
    nc = _NC_CACHE[key]
    in_maps = []
    for b in range(_cores):
        m = dict(shared)
        m["x"] = np.ascontiguousarray(x[b])
        in_maps.append(m)
    res = run_bass_kernel_spmd(nc, in_maps, core_ids=list(range(_cores)))
    out = np.stack([res.results[b]["out"] for b in range(_cores)], 0).astype(np.float32)
    if _dbg:
        return out, np.stack([res.results[b]["dbg"] for b in range(_cores)], 0)
    return out
```

```python
import math
from contextlib import ExitStack

import numpy as np
import concourse.bass as bass
import concourse.mybir as mybir
from concourse.bass_utils import run_bass_kernel_spmd

F32 = mybir.dt.float32
BF16 = mybir.dt.bfloat16
I32 = mybir.dt.int32
ALU = mybir.AluOpType
AF = mybir.ActivationFunctionType
AX = mybir.AxisListType

S_ = 2048
D = 1024
NT = 16
INW = 4864
CAPG = 384
CAPE = 128
NEGM = -30000.0
GROUPS = ((1, 0), (4, 1), (16, 2))
POOLW = (2, 4, 8, 16)
C_ID, C_TRI, C_ONE, C_IOTA, C_EDGE, C_SEL = 0, 128, 256, 384, 768, 832
C_N = 896


class Sched:
    ENG = ["pe", "act", "dve", "pool", "sp"]
    RING = 12

    def __init__(self, nc, es):
        self.nc = nc
        self.obj = {"pe": nc.tensor, "act": nc.scalar, "dve": nc.vector, "pool": nc.gpsimd, "sp": nc.sync}
        self.sem = {e: es.enter_context(nc.semaphore("s_" + e)) for e in self.ENG}
        self.cnt = {e: 0 for e in self.ENG}
        self.seen = {x: {} for x in self.ENG}
        self.lastw = {}
        self.reads = {}
        self.ring = {}
        self.ring_i = {}
        for q in ("sp", "pool"):
            self.ring[q] = []
            self.ring_i[q] = 0
            for i in range(self.RING):
                name = "q_%s_%d" % (q, i)
                self.sem[name] = es.enter_context(nc.semaphore(name))
                self.cnt[name] = 0
                self.ring[q].append(name)

    def _dep(self, x, e, ts, raw=False):
        if e == x and x in ("pe", "sp"):
            return
        if self.seen[x].get(e, 0) < ts:
            self.seen[x][e] = ts
            self.obj[x].wait_ge(self.sem[e], ts)

    def _deps(self, x, reads, writes):
        for r in reads:
            w = self.lastw.get(r)
            if w is not None:
                self._dep(x, w[0], w[1], raw=True)
        for r in writes:
            w = self.lastw.get(r)
            if w is not None:
                self._dep(x, *w)
            rd = self.reads.get(r)
            if rd:
                for e, ts in rd.items():
                    self._dep(x, e, ts)

    def _mark(self, who, ts, reads, writes):
        for r in reads:
            self.reads.setdefault(r, {})[who] = ts
        for r in writes:
            self.lastw[r] = (who, ts)
            self.reads[r] = {}

    def op(self, x, fn, reads=(), writes=()):
        self._deps(x, reads, writes)
        self.cnt[x] += 1
        ts = self.cnt[x]
        fn(self.obj[x]).then_inc(self.sem[x], 1)
        self._mark(x, ts, reads, writes)

    def dma(self, q, fn, reads=(), writes=()):
        self._deps(q, reads, writes)
        i = self.ring_i[q]
        self.ring_i[q] = (i + 1) % self.RING
        name = self.ring[q][i]
        if self.cnt[name] > 0:
            self._dep(q, name, self.cnt[name])
        self.cnt[name] += 16
        ts = self.cnt[name]
        fn(self.obj[q]).then_inc(self.sem[name], 16)
        self._mark(name, ts, reads, writes)

    def barrier(self):
        for x in self.ENG:
            for e in self.cnt:
                if self.cnt[e] > 0:
                    self._dep(x, e, self.cnt[e])

    def finish(self):
        for name in self.cnt:
            if name.startswith("q_") and self.cnt[name] > 0:
                self._dep("sp", name, self.cnt[name])
        for e in self.ENG:
            if self.cnt[e] > 0:
                self._dep("sp", e, self.cnt[e])


def bias_index(h, ty):
    return h * 3 + ty if h < 8 else 24 + (h - 8)


def tok_slice(dil, sub, blk):
    st = sub + dil * 128 * blk
    return slice(st, st + dil * 127 + 1, dil)


def build(dbg=0):
    nc = bass.Bass("TRN2", target_bir_lowering=False)
    dt = lambda name, shape, d=F32, kind="ExternalInput": nc.dram_tensor(name, shape, d, kind=kind).ap()
    x_d = dt("x", [S_, D])
    w_in_d = dt("w_in", [2, D, INW])
    pool_w_d = dt("pool_w", [2, 4, 128, 128])
    w_pa_d = dt("w_proj_attn", [2, 256, D])
    w_pp_d = dt("w_proj_pool", [2, 512, D])
    w_out_d = dt("w_out", [2, D, D])
    wr_d = dt("wr", [2, D, 72])
    wg_d = dt("w_gate_e", [2, 64, D, 512])
    wu_d = dt("w_up_e", [2, 64, D, 512])
    wd_d = dt("w_down_e", [2, 64, 512, D])
    gvec_d = dt("gvec", [128, 5, D])
    pscale_d = dt("pscale", [128, 8])
    bias_d = dt("biasT", [128, 28, 128])
    cf_d = dt("cf32", [128, C_N])
    xg_d = dt("xg_scr", [64 * CAPE, D], BF16, kind="Internal")
    yg_d = dt("yg_scr", [64 * CAPE, D], BF16, kind="Internal")
    out_d = dt("out", [S_, D], kind="ExternalOutput")
    dbg_d = dt("dbg", [S_, D], kind="ExternalOutput") if dbg else None

    with ExitStack() as es:
        uid = [0]

        def sb(name, shape, d=F32, st=es):
            uid[0] += 1
            return st.enter_context(nc.sbuf_tensor("sb%d_%s" % (uid[0], name), shape, d))

        psum = lambda name, shape, d=F32: es.enter_context(nc.psum_tensor("ps_" + name, shape, d))
        S = Sched(nc, es)
        h = sb("h", [128, NT, D])
        xnT = sb("xnT", [128, 8, S_], BF16)
        biasT = sb("biasT", [128, 28, 128], BF16)
        cf = sb("cf", [128, C_N])
        gv = sb("gv", [128, D])
        pscale = sb("pscale", [128, 8])
        identb = sb("identb", [128, 128], BF16)
        ssq = sb("ssq", [128, NT])
        rstd = sb("rstd", [128, NT])
        junk = sb("junk", [128, D], BF16)
        B = [psum("B%d" % i, [128, 512]) for i in range(6)]
        tgen = [0]
        tstack = [ExitStack()]
        Tb = [None, None]

        def alloc_T():
            tgen[0] += 1
            tstack[0] = ExitStack()
            for i in range(2):
                Tb[i] = tstack[0].enter_context(nc.psum_tensor("ps_T%d_%d" % (i, tgen[0]), [128, 8, 128], BF16))

        alloc_T()
        es.callback(lambda: tstack[0].close())
        ident = cf[:, C_ID:C_ID + 128]
        tri = cf[:, C_TRI:C_TRI + 128]
        ones = cf[:, C_ONE:C_ONE + 128]
        iota = cf[:, C_IOTA:C_IOTA + CAPG]
        sel65 = cf[:, C_SEL:C_SEL + 64]

        S.dma("sp", lambda e: e.dma_start(out=cf[:], in_=cf_d), writes=["cf"])
        S.dma("pool", lambda e: e.dma_start(out=biasT[:], in_=bias_d), writes=["biasT"])
        S.dma("sp", lambda e: e.dma_start(out=pscale[:], in_=pscale_d), writes=["pscale"])
        for tt in range(NT):
            S.dma("sp", lambda e, tt=tt: e.dma_start(out=h[:, tt, :], in_=x_d[tt * 128:(tt + 1) * 128, :]),
                  writes=["h%d" % tt])
        S.op("dve", lambda e: e.tensor_copy(out=identb[:], in_=ident), reads=["cf"], writes=["identb"])

        pp_i = [0]
        pp_banks = [[0, 1, 2, 3, 4, 5]]

        def next_pp():
            lst = pp_banks[0]
            pp_i[0] = (pp_i[0] + 1) % len(lst)
            i = lst[pp_i[0]]
            return B[i], "B%d" % i

        bc_reg = nc.gpsimd.alloc_register("bc_reg")
        nc.gpsimd.reg_mov(bc_reg, 64 * CAPE - 1)
        pq_i = [0]
        pq_list = [(B[i], "B%d" % i) for i in range(6)]

        def next_pq():
            i = pq_i[0]
            pq_i[0] = (i + 1) % 6
            return pq_list[i]

        def rms_stats(gidx):
            S.dma("sp", lambda e: e.dma_start(out=gv[:], in_=gvec_d[:, gidx, :]), writes=["gv"])
            for tt in range(NT):
                S.op("act", lambda e, tt=tt: e.activation(out=junk[:], in_=h[:, tt, :], func=AF.Square,
                                                          accum_out=ssq[:, tt:tt + 1]),
                     reads=["h%d" % tt], writes=["junk", "ssq"])
            S.op("dve", lambda e: e.tensor_scalar(out=rstd[:], in0=ssq[:], scalar1=1.0 / D, scalar2=1e-6,
                                                  op0=ALU.mult, op1=ALU.add), reads=["ssq"], writes=["rstd"])
            S.op("act", lambda e: e.activation(out=rstd[:], in_=rstd[:], func=AF.Sqrt), reads=["rstd"], writes=["rstd"])
            S.op("dve", lambda e: e.reciprocal(out=rstd[:], in_=rstd[:]), reads=["rstd"], writes=["rstd"])

        def norm_tile(tt, out_ap, out_res, rres="rstd"):
            S.op("dve", lambda e: e.scalar_tensor_tensor(out=out_ap, in0=h[:, tt, :], scalar=rstd[:, tt:tt + 1],
                                                         in1=gv[:], op0=ALU.mult, op1=ALU.mult),
                 reads=["h%d" % tt, rres, "gv"], writes=[out_res])

        def rms_tile(tt):
            rms_a(tt)
            return rms_b(tt)

        def rms_a(tt):
            rr = "rstd_t%d" % tt
            S.op("act", lambda e: e.activation(out=junk[:], in_=h[:, tt, :], func=AF.Square, accum_out=ssq[:, tt:tt + 1]),
                 reads=["h%d" % tt], writes=["junk", "ssq_t%d" % tt])
            S.op("dve", lambda e: e.tensor_scalar(out=rstd[:, tt:tt + 1], in0=ssq[:, tt:tt + 1], scalar1=1.0 / D, scalar2=1e-6,
                                                  op0=ALU.mult, op1=ALU.add), reads=["ssq_t%d" % tt], writes=[rr])

        def rms_b(tt):
            rr = "rstd_t%d" % tt
            S.op("act", lambda e: e.activation(out=rstd[:, tt:tt + 1], in_=rstd[:, tt:tt + 1], func=AF.Ln), reads=[rr], writes=[rr])
            S.op("act", lambda e: e.activation(out=rstd[:, tt:tt + 1], in_=rstd[:, tt:tt + 1], func=AF.Exp, scale=-0.5),
                 reads=[rr], writes=[rr])
            return rr

        def mixer(l):
            m0 = ExitStack()
            if l == 0:
                rms_stats(2 * l)
                xnb = [sb("xnb%d" % i, [128, D], BF16, m0) for i in range(2)]
            for tt in (range(NT) if l == 0 else ()):
                xb = xnb[tt % 2]
                xr = "xnb%d" % (tt % 2)
                norm_tile(tt, xb[:], xr)
                Tt, Tr = Tb[tt % 2], "T%d" % (tt % 2)
                for c in range(8):
                    S.op("pe", lambda e, c=c, xb=xb, Tt=Tt: e.transpose(out=Tt[:, c, :], in_=xb[:, c * 128:(c + 1) * 128],
                                                                        identity=identb[:]),
                         reads=[xr, "identb"], writes=[Tr])
                if tt % 2 == 0:
                    S.op("act", lambda e, tt=tt, Tt=Tt: e.copy(out=xnT[:, :, tt * 128:(tt + 1) * 128], in_=Tt[:]),
                         reads=[Tr], writes=["xnT%d" % tt])
                else:
                    S.op("dve", lambda e, tt=tt, Tt=Tt: e.tensor_copy(out=xnT[:, :, tt * 128:(tt + 1) * 128], in_=Tt[:]),
                         reads=[Tr], writes=["xnT%d" % tt])
            xall = ["xnT%d" % t for t in range(NT)]
            S.barrier()
            m0.close()
            if l == 0:
                S.op("pool", lambda e: e.memset(junk[:], 0.0), writes=["junk"])
                for a_ in range(64 * CAPE // 128):
                    S.dma("sp", lambda e, a_=a_: e.dma_start(out=xg_d[a_ * 128:(a_ + 1) * 128, :], in_=junk[:]),
                          reads=["junk"], writes=["xg_init"])
            with ExitStack() as mx:
                oT = sb("oT", [128, 4, S_], BF16, mx)
                with ExitStack() as ph:
                    accO = sb("accO", [128, 4, S_], F32, ph)
                    S.op("pool", lambda e: e.memset(accO[:], 0.0), writes=["accO%d" % i for i in range(4)])
                    ph2 = ExitStack()
                    wh = [sb("wh%d" % i, [128, 8, 3, 128], BF16, ph2) for i in range(2)]
                    qA0 = [sb("qA0_%d" % i, [128, S_], BF16, ph2) for i in range(2)]
                    q0B = [sb("q0B_%d" % i, [128, S_], BF16, ph2) for i in range(2)]
                    kAB = [sb("kAB%d" % i, [128, S_], BF16, ph2) for i in range(2)]
                    vhp = [sb("vhp%d" % i, [128, NT, 2, 65], BF16, ph2) for i in range(2)]
                    pt = [sb("pt%d" % i, [128, 384], BF16, ph2) for i in range(3)]
                    tstack[0].close()
                    Bx = [ph2.enter_context(nc.psum_tensor("ps_Bx%d_%d" % (i, tgen[0]), [128, 512], F32)) for i in range(2)]
                    sc_banks = [(B[2], "B2"), (B[3], "B3"), (Bx[0], "Bx0")]
                    pv_banks = [(B[4], "B4"), (B[5], "B5"), (Bx[1], "Bx1")]
                    for i in range(2):
                        S.op("pool", lambda e, i=i: e.memset(vhp[i][:], 1.0), writes=["vhp%d" % i])
                        S.op("pool", lambda e, i=i: e.memset(qA0[i][:], 0.0), writes=["qA0_%d" % i])
                        S.op("pool", lambda e, i=i: e.memset(q0B[i][:], 0.0), writes=["q0B_%d" % i])
                    wqkv = w_in_d[l][:, 0:2304].rearrange("(c p) (t n) -> p c t n", p=128, t=3)
                    rot = [0]

                    def proj_gen(p):
                        gi = p // 2
                        dil = GROUPS[gi][0]
                        nsub = dil
                        nblk = 16 // dil
                        b_ = p % 2
                        w_ = wh[b_]
                        wr_ = "wh%d" % b_
                        for t3 in range(3):
                            S.dma("pool", lambda e, w_=w_, p=p, t3=t3: e.dma_start(
                                out=w_[:, :, t3, :], in_=wqkv[:, :, t3, p * 128:(p + 1) * 128]), writes=[wr_])
                        for tb in range(4):
                            tsl = slice(tb * 512, (tb + 1) * 512)
                            pp, pr = next_pp()
                            for c in range(8):
                                S.op("pe", lambda e, pp=pp, c=c, tsl=tsl, w_=w_: e.matmul(
                                    out=pp[:], lhsT=w_[:, c, 0, :], rhs=xnT[:, c, tsl], start=(c == 0), stop=(c == 7)),
                                    reads=[wr_] + xall[4 * tb:4 * tb + 4], writes=[pr])
                            S.op("act", lambda e, pp=pp, tsl=tsl: e.activation(
                                out=qA0[b_][0:64, tsl], in_=pp[0:64, :], func=AF.Copy, scale=0.125), reads=[pr], writes=["qA0_%d" % b_])
                            S.op("act", lambda e, pp=pp, tsl=tsl: e.activation(
                                out=q0B[b_][64:128, tsl], in_=pp[64:128, :], func=AF.Copy, scale=0.125), reads=[pr], writes=["q0B_%d" % b_])
                            yield
                        for tb in range(4):
                            tsl = slice(tb * 512, (tb + 1) * 512)
                            pp, pr = next_pp()
                            for c in range(8):
                                S.op("pe", lambda e, pp=pp, c=c, tsl=tsl, w_=w_: e.matmul(
                                    out=pp[:], lhsT=w_[:, c, 1, :], rhs=xnT[:, c, tsl], start=(c == 0), stop=(c == 7)),
                                    reads=[wr_] + xall[4 * tb:4 * tb + 4], writes=[pr])
                            S.op("dve", lambda e, pp=pp, tsl=tsl: e.tensor_copy(out=kAB[b_][:, tsl], in_=pp[:]),
                                 reads=[pr], writes=["kAB%d" % b_])
                            yield
                        for sub in range(nsub):
                            for blk in range(nblk):
                                ti = sub * nblk + blk
                                tsl = tok_slice(dil, sub, blk)
                                pp, pr = next_pp()
                                for c in range(8):
                                    S.op("pe", lambda e, pp=pp, c=c, tsl=tsl, w_=w_: e.matmul(
                                        out=pp[:, 0:128], lhsT=xnT[:, c, tsl], rhs=w_[:, c, 2, :],
                                        start=(c == 0), stop=(c == 7)), reads=[wr_] + xall, writes=[pr])
                                S.op("dve", lambda e, pp=pp, ti=ti: e.tensor_copy(
                                    out=vhp[b_][:, ti, :, 0:64], in_=pp[:, 0:128].rearrange("p (a b) -> p a b", a=2)),
                                    reads=[pr], writes=["vhp%d" % b_])
                                yield

                    def attn_gen(hd):
                        gi = hd // 4
                        hs = hd % 4
                        dil = GROUPS[gi][0]
                        nsub = dil
                        nblk = 16 // dil
                        b_ = (hd // 2) % 2
                        ab = hd % 2
                        qT = (qA0, q0B)[ab][b_]
                        qTr = ("qA0_%d", "q0B_%d")[ab] % b_
                        kT, kTr = kAB[b_], "kAB%d" % b_
                        vh, vhr = vhp[b_], "vhp%d" % b_
                        for sub in range(nsub):
                            for blk in range(nblk):
                                qsl = tok_slice(dil, sub, blk)
                                kts = [(blk + dk, dk + 1) for dk in (-1, 0, 1) if 0 <= blk + dk < nblk]
                                n = len(kts)
                                b0 = bias_index(hd, kts[0][1])
                                ri = rot[0] % 3
                                rot[0] += 1
                                psc, pscr = sc_banks[ri]
                                pob, pobr = pv_banks[ri]
                                ptb, ptr = pt[ri], "pt%d" % ri
                                S.op("pe", lambda e, psc=psc, n=n, b0=b0: e.matmul(
                                    out=psc[:, 0:n * 128], lhsT=identb[:], rhs=biasT[:, b0:b0 + n, :].rearrange("p a b -> p (a b)"),
                                    start=True, stop=False), reads=["identb", "biasT"], writes=[pscr])
                                for j, (kb, ty) in enumerate(kts):
                                    ksl = tok_slice(dil, sub, kb)
                                    S.op("pe", lambda e, psc=psc, j=j, n=n, ksl=ksl, qsl=qsl, qT=qT, kT=kT: e.matmul(
                                        out=psc[:, j * 128:(j + 1) * 128], lhsT=kT[:, ksl], rhs=qT[:, qsl],
                                        start=False, stop=(j == n - 1)), reads=[qTr, kTr], writes=[pscr])
                                S.op("act", lambda e, psc=psc, ptb=ptb, n=n: e.activation(
                                    out=ptb[:, 0:n * 128], in_=psc[:, 0:n * 128], func=AF.Exp), reads=[pscr], writes=[ptr])
                                for j, (kb, ty) in enumerate(kts):
                                    S.op("pe", lambda e, pob=pob, ptb=ptb, j=j, n=n, ti=sub * nblk + kb, vh=vh: e.matmul(
                                        out=pob[0:65, 0:128], lhsT=vh[:, ti, ab, :], rhs=ptb[:, j * 128:(j + 1) * 128],
                                        start=(j == 0), stop=(j == n - 1)), reads=[vhr, ptr], writes=[pobr])
                                S.op("dve", lambda e, pob=pob, hs=hs, qsl=qsl: e.tensor_tensor(
                                    out=accO[0:65, hs, qsl], in0=accO[0:65, hs, qsl], in1=pob[0:65, 0:128], op=ALU.add),
                                    reads=[pobr, "accO%d" % hs], writes=["accO%d" % hs])
                                yield

                    pp_banks[0] = [0, 1]
                    for _ in proj_gen(0):
                        pass
                    NPROJ, NATT = 24, 32
                    for p in range(6):
                        pg_ = proj_gen(p + 1) if p + 1 < 6 else None
                        done, step = 0, 0
                        for hd in (2 * p, 2 * p + 1):
                            for _ in attn_gen(hd):
                                step += 1
                                if pg_ is not None:
                                    want = (step * NPROJ + NATT - 1) // NATT
                                    while done < want:
                                        if next(pg_, "end") == "end":
                                            pg_ = None
                                            break
                                        done += 1
                        if pg_ is not None:
                            for _ in pg_:
                                pass
                    S.barrier()
                    ph2.close()
                    alloc_T()
                    pp_banks[0] = [0, 1, 2, 3, 4, 5]
                    rec = sb("rec", [128, 512], F32, ph)
                    for hs in range(4):
                        for tb in range(4):
                            tsl = slice(tb * 512, (tb + 1) * 512)
                            pp, pr = next_pp()
                            S.op("pe", lambda e, pp=pp, hs=hs, tsl=tsl: e.matmul(
                                out=pp[0:64, :], lhsT=sel65[0:65, :], rhs=accO[0:65, hs, tsl], start=True, stop=True),
                                reads=["cf", "accO%d" % hs], writes=[pr])
                            S.op("act", lambda e, pp=pp: e.activation(out=rec[0:64, :], in_=pp[0:64, :], func=AF.Ln),
                                 reads=[pr], writes=["rec"])
                            S.op("act", lambda e: e.activation(out=rec[0:64, :], in_=rec[0:64, :], func=AF.Exp, scale=-1.0),
                                 reads=["rec"], writes=["rec"])
                            S.op("dve", lambda e, hs=hs, tsl=tsl: e.tensor_tensor(
                                out=oT[0:64, hs, tsl], in0=accO[0:64, hs, tsl], in1=rec[0:64, :], op=ALU.mult),
                                reads=["rec", "accO%d" % hs], writes=["oT"])
                    S.barrier()
                mixedT = sb("mixedT", [128, 4, S_], BF16, mx)
                with ExitStack() as ph:
                    wu = sb("wu", [128, 8, 512], BF16, ph)
                    pw = sb("pw", [128, 4, 128], BF16, ph)
                    U2 = [sb("U%d" % i, [128, S_ + 32], F32, ph) for i in range(2)]
                    T1 = sb("T1", [128, S_ + 32], F32, ph)
                    T2 = sb("T2", [128, S_ + 32], F32, ph)
                    pooled = sb("pooled", [128, S_], BF16, ph)
                    S.dma("pool", lambda e: e.dma_start(
                        out=wu[:], in_=w_in_d[l][:, 2304:2816].rearrange("(c p) n -> p c n", p=128)), writes=["wu"])
                    S.dma("pool", lambda e: e.dma_start(
                        out=pw[:], in_=pool_w_d[l].rearrange("g c e -> c g e")), writes=["pw"])
                    for i in range(2):
                        S.op("pool", lambda e, i=i: e.memset(U2[i][:], 0.0), writes=["U%d" % i])

                    def uproj(g):
                        Ug, Ur = U2[g % 2], "U%d" % (g % 2)
                        for tb in range(4):
                            pp, pr = next_pp()
                            for c in range(8):
                                S.op("pe", lambda e, pp=pp, c=c, tb=tb, g=g: e.matmul(
                                    out=pp[:], lhsT=wu[:, c, g * 128:(g + 1) * 128], rhs=xnT[:, c, tb * 512:(tb + 1) * 512],
                                    start=(c == 0), stop=(c == 7)), reads=["wu"] + xall[4 * tb:4 * tb + 4], writes=[pr])
                            S.op("act", lambda e, pp=pp, tb=tb, Ug=Ug: e.copy(out=Ug[:, 16 + tb * 512:16 + (tb + 1) * 512], in_=pp[:]),
                                 reads=[pr], writes=[Ur])

                    uproj(0)
                    for g in range(4):
                        w = POOLW[g]
                        U, Ur = U2[g % 2], "U%d" % (g % 2)
                        if g + 1 < 4:
                            uproj(g + 1)
                        S.op("dve", lambda e, U=U: e.tensor_tensor(out=T1[:, 8:2072], in0=U[:, 7:2071], in1=U[:, 8:2072], op=ALU.add),
                             reads=[Ur], writes=["T1"])
                        cur, curr, oth, othr = T1, "T1", T2, "T2"
                        lo, hi, s = 8, 2072, 1
                        for lvl in range(g):
                            lo += 2 * s if lvl > 0 else 2
                            nlo, nhi = (10, 2070) if lvl == 0 else ((12, 2068) if lvl == 1 else (16, 2064))
                            S.op("dve", lambda e, cur=cur, oth=oth, s=s, nlo=nlo, nhi=nhi: e.tensor_tensor(
                                out=oth[:, nlo:nhi], in0=cur[:, nlo - s:nhi - s], in1=cur[:, nlo + s:nhi + s], op=ALU.add),
                                reads=[curr], writes=[othr])
                            cur, curr, oth, othr = oth, othr, cur, curr
                            s *= 2
                        S.op("dve", lambda e, cur=cur, oth=oth, w=w: e.tensor_scalar(
                            out=oth[:, 16:2064], in0=cur[:, 16:2064], scalar1=1.0 / w, scalar2=None, op0=ALU.mult),
                            reads=[curr], writes=[othr])
                        S.op("dve", lambda e, oth=oth, g=g: e.tensor_tensor(
                            out=oth[:, 16:24], in0=oth[:, 16:24], in1=cf[:, C_EDGE + g * 16:C_EDGE + g * 16 + 8], op=ALU.mult),
                            reads=["cf", othr], writes=[othr])
                        S.op("dve", lambda e, oth=oth, g=g: e.tensor_tensor(
                            out=oth[:, 2056:2064], in0=oth[:, 2056:2064], in1=cf[:, C_EDGE + g * 16 + 8:C_EDGE + g * 16 + 16],
                            op=ALU.mult), reads=["cf", othr], writes=[othr])
                        S.op("dve", lambda e, oth=oth, U=U: e.tensor_tensor(
                            out=pooled[:], in0=oth[:, 16:2064], in1=U[:, 16:2064], op=ALU.subtract),
                            reads=[othr, Ur], writes=["pooled"])
                        for tb in range(4):
                            pp, pr = next_pp()
                            S.op("pe", lambda e, pp=pp, g=g, tb=tb: e.matmul(
                                out=pp[:], lhsT=pw[:, g, :], rhs=pooled[:, tb * 512:(tb + 1) * 512], start=True, stop=True),
                                reads=["pw", "pooled"], writes=[pr])
                            S.op("act", lambda e, pp=pp, g=g, tb=tb: e.activation(
                                out=mixedT[:, g, tb * 512:(tb + 1) * 512], in_=pp[:], func=AF.Copy,
                                scale=pscale[:, l * 4 + g:l * 4 + g + 1]), reads=[pr, "pscale"], writes=["mixedT"])
                    S.barrier()
                with ExitStack() as ph:
                    wpa = sb("wpa", [128, 4, D], BF16, ph)
                    wpp = sb("wpp", [128, 4, D], BF16, ph)
                    wo = sb("wo", [128, 8, D], BF16, ph)
                    wgt = [sb("wgt%d" % i, [128, 8, 2, 128], BF16, ph) for i in range(2)]
                    g0 = sb("g0", [128, 512], F32, ph)
                    g1 = sb("g1", [128, 512], F32, ph)
                    t1 = sb("t1", [128, 512], F32, ph)
                    t2 = sb("t2", [128, 512], F32, ph)
                    yT = sb("yT", [128, 8, 512], BF16, ph)
                    S.dma("pool", lambda e: e.dma_start(
                        out=wpa[0:64, :, :], in_=w_pa_d[l].rearrange("(h p) n -> p h n", p=64)), writes=["wpa"])
                    S.dma("pool", lambda e: e.dma_start(
                        out=wpp[:], in_=w_pp_d[l].rearrange("(g p) n -> p g n", p=128)), writes=["wpp"])
                    S.dma("pool", lambda e: e.dma_start(
                        out=wo[:], in_=w_out_d[l].rearrange("(c p) n -> p c n", p=128)), writes=["wo"])
                    wgates = w_in_d[l][:, 2816:4864].rearrange("(c p) (t n) -> p c t n", p=128, t=2)
                    it = 0
                    for tb in range(4):
                        tsl = slice(tb * 512, (tb + 1) * 512)
                        xr = xall[4 * tb:4 * tb + 4]
                        for dc in range(8):
                            wg_ = wgt[it % 2]
                            wgr = "wgt%d" % (it % 2)
                            it += 1
                            for t2_ in range(2):
                                S.dma("pool", lambda e, wg_=wg_, dc=dc, t2_=t2_: e.dma_start(
                                    out=wg_[:, :, t2_, :], in_=wgates[:, :, t2_, dc * 128:(dc + 1) * 128]), writes=[wgr])
                            pc, pcr = next_pp()
                            for c in range(8):
                                S.op("pe", lambda e, pc=pc, c=c, wg_=wg_: e.matmul(
                                    out=pc[:], lhsT=wg_[:, c, 0, :], rhs=xnT[:, c, tsl], start=(c == 0), stop=(c == 7)),
                                    reads=[wgr] + xr, writes=[pcr])
                            pd, pdr = next_pp()
                            for c in range(8):
                                S.op("pe", lambda e, pd=pd, c=c, wg_=wg_: e.matmul(
                                    out=pd[:], lhsT=wg_[:, c, 1, :], rhs=xnT[:, c, tsl], start=(c == 0), stop=(c == 7)),
                                    reads=[wgr] + xr, writes=[pdr])
                            pa, par = next_pp()
                            for hs in range(4):
                                S.op("pe", lambda e, pa=pa, hs=hs, dc=dc: e.matmul(
                                    out=pa[:], lhsT=wpa[0:64, hs, dc * 128:(dc + 1) * 128], rhs=oT[0:64, hs, tsl],
                                    start=(hs == 0), stop=(hs == 3)), reads=["wpa", "oT"], writes=[par])
                            pb, pbr = next_pp()
                            for g in range(4):
                                S.op("pe", lambda e, pb=pb, g=g, dc=dc: e.matmul(
                                    out=pb[:], lhsT=wpp[:, g, dc * 128:(dc + 1) * 128], rhs=mixedT[:, g, tsl],
                                    start=(g == 0), stop=(g == 3)), reads=["wpp", "mixedT"], writes=[pbr])
                            S.op("act", lambda e, pc=pc: e.activation(out=g0[:], in_=pc[:], func=AF.Sigmoid),
                                 reads=[pcr], writes=["g0"])
                            S.op("act", lambda e, pd=pd: e.activation(out=g1[:], in_=pd[:], func=AF.Sigmoid),
                                 reads=[pdr], writes=["g1"])
                            S.op("dve", lambda e, pa=pa: e.tensor_tensor(out=t1[:], in0=g0[:], in1=pa[:], op=ALU.mult),
                                 reads=["g0", par], writes=["t1"])
                            S.op("dve", lambda e, pb=pb: e.tensor_tensor(out=t2[:], in0=g1[:], in1=pb[:], op=ALU.mult),
                                 reads=["g1", pbr], writes=["t2"])
                            S.op("dve", lambda e, dc=dc: e.tensor_tensor(out=yT[:, dc, :], in0=t1[:], in1=t2[:], op=ALU.add),
                                 reads=["t1", "t2"], writes=["yT"])
                        for ti in range(4):
                            tt = tb * 4 + ti
                            for nh in range(2):
                                pp, pr = next_pp()
                                for c in range(8):
                                    S.op("pe", lambda e, pp=pp, c=c, ti=ti, nh=nh: e.matmul(
                                        out=pp[:], lhsT=yT[:, c, ti * 128:(ti + 1) * 128], rhs=wo[:, c, nh * 512:(nh + 1) * 512],
                                        start=(c == 0), stop=(c == 7)), reads=["yT", "wo"], writes=[pr])
                                S.op("dve", lambda e, pp=pp, tt=tt, nh=nh: e.tensor_tensor(
                                    out=h[:, tt, nh * 512:(nh + 1) * 512], in0=h[:, tt, nh * 512:(nh + 1) * 512], in1=pp[:],
                                    op=ALU.add), reads=[pr, "h%d" % tt], writes=["h%d" % tt])
                    S.barrier()

        def moe(l):
            rms_stats(2 * l + 1)
            with ExitStack() as ph:
                desti = sb("desti", [128, NT, 2], I32, ph)
                cwt = sb("cwt", [128, NT, 2], F32, ph)
                NSLOT, NPRE = 8, 4
                wslot = [sb("ws%d" % i, [128, 4096], BF16, ph) for i in range(NPRE)]

                def wview(m):
                    t = wslot[m % NSLOT]
                    if m % 3 == 2:
                        return t[:].rearrange("p (c n) -> p c n", c=4)
                    return t[:].rearrange("p (c n) -> p c n", c=8)

                def issue_w(m):
                    if m >= 64 * 3:
                        return
                    ex_, j_ = m // 3, m % 3
                    if j_ == 2:
                        src = wd_d[l, ex_].rearrange("(c p) n -> p c n", p=128)
                        dst = wview(m)
                    else:
                        src = (wg_d, wu_d)[j_][l, ex_].rearrange("(c p r) n -> p c (r n)", p=128, r=2)
                        dst = wslot[m % NSLOT][:].rearrange("p (c n) -> p c n", c=4)
                    S.dma("pool", lambda e, src=src, dst=dst: e.dma_start(out=dst, in_=src), writes=["ws%d" % (m % NSLOT)])

                for m_ in range(NPRE):
                    issue_w(m_)
                rp = ExitStack()
                T = NT
                destf = sb("destf", [128, T, 2], F32, rp)
                wr = sb("wr", [128, 8, 72], F32, rp)
                xf2 = [sb("xf%d" % i, [128, D], F32, rp) for i in range(2)]
                xfT2 = [sb("xfT%d" % i, [128, 8, 128], F32, rp) for i in range(2)]
                lgA = sb("lgA", [128, T, 72], F32, rp)
                ohgA = sb("ohgA", [128, T, 8], F32, rp)
                t8A = sb("t8A", [128, T, 8], F32, rp)
                emA = sb("emA", [128, T, 64], F32, rp)
                em2A = sb("em2A", [128, T, 64], F32, rp)
                oh1A = sb("oh1A", [128, T, 64], F32, rp)
                oh2A = sb("oh2A", [128, T, 64], F32, rp)
                ohEA = sb("ohEA", [128, T, 64], F32, rp)
                t64A = sb("t64A", [128, T, 64], F32, rp)
                csA = sb("csA", [128, T, 64], F32, rp)
                rankA = sb("rankA", [128, T, 64], F32, rp)
                smA = sb("smA", [128, 16, T], F32, rp)
                iota64 = cf[:, C_IOTA:C_IOTA + 64]
                xtk = lambda tt, c0=0, n=1024: xnT[:, tt // 2, (tt % 2) * 1024 + c0:(tt % 2) * 1024 + c0 + n]
                S.dma("sp", lambda e: e.dma_start(out=wr[:], in_=wr_d[l].rearrange("(c p) n -> p c n", p=128)), writes=["wr"])
                S.op("pool", lambda e: e.memset(csA[:, 0, :], 0.0), writes=["csA"])
                def r_stage_a(tt):
                    xf = xf2[tt % 2]
                    xfr = "xf%d" % (tt % 2)
                    norm_tile(tt, xf[:], xfr)
                    S.op("pool", lambda e, tt=tt, xf=xf: e.tensor_copy(out=xtk(tt), in_=xf[:]), reads=[xfr], writes=["xtok%d" % tt])
                    xfT = xfT2[tt % 2]
                    for half in range(2):
                        bk = 2 + 2 * (tt % 2) + half
                        bname = "B%d" % bk
                        for c4 in range(4):
                            c = half * 4 + c4
                            S.op("pe", lambda e, c=c, c4=c4, bk=bk, xf=xf: e.transpose(
                                out=B[bk][:, c4 * 128:(c4 + 1) * 128], in_=xf[:, c * 128:(c + 1) * 128], identity=ident),
                                reads=[xfr, "cf"], writes=[bname])
                        xres = "xfT%d_%d" % (tt % 2, half)
                        if half == 0:
                            S.op("act", lambda e, bk=bk, xfT=xfT: e.copy(
                                out=xfT[:, 0:4, :], in_=B[bk][:].rearrange("p (a b) -> p a b", a=4)), reads=[bname], writes=[xres])
                        else:
                            S.op("dve", lambda e, bk=bk, xfT=xfT: e.tensor_copy(
                                out=xfT[:, 4:8, :], in_=B[bk][:].rearrange("p (a b) -> p a b", a=4)), reads=[bname], writes=[xres])

                def r_stage_b(tt):
                    xfT = xfT2[tt % 2]
                    pp, pr = next_pp()
                    for c in range(8):
                        S.op("pe", lambda e, c=c, pp=pp, xfT=xfT: e.matmul(out=pp[:, 0:72], lhsT=xfT[:, c, :], rhs=wr[:, c, :],
                                                                           start=(c == 0), stop=(c == 7)),
                             reads=["xfT%d_0" % (tt % 2), "xfT%d_1" % (tt % 2), "wr"], writes=[pr])
                    S.op("dve", lambda e, tt=tt, pp=pp: e.tensor_copy(out=lgA[:, tt, :], in_=pp[:, 0:72]), reads=[pr], writes=["lgA"])

                pp_banks[0] = [0, 1]
                for tt in range(NT):
                    r_stage_a(tt)
                    if tt > 0:
                        r_stage_b(tt - 1)
                r_stage_b(NT - 1)
                pp_banks[0] = [0, 1, 2, 3, 4, 5]
                sA = lambda j: smA[:, j, :]
                bc = lambda ap2, n: ap2.unsqueeze(2).to_broadcast([128, T, n])
                V = lambda fn, reads, writes: S.op("dve", fn, reads=reads, writes=writes)
                V(lambda e: e.tensor_reduce(out=sA(0), in_=lgA[:, :, 0:8], axis=AX.X, op=ALU.max), ["lgA"], ["smA"])
                V(lambda e: e.tensor_tensor(out=ohgA[:], in0=lgA[:, :, 0:8], in1=bc(sA(0), 8), op=ALU.is_equal),
                  ["lgA", "smA"], ["ohgA"])
                V(lambda e: e.tensor_tensor(out=t8A[:], in0=lgA[:, :, 0:8], in1=bc(sA(0), 8), op=ALU.subtract),
                  ["lgA", "smA"], ["t8A"])
                S.op("act", lambda e: e.activation(out=t8A[:], in_=t8A[:], func=AF.Exp), reads=["t8A"], writes=["t8A"])
                V(lambda e: e.tensor_reduce(out=sA(2), in_=t8A[:], axis=AX.X, op=ALU.add), ["t8A"], ["smA"])
                V(lambda e: e.reciprocal(out=sA(3), in_=sA(2)), ["smA"], ["smA"])
                V(lambda e: e.tensor_scalar(out=t8A[:], in0=ohgA[:], scalar1=-1.0, scalar2=1e9, op0=ALU.add, op1=ALU.mult),
                  ["ohgA", "t8A"], ["t8A"])
                V(lambda e: e.tensor_tensor(out=emA[:].rearrange("p t (g e) -> p t g e", g=8),
                                            in0=lgA[:, :, 8:72].rearrange("p t (g e) -> p t g e", g=8),
                                            in1=t8A[:].unsqueeze(3).to_broadcast([128, T, 8, 8]), op=ALU.add),
                  ["lgA", "t8A"], ["emA"])
                V(lambda e: e.tensor_reduce(out=sA(4), in_=emA[:], axis=AX.X, op=ALU.max), ["emA"], ["smA"])
                V(lambda e: e.tensor_tensor(out=oh1A[:], in0=emA[:], in1=bc(sA(4), 64), op=ALU.is_equal), ["emA", "smA"], ["oh1A"])
                V(lambda e: e.scalar_tensor_tensor(out=em2A[:], in0=oh1A[:], scalar=-1e9, in1=emA[:], op0=ALU.mult, op1=ALU.add),
                  ["oh1A", "emA"], ["em2A"])
                V(lambda e: e.tensor_reduce(out=sA(5), in_=em2A[:], axis=AX.X, op=ALU.max), ["em2A"], ["smA"])
                V(lambda e: e.tensor_tensor(out=oh2A[:], in0=em2A[:], in1=bc(sA(5), 64), op=ALU.is_equal), ["em2A", "smA"], ["oh2A"])
                V(lambda e: e.tensor_tensor(out=sA(6), in0=sA(5), in1=sA(4), op=ALU.subtract), ["smA"], ["smA"])
                S.op("act", lambda e: e.activation(out=sA(7), in_=sA(6), func=AF.Exp), reads=["smA"], writes=["smA"])
                V(lambda e: e.tensor_scalar(out=sA(8), in0=sA(7), scalar1=1.0, scalar2=None, op0=ALU.add), ["smA"], ["smA"])
                V(lambda e: e.reciprocal(out=sA(9), in_=sA(8)), ["smA"], ["smA"])
                V(lambda e: e.tensor_tensor(out=sA(10), in0=sA(9), in1=sA(3), op=ALU.mult), ["smA"], ["smA"])
                V(lambda e: e.tensor_tensor(out=sA(11), in0=sA(10), in1=sA(7), op=ALU.mult), ["smA"], ["smA"])
                V(lambda e: e.tensor_tensor(out=ohEA[:], in0=oh1A[:], in1=oh2A[:], op=ALU.add), ["oh1A", "oh2A"], ["ohEA"])
                for tt in range(1, NT):
                    V(lambda e, tt=tt: e.tensor_tensor(out=csA[:, tt, :], in0=csA[:, tt - 1, :], in1=ohEA[:, tt - 1, :], op=ALU.add),
                      ["csA", "ohEA"], ["csA"])
                for half in range(2):
                    pk, pkr = next_pp()
                    for t8 in range(8):
                        tt = half * 8 + t8
                        S.op("pe", lambda e, tt=tt, t8=t8, pk=pk: e.matmul(out=pk[:, t8 * 64:(t8 + 1) * 64], lhsT=tri,
                                                                            rhs=ohEA[:, tt, :], start=True, stop=False),
                             reads=["cf", "ohEA"], writes=[pkr])
                        S.op("pe", lambda e, tt=tt, t8=t8, pk=pk: e.matmul(out=pk[:, t8 * 64:(t8 + 1) * 64], lhsT=ones,
                                                                            rhs=csA[:, tt, :], start=False, stop=True),
                             reads=["cf", "csA"], writes=[pkr])
                    S.op("act", lambda e, half=half, pk=pk: e.copy(out=rankA[:, half * 8:(half + 1) * 8, :],
                                                                   in_=pk[:].rearrange("p (a c) -> p a c", c=64)),
                         reads=[pkr], writes=["rankA"])
                for k, ohk, ohr in ((0, oh1A, "oh1A"), (1, oh2A, "oh2A")):
                    V(lambda e, ohk=ohk: e.tensor_tensor(out=t64A[:], in0=ohk[:], in1=rankA[:], op=ALU.mult),
                      ["rankA", ohr, "t64A"], ["t64A"])
                    V(lambda e, k=k: e.tensor_reduce(out=sA(12 + k), in_=t64A[:], axis=AX.X, op=ALU.add), ["t64A"], ["smA"])
                    V(lambda e, ohk=ohk: e.tensor_tensor(out=t64A[:], in0=ohk[:], in1=iota64.unsqueeze(1).to_broadcast([128, T, 64]),
                                                         op=ALU.mult), ["cf", ohr, "t64A"], ["t64A"])
                    V(lambda e, k=k: e.tensor_reduce(out=sA(14 + k), in_=t64A[:], axis=AX.X, op=ALU.add), ["t64A"], ["smA"])
                    V(lambda e, k=k: e.scalar_tensor_tensor(out=destf[:, :, k], in0=sA(14 + k), scalar=float(CAPE), in1=sA(12 + k),
                                                            op0=ALU.mult, op1=ALU.add), ["smA"], ["destf"])
                    V(lambda e, k=k: e.tensor_scalar(out=sA(1), in0=sA(12 + k), scalar1=float(CAPE), scalar2=1e6,
                                                     op0=ALU.is_ge, op1=ALU.mult), ["smA"], ["smA"])
                    V(lambda e, k=k: e.tensor_tensor(out=destf[:, :, k], in0=destf[:, :, k], in1=sA(1), op=ALU.add),
                      ["smA", "destf"], ["destf"])
                    V(lambda e, k=k: e.tensor_scalar(out=sA(1), in0=sA(12 + k), scalar1=float(CAPE), scalar2=None, op0=ALU.is_lt),
                      ["smA"], ["smA"])
                    V(lambda e, k=k: e.tensor_tensor(out=cwt[:, :, k], in0=sA(10 + k), in1=sA(1), op=ALU.mult),
                      ["smA"], ["cwt"])
                S.op("dve", lambda e: e.tensor_copy(out=desti[:], in_=destf[:]), reads=["destf"], writes=["desti"])
                S.barrier()
                rp.close()
                xgs = []
                for tt in range(NT):
                    for k in range(2):
                        nm = "xgs%d" % (tt * 2 + k)
                        xgs.append(nm)
                        S.dma("pool", lambda e, tt=tt, k=k: e.indirect_dma_start(
                            out=xg_d, out_offset=bass.IndirectOffsetOnAxis(ap=desti[:, tt, k:k + 1], axis=0),
                            in_=xtk(tt), in_offset=None, bounds_check=bc_reg, oob_is_err=False),
                            reads=["xtok%d" % tt, "desti", "xg_init"], writes=[nm])
                xe = [sb("xe%d" % i, [128, D], BF16, ph) for i in range(2)]
                xeT = [sb("xeT%d" % i, [128, 8, 128], BF16, ph) for i in range(2)]
                wslot += [sb("ws%d" % i, [128, 4096], BF16, ph) for i in range(NPRE, NSLOT)]
                for m_ in range(NPRE, NSLOT):
                    issue_w(m_)
                sil = [sb("sil%d" % i, [128, 512], F32, ph) for i in range(2)]
                hw = [sb("hw%d" % i, [128, 512], BF16, ph) for i in range(2)]
                ye = [sb("ye%d" % i, [128, D], BF16, ph) for i in range(2)]
                gy = [sb("gy%d" % i, [128, D], BF16, ph) for i in range(4)]
                xnb2 = [sb("xnbm%d" % i, [128, D], BF16, ph) for i in range(2)] if l == 0 else None
                for i in range(4):
                    S.op("pool", lambda e, i=i: e.memset(gy[i][:], 0.0), writes=["gy%d" % i])
                ygs = []
                def stage_a(ex):
                    k = ex % 2
                    wg_v, wu_v = wview(3 * ex), wview(3 * ex + 1)
                    wg_r, wu_r = ["ws%d" % ((3 * ex + j_) % NSLOT) for j_ in range(2)]
                    S.dma("sp", lambda e, k=k, ex=ex: e.dma_start(out=xe[k][:], in_=xg_d[ex * CAPE:(ex + 1) * CAPE, :]),
                          reads=xgs, writes=["xe%d" % k])
                    for c in range(8):
                        S.op("pe", lambda e, c=c, k=k: e.transpose(out=Tb[k][:, c, :], in_=xe[k][:, (c // 2) * 256 + (c % 2):(c // 2) * 256 + 256:2],
                                                                   identity=identb[:]),
                             reads=["xe%d" % k, "identb"], writes=["T%d" % k])
                    S.op("act", lambda e, k=k: e.copy(out=xeT[k][:], in_=Tb[k][:]), reads=["T%d" % k], writes=["xeT%d" % k])
                    pg, pgr = next_pq()
                    for fc in range(4):
                        for c in range(8):
                            S.op("pe", lambda e, pg=pg, c=c, fc=fc, k=k: e.matmul(
                                out=pg[:, fc * 128:(fc + 1) * 128], lhsT=wg_v[:, c, fc * 128:(fc + 1) * 128], rhs=xeT[k][:, c, :],
                                start=(c == 0), stop=(c == 7)), reads=[wg_r, "xeT%d" % k], writes=[pgr])
                    issue_w(3 * ex + NSLOT)
                    pu, pur = next_pq()
                    for fc in range(4):
                        for c in range(8):
                            S.op("pe", lambda e, pu=pu, c=c, fc=fc, k=k: e.matmul(
                                out=pu[:, fc * 128:(fc + 1) * 128], lhsT=wu_v[:, c, fc * 128:(fc + 1) * 128], rhs=xeT[k][:, c, :],
                                start=(c == 0), stop=(c == 7)), reads=[wu_r, "xeT%d" % k], writes=[pur])
                    issue_w(3 * ex + 1 + NSLOT)
                    S.op("act", lambda e, pg=pg, k=k: e.activation(out=sil[k][:], in_=pg[:], func=AF.Silu),
                         reads=[pgr], writes=["sil%d" % k])
                    S.op("dve", lambda e, pu=pu, k=k: e.tensor_tensor(out=hw[k][:], in0=sil[k][:], in1=pu[:], op=ALU.mult),
                         reads=["sil%d" % k, pur], writes=["hw%d" % k])

                def stage_b(ex):
                    k = ex % 2
                    wd_v = wview(3 * ex + 2)
                    wd_r = "ws%d" % ((3 * ex + 2) % NSLOT)
                    for nh in range(2):
                        pp, pr = next_pq()
                        for fc in range(4):
                            S.op("pe", lambda e, pp=pp, fc=fc, nh=nh, k=k: e.matmul(
                                out=pp[:], lhsT=hw[k][:, fc * 128:(fc + 1) * 128], rhs=wd_v[:, fc, nh * 512:(nh + 1) * 512],
                                start=(fc == 0), stop=(fc == 3)), reads=["hw%d" % k, wd_r], writes=[pr])
                        if nh == 0:
                            S.op("act", lambda e, pp=pp, k=k: e.copy(out=ye[k][:, 0:512], in_=pp[:]), reads=[pr], writes=["ye%d" % k])
                        else:
                            S.op("dve", lambda e, pp=pp, k=k: e.tensor_copy(out=ye[k][:, 512:1024], in_=pp[:]),
                                 reads=[pr], writes=["ye%d" % k])
                    issue_w(3 * ex + 2 + NSLOT)
                    nm = "ygs%d" % ex
                    ygs.append(nm)
                    S.dma("sp", lambda e, k=k, ex=ex: e.dma_start(out=yg_d[ex * CAPE:(ex + 1) * CAPE, :], in_=ye[k][:]),
                          reads=["ye%d" % k], writes=[nm])

                for ex in range(64):
                    stage_a(ex)
                    if ex > 0:
                        stage_b(ex - 1)
                stage_b(63)
                S.dma("sp", lambda e: e.dma_start(out=gv[:], in_=gvec_d[:, 2 if l == 0 else 4, :]), writes=["gv"])
                gi_box = [0]

                def c_stage1(tt):
                    for k in range(2):
                        j = gi_box[0] % 4
                        gi_box[0] += 1
                        S.dma("pool", lambda e, tt=tt, k=k, j=j: e.indirect_dma_start(
                            out=gy[j][:, :], out_offset=None, in_=yg_d,
                            in_offset=bass.IndirectOffsetOnAxis(ap=desti[:, tt, k:k + 1], axis=0),
                            bounds_check=bc_reg, oob_is_err=False),
                            reads=ygs + ["desti"], writes=["gy%d" % j])
                        S.op("dve", lambda e, tt=tt, k=k, j=j: e.scalar_tensor_tensor(
                            out=h[:, tt, :], in0=gy[j][:], scalar=cwt[:, tt, k:k + 1], in1=h[:, tt, :],
                            op0=ALU.mult, op1=ALU.add), reads=["gy%d" % j, "cwt", "h%d" % tt], writes=["h%d" % tt])
                    rms_a(tt)

                def c_stage2(tt):
                    rr = rms_b(tt)
                    if l == 0:
                        xb, xr = xnb2[tt % 2], "xnbm%d" % (tt % 2)
                        norm_tile(tt, xb[:], xr, rres=rr)
                        Tt, Tr = Tb[tt % 2], "T%d" % (tt % 2)
                        for c in range(8):
                            S.op("pe", lambda e, c=c, xb=xb, Tt=Tt: e.transpose(out=Tt[:, c, :], in_=xb[:, c * 128:(c + 1) * 128],
                                                                                identity=identb[:]),
                                 reads=[xr, "identb"], writes=[Tr])
                        if tt % 2 == 0:
                            S.op("act", lambda e, tt=tt, Tt=Tt: e.copy(out=xnT[:, :, tt * 128:(tt + 1) * 128], in_=Tt[:]),
                                 reads=[Tr], writes=["xnT%d" % tt])
                        else:
                            S.op("dve", lambda e, tt=tt, Tt=Tt: e.tensor_copy(out=xnT[:, :, tt * 128:(tt + 1) * 128], in_=Tt[:]),
                                 reads=[Tr], writes=["xnT%d" % tt])
                    else:
                        for hf in range(2):
                            csl = slice(hf * 512, (hf + 1) * 512)
                            S.op("dve", lambda e, tt=tt, hf=hf, csl=csl: e.scalar_tensor_tensor(
                                out=sil[hf][:], in0=h[:, tt, csl], scalar=rstd[:, tt:tt + 1], in1=gv[:, csl],
                                op0=ALU.mult, op1=ALU.mult), reads=["h%d" % tt, rr, "gv"], writes=["sil%d" % hf])
                            S.dma("sp", lambda e, tt=tt, hf=hf, csl=csl: e.dma_start(out=out_d[tt * 128:(tt + 1) * 128, csl], in_=sil[hf][:]),
                                  reads=["sil%d" % hf])

                for tt in range(NT):
                    c_stage1(tt)
                    if tt > 0:
                        c_stage2(tt - 1)
                c_stage2(NT - 1)
                S.barrier()

        def dump_h(dst):
            for tt in range(NT):
                S.dma("sp", lambda e, tt=tt: e.dma_start(out=dst[tt * 128:(tt + 1) * 128, :], in_=h[:, tt, :]),
                      reads=["h%d" % tt])

        stages = [(0, "mix"), (0, "moe"), (1, "mix"), (1, "moe")]
        for si, (l, kind) in enumerate(stages):
            if kind == "mix":
                mixer(l)
            else:
                moe(l)
            if dbg and dbg == si + 1:
                dump_h(dbg_d)
        S.finish()
    return nc


def _t5_bucket(rel):
    nb = 16
    ret = np.where(rel > 0, nb, 0)
    n = np.abs(rel)
    max_exact = 8
    nf = np.maximum(n, max_exact).astype(np.float32)
    large = max_exact + (np.log(nf / np.float32(max_exact)) / np.float32(math.log(1024 / max_exact))
                         * np.float32(nb - max_exact)).astype(np.int32)
    large = np.minimum(large, nb - 1)
    return ret + np.where(n < max_exact, n, large)


def _host_consts(rel_bias):
    kk = np.arange(128)[:, None]
    qq = np.arange(128)[None, :]
    biasT = np.zeros((128, 28, 128), np.float32)
    for hd in range(12):
        dil = GROUPS[hd // 4][0]
        for ty in range(3):
            if hd >= 8 and ty != 1:
                continue
            rel = kk - qq + (ty - 1) * 128
            b = rel_bias[_t5_bucket(rel * dil), hd]
            biasT[:, bias_index(hd, ty), :] = np.where(np.abs(rel) <= 64, b, np.float32(NEGM))
    cf = np.zeros((128, C_N), np.float32)
    cf[:, C_ID:C_ID + 128] = np.eye(128, dtype=np.float32)
    cf[:, C_TRI:C_TRI + 128] = (kk < qq).astype(np.float32)
    cf[:, C_ONE:C_ONE + 128] = 1.0
    cf[:, C_IOTA:C_IOTA + CAPG] = np.arange(CAPG, dtype=np.float32)[None, :]
    for g, w in enumerate(POOLW):
        e = np.ones(16, np.float32)
        for i in range(8):
            cnt = min(i + w - w // 2, S_) - max(i - w // 2, 0)
            e[i] = w / cnt
            ii = S_ - 8 + i
            cnt = min(ii + w - w // 2, S_) - max(ii - w // 2, 0)
            e[8 + i] = w / cnt
        cf[:, C_EDGE + g * 16:C_EDGE + g * 16 + 16] = e[None, :]
    cf[64, C_SEL:C_SEL + 64] = 1.0
    return biasT, cf


_NC_CACHE = {}


def kernel(x, rel_bias, norm_mix_g, w_in, pool_w, pool_scale, w_proj_attn, w_proj_pool, w_out, norm_ffn_g,
           w_router_group, w_router_expert, w_gate_e, w_up_e, w_down_e, norm_final_g, _dbg=0, _cores=8):
    f = lambda a: np.ascontiguousarray(np.asarray(a, dtype=np.float32))
    x = f(x)
    rel_bias = f(rel_bias)
    biasT, cf = _host_consts(rel_bias)
    gvec = np.stack([f(norm_mix_g)[0], f(norm_ffn_g)[0], f(norm_mix_g)[1], f(norm_ffn_g)[1], f(norm_final_g)], 0)
    gvec = np.ascontiguousarray(np.broadcast_to(gvec[None], (128, 5, D)))
    pscale = np.ascontiguousarray(f(pool_scale).reshape(2, 4, 128).transpose(2, 0, 1).reshape(128, 8))
    wre = f(w_router_expert).transpose(0, 2, 1, 3).reshape(2, D, 64)
    wr = np.ascontiguousarray(np.concatenate([f(w_router_group), wre], axis=2))
    shared = {
        "w_in": f(w_in), "pool_w": f(pool_w), "w_proj_attn": f(w_proj_attn), "w_proj_pool": f(w_proj_pool),
        "w_out": f(w_out), "wr": wr, "w_gate_e": f(w_gate_e), "w_up_e": f(w_up_e), "w_down_e": f(w_down_e),
        "gvec": gvec, "pscale": pscale, "biasT": biasT, "cf32": cf,
    }
    key = _dbg
    if key not in _NC_CACHE:
        _NC_CACHE[key] = build(_dbg)
    nc = _NC_CACHE[key]
    in_maps = []
    for b in range(_cores):
        m = dict(shared)
        m["x"] = np.ascontiguousarray(x[b])
        in_maps.append(m)
    res = run_bass_kernel_spmd(nc, in_maps, core_ids=list(range(_cores)))
    out = np.stack([res.results[b]["out"] for b in range(_cores)], 0).astype(np.float32)
    if _dbg:
        return out, np.stack([res.results[b]["dbg"] for b in range(_cores)], 0)
    return out
```

```python
import math
from contextlib import ExitStack

import numpy as np
import concourse.bass as bass
import concourse.mybir as mybir
from concourse.bass_utils import run_bass_kernel_spmd

F32 = mybir.dt.float32
BF16 = mybir.dt.bfloat16
I32 = mybir.dt.int32
ALU = mybir.AluOpType
AF = mybir.ActivationFunctionType
AX = mybir.AxisListType

S_ = 2048
D = 1024
NT = 16
INW = 4864
CAPG = 384
CAPE = 128
NEGM = -30000.0
GROUPS = ((1, 0), (4, 1), (16, 2))
POOLW = (2, 4, 8, 16)
C_ID, C_TRI, C_ONE, C_IOTA, C_EDGE, C_SEL = 0, 128, 256, 384, 768, 832
C_N = 896


class Sched:
    ENG = ["pe", "act", "dve", "pool", "sp"]
    RING = 12

    def __init__(self, nc, es):
        self.nc = nc
        self.obj = {"pe": nc.tensor, "act": nc.scalar, "dve": nc.vector, "pool": nc.gpsimd, "sp": nc.sync}
        self.sem = {e: es.enter_context(nc.semaphore("s_" + e)) for e in self.ENG}
        self.cnt = {e: 0 for e in self.ENG}
        self.seen = {x: {} for x in self.ENG}
        self.lastw = {}
        self.reads = {}
        self.ring = {}
        self.ring_i = {}
        for q in ("sp", "pool"):
            self.ring[q] = []
            self.ring_i[q] = 0
            for i in range(self.RING):
                name = "q_%s_%d" % (q, i)
                self.sem[name] = es.enter_context(nc.semaphore(name))
                self.cnt[name] = 0
                self.ring[q].append(name)

    def _dep(self, x, e, ts, raw=False):
        if e == x and x in ("pe", "sp"):
            return
        if self.seen[x].get(e, 0) < ts:
            self.seen[x][e] = ts
            self.obj[x].wait_ge(self.sem[e], ts)

    def _deps(self, x, reads, writes):
        for r in reads:
            w = self.lastw.get(r)
            if w is not None:
                self._dep(x, w[0], w[1], raw=True)
        for r in writes:
            w = self.lastw.get(r)
            if w is not None:
                self._dep(x, *w)
            rd = self.reads.get(r)
            if rd:
                for e, ts in rd.items():
                    self._dep(x, e, ts)

    def _mark(self, who, ts, reads, writes):
        for r in reads:
            self.reads.setdefault(r, {})[who] = ts
        for r in writes:
            self.lastw[r] = (who, ts)
            self.reads[r] = {}

    def op(self, x, fn, reads=(), writes=()):
        self._deps(x, reads, writes)
        self.cnt[x] += 1
        ts = self.cnt[x]
        fn(self.obj[x]).then_inc(self.sem[x], 1)
        self._mark(x, ts, reads, writes)

    def dma(self, q, fn, reads=(), writes=()):
        self._deps(q, reads, writes)
        i = self.ring_i[q]
        self.ring_i[q] = (i + 1) % self.RING
        name = self.ring[q][i]
        if self.cnt[name] > 0:
            self._dep(q, name, self.cnt[name])
        self.cnt[name] += 16
        ts = self.cnt[name]
        fn(self.obj[q]).then_inc(self.sem[name], 16)
        self._mark(name, ts, reads, writes)

    def barrier(self):
        for x in self.ENG:
            for e in self.cnt:
                if self.cnt[e] > 0:
                    self._dep(x, e, self.cnt[e])

    def finish(self):
        for name in self.cnt:
            if name.startswith("q_") and self.cnt[name] > 0:
                self._dep("sp", name, self.cnt[name])
        for e in self.ENG:
            if self.cnt[e] > 0:
                self._dep("sp", e, self.cnt[e])


def bias_index(h, ty):
    return h * 3 + ty if h < 8 else 24 + (h - 8)


def tok_slice(dil, sub, blk):
    st = sub + dil * 128 * blk
    return slice(st, st + dil * 127 + 1, dil)


def build(dbg=0):
    nc = bass.Bass("TRN2", target_bir_lowering=False)
    dt = lambda name, shape, d=F32, kind="ExternalInput": nc.dram_tensor(name, shape, d, kind=kind).ap()
    x_d = dt("x", [S_, D])
    w_in_d = dt("w_in", [2, D, INW])
    pool_w_d = dt("pool_w", [2, 4, 128, 128])
    w_pa_d = dt("w_proj_attn", [2, 256, D])
    w_pp_d = dt("w_proj_pool", [2, 512, D])
    w_out_d = dt("w_out", [2, D, D])
    wr_d = dt("wr", [2, D, 72])
    wg_d = dt("w_gate_e", [2, 64, D, 512])
    wu_d = dt("w_up_e", [2, 64, D, 512])
    wd_d = dt("w_down_e", [2, 64, 512, D])
    gvec_d = dt("gvec", [128, 5, D])
    pscale_d = dt("pscale", [128, 8])
    bias_d = dt("biasT", [128, 28, 128])
    cf_d = dt("cf32", [128, C_N])
    xg_d = dt("xg_scr", [64 * CAPE, D], BF16, kind="Internal")
    yg_d = dt("yg_scr", [64 * CAPE, D], BF16, kind="Internal")
    out_d = dt("out", [S_, D], kind="ExternalOutput")
    dbg_d = dt("dbg", [S_, D], kind="ExternalOutput") if dbg else None

    with ExitStack() as es:
        uid = [0]

        def sb(name, shape, d=F32, st=es):
            uid[0] += 1
            return st.enter_context(nc.sbuf_tensor("sb%d_%s" % (uid[0], name), shape, d))

        psum = lambda name, shape, d=F32: es.enter_context(nc.psum_tensor("ps_" + name, shape, d))
        S = Sched(nc, es)
        h = sb("h", [128, NT, D])
        xnT = sb("xnT", [128, 8, S_], BF16)
        biasT = sb("biasT", [128, 28, 128], BF16)
        cf = sb("cf", [128, C_N])
        gv = sb("gv", [128, D])
        pscale = sb("pscale", [128, 8])
        identb = sb("identb", [128, 128], BF16)
        ssq = sb("ssq", [128, NT])
        rstd = sb("rstd", [128, NT])
        junk = sb("junk", [128, D], BF16)
        B = [psum("B%d" % i, [128, 512]) for i in range(6)]
        tgen = [0]
        tstack = [ExitStack()]
        Tb = [None, None]

        def alloc_T():
            tgen[0] += 1
            tstack[0] = ExitStack()
            for i in range(2):
                Tb[i] = tstack[0].enter_context(nc.psum_tensor("ps_T%d_%d" % (i, tgen[0]), [128, 8, 128], BF16))

        alloc_T()
        es.callback(lambda: tstack[0].close())
        ident = cf[:, C_ID:C_ID + 128]
        tri = cf[:, C_TRI:C_TRI + 128]
        ones = cf[:, C_ONE:C_ONE + 128]
        iota = cf[:, C_IOTA:C_IOTA + CAPG]
        sel65 = cf[:, C_SEL:C_SEL + 64]

        S.dma("sp", lambda e: e.dma_start(out=cf[:], in_=cf_d), writes=["cf"])
        S.dma("pool", lambda e: e.dma_start(out=biasT[:], in_=bias_d), writes=["biasT"])
        S.dma("sp", lambda e: e.dma_start(out=pscale[:], in_=pscale_d), writes=["pscale"])
        for tt in range(NT):
            S.dma("sp", lambda e, tt=tt: e.dma_start(out=h[:, tt, :], in_=x_d[tt * 128:(tt + 1) * 128, :]),
                  writes=["h%d" % tt])
        S.op("dve", lambda e: e.tensor_copy(out=identb[:], in_=ident), reads=["cf"], writes=["identb"])

        pp_i = [0]
        pp_banks = [[0, 1, 2, 3, 4, 5]]

        def next_pp():
            lst = pp_banks[0]
            pp_i[0] = (pp_i[0] + 1) % len(lst)
            i = lst[pp_i[0]]
            return B[i], "B%d" % i

        bc_reg = nc.gpsimd.alloc_register("bc_reg")
        nc.gpsimd.reg_mov(bc_reg, 64 * CAPE - 1)
        pq_i = [0]
        pq_list = [(B[i], "B%d" % i) for i in range(6)]

        def next_pq():
            i = pq_i[0]
            pq_i[0] = (i + 1) % 6
            return pq_list[i]

        def rms_stats(gidx):
            S.dma("sp", lambda e: e.dma_start(out=gv[:], in_=gvec_d[:, gidx, :]), writes=["gv"])
            for tt in range(NT):
                S.op("act", lambda e, tt=tt: e.activation(out=junk[:], in_=h[:, tt, :], func=AF.Square,
                                                          accum_out=ssq[:, tt:tt + 1]),
                     reads=["h%d" % tt], writes=["junk", "ssq"])
            S.op("dve", lambda e: e.tensor_scalar(out=rstd[:], in0=ssq[:], scalar1=1.0 / D, scalar2=1e-6,
                                                  op0=ALU.mult, op1=ALU.add), reads=["ssq"], writes=["rstd"])
            S.op("act", lambda e: e.activation(out=rstd[:], in_=rstd[:], func=AF.Sqrt), reads=["rstd"], writes=["rstd"])
            S.op("dve", lambda e: e.reciprocal(out=rstd[:], in_=rstd[:]), reads=["rstd"], writes=["rstd"])

        def norm_tile(tt, out_ap, out_res, rres="rstd"):
            S.op("dve", lambda e: e.scalar_tensor_tensor(out=out_ap, in0=h[:, tt, :], scalar=rstd[:, tt:tt + 1],
                                                         in1=gv[:], op0=ALU.mult, op1=ALU.mult),
                 reads=["h%d" % tt, rres, "gv"], writes=[out_res])

        def rms_tile(tt):
            rms_a(tt)
            return rms_b(tt)

        def rms_a(tt):
            rr = "rstd_t%d" % tt
            S.op("act", lambda e: e.activation(out=junk[:], in_=h[:, tt, :], func=AF.Square, accum_out=ssq[:, tt:tt + 1]),
                 reads=["h%d" % tt], writes=["junk", "ssq_t%d" % tt])
            S.op("dve", lambda e: e.tensor_scalar(out=rstd[:, tt:tt + 1], in0=ssq[:, tt:tt + 1], scalar1=1.0 / D, scalar2=1e-6,
                                                  op0=ALU.mult, op1=ALU.add), reads=["ssq_t%d" % tt], writes=[rr])

        def rms_b(tt):
            rr = "rstd_t%d" % tt
            S.op("act", lambda e: e.activation(out=rstd[:, tt:tt + 1], in_=rstd[:, tt:tt + 1], func=AF.Ln), reads=[rr], writes=[rr])
            S.op("act", lambda e: e.activation(out=rstd[:, tt:tt + 1], in_=rstd[:, tt:tt + 1], func=AF.Exp, scale=-0.5),
                 reads=[rr], writes=[rr])
            return rr

        def mixer(l):
            m0 = ExitStack()
            if l == 0:
                rms_stats(2 * l)
                xnb = [sb("xnb%d" % i, [128, D], BF16, m0) for i in range(2)]
            for tt in (range(NT) if l == 0 else ()):
                xb = xnb[tt % 2]
                xr = "xnb%d" % (tt % 2)
                norm_tile(tt, xb[:], xr)
                Tt, Tr = Tb[tt % 2], "T%d" % (tt % 2)
                for c in range(8):
                    S.op("pe", lambda e, c=c, xb=xb, Tt=Tt: e.transpose(out=Tt[:, c, :], in_=xb[:, c * 128:(c + 1) * 128],
                                                                        identity=identb[:]),
                         reads=[xr, "identb"], writes=[Tr])
                if tt % 2 == 0:
                    S.op("act", lambda e, tt=tt, Tt=Tt: e.copy(out=xnT[:, :, tt * 128:(tt + 1) * 128], in_=Tt[:]),
                         reads=[Tr], writes=["xnT%d" % tt])
                else:
                    S.op("dve", lambda e, tt=tt, Tt=Tt: e.tensor_copy(out=xnT[:, :, tt * 128:(tt + 1) * 128], in_=Tt[:]),
                         reads=[Tr], writes=["xnT%d" % tt])
            xall = ["xnT%d" % t for t in range(NT)]
            S.barrier()
            m0.close()
            if l == 0:
                S.op("pool", lambda e: e.memset(junk[:], 0.0), writes=["junk"])
                for a_ in range(64 * CAPE // 128):
                    S.dma("sp", lambda e, a_=a_: e.dma_start(out=xg_d[a_ * 128:(a_ + 1) * 128, :], in_=junk[:]),
                          reads=["junk"], writes=["xg_init"])
            with ExitStack() as mx:
                oT = sb("oT", [128, 4, S_], BF16, mx)
                with ExitStack() as ph:
                    accO = sb("accO", [128, 4, S_], F32, ph)
                    ph2 = ExitStack()
                    wh = [sb("wh%d" % i, [128, 8, 3, 128], BF16, ph2) for i in range(2)]
                    qA0 = [sb("qA0_%d" % i, [128, S_], BF16, ph2) for i in range(2)]
                    q0B = [sb("q0B_%d" % i, [128, S_], BF16, ph2) for i in range(2)]
                    kAB = [sb("kAB%d" % i, [128, S_], BF16, ph2) for i in range(2)]
                    vhp = [sb("vhp%d" % i, [128, NT, 2, 65], BF16, ph2) for i in range(2)]
                    pt = [sb("pt%d" % i, [128, 384], BF16, ph2) for i in range(3)]
                    tstack[0].close()
                    Bx = [ph2.enter_context(nc.psum_tensor("ps_Bx%d_%d" % (i, tgen[0]), [128, 512], F32)) for i in range(2)]
                    sc_banks = [(B[2], "B2"), (B[3], "B3"), (Bx[0], "Bx0")]
                    pv_banks = [(B[4], "B4"), (B[5], "B5"), (Bx[1], "Bx1")]
                    wqkv = w_in_d[l][:, 0:2304].rearrange("(c p) (t n) -> p c t n", p=128, t=3)
                    wh_loaded = set()

                    def load_wh(p):
                        if p in wh_loaded:
                            return
                        wh_loaded.add(p)
                        for t3 in range(3):
                            S.dma("pool", lambda e, p=p, t3=t3: e.dma_start(
                                out=wh[p % 2][:, :, t3, :], in_=wqkv[:, :, t3, p * 128:(p + 1) * 128]), writes=["wh%d" % (p % 2)])

                    load_wh(0)
                    for i in range(2):
                        S.op("pool", lambda e, i=i: e.memset(qA0[i][:], 0.0), writes=["qA0_%d" % i])
                        S.op("pool", lambda e, i=i: e.memset(q0B[i][:], 0.0), writes=["q0B_%d" % i])
                    for i in range(2):
                        S.op("pool", lambda e, i=i: e.memset(vhp[i][:], 1.0), writes=["vhp%d" % i])
                    S.op("pool", lambda e: e.memset(accO[:], 0.0), writes=["accO%d" % i for i in range(4)])
                    rot = [0]

                    def proj_gen(p):
                        gi = p // 2
                        dil = GROUPS[gi][0]
                        nsub = dil
                        nblk = 16 // dil
                        b_ = p % 2
                        w_ = wh[b_]
                        wr_ = "wh%d" % b_
                        load_wh(p)
                        for tb in range(4):
                            tsl = slice(tb * 512, (tb + 1) * 512)
                            pp, pr = next_pp()
                            for c in range(8):
                                S.op("pe", lambda e, pp=pp, c=c, tsl=tsl, w_=w_: e.matmul(
                                    out=pp[:], lhsT=w_[:, c, 0, :], rhs=xnT[:, c, tsl], start=(c == 0), stop=(c == 7)),
                                    reads=[wr_] + xall[4 * tb:4 * tb + 4], writes=[pr])
                            S.op("act", lambda e, pp=pp, tsl=tsl: e.activation(
                                out=qA0[b_][0:64, tsl], in_=pp[0:64, :], func=AF.Copy, scale=0.125), reads=[pr], writes=["qA0_%d" % b_])
                            S.op("act", lambda e, pp=pp, tsl=tsl: e.activation(
                                out=q0B[b_][64:128, tsl], in_=pp[64:128, :], func=AF.Copy, scale=0.125), reads=[pr], writes=["q0B_%d" % b_])
                            yield
                        for tb in range(4):
                            tsl = slice(tb * 512, (tb + 1) * 512)
                            pp, pr = next_pp()
                            for c in range(8):
                                S.op("pe", lambda e, pp=pp, c=c, tsl=tsl, w_=w_: e.matmul(
                                    out=pp[:], lhsT=w_[:, c, 1, :], rhs=xnT[:, c, tsl], start=(c == 0), stop=(c == 7)),
                                    reads=[wr_] + xall[4 * tb:4 * tb + 4], writes=[pr])
                            S.op("dve", lambda e, pp=pp, tsl=tsl: e.tensor_copy(out=kAB[b_][:, tsl], in_=pp[:]),
                                 reads=[pr], writes=["kAB%d" % b_])
                            yield
                        for sub in range(nsub):
                            for blk in range(nblk):
                                ti = sub * nblk + blk
                                tsl = tok_slice(dil, sub, blk)
                                pp, pr = next_pp()
                                for c in range(8):
                                    S.op("pe", lambda e, pp=pp, c=c, tsl=tsl, w_=w_: e.matmul(
                                        out=pp[:, 0:128], lhsT=xnT[:, c, tsl], rhs=w_[:, c, 2, :],
                                        start=(c == 0), stop=(c == 7)), reads=[wr_] + xall, writes=[pr])
                                S.op("dve", lambda e, pp=pp, ti=ti: e.tensor_copy(
                                    out=vhp[b_][:, ti, :, 0:64], in_=pp[:, 0:128].rearrange("p (a b) -> p a b", a=2)),
                                    reads=[pr], writes=["vhp%d" % b_])
                                yield

                    def attn_gen(hd):
                        gi = hd // 4
                        hs = hd % 4
                        dil = GROUPS[gi][0]
                        nsub = dil
                        nblk = 16 // dil
                        b_ = (hd // 2) % 2
                        ab = hd % 2
                        qT = (qA0, q0B)[ab][b_]
                        qTr = ("qA0_%d", "q0B_%d")[ab] % b_
                        kT, kTr = kAB[b_], "kAB%d" % b_
                        vh, vhr = vhp[b_], "vhp%d" % b_
                        for sub in range(nsub):
                            for blk in range(nblk):
                                qsl = tok_slice(dil, sub, blk)
                                kts = [(blk + dk, dk + 1) for dk in (-1, 0, 1) if 0 <= blk + dk < nblk]
                                n = len(kts)
                                b0 = bias_index(hd, kts[0][1])
                                ri = rot[0] % 3
                                rot[0] += 1
                                psc, pscr = sc_banks[ri]
                                pob, pobr = pv_banks[ri]
                                ptb, ptr = pt[ri], "pt%d" % ri
                                S.op("pe", lambda e, psc=psc, n=n, b0=b0: e.matmul(
                                    out=psc[:, 0:n * 128], lhsT=identb[:], rhs=biasT[:, b0:b0 + n, :].rearrange("p a b -> p (a b)"),
                                    start=True, stop=False), reads=["identb", "biasT"], writes=[pscr])
                                for j, (kb, ty) in enumerate(kts):
                                    ksl = tok_slice(dil, sub, kb)
                                    S.op("pe", lambda e, psc=psc, j=j, n=n, ksl=ksl, qsl=qsl, qT=qT, kT=kT: e.matmul(
                                        out=psc[:, j * 128:(j + 1) * 128], lhsT=kT[:, ksl], rhs=qT[:, qsl],
                                        start=False, stop=(j == n - 1)), reads=[qTr, kTr], writes=[pscr])
                                S.op("act", lambda e, psc=psc, ptb=ptb, n=n: e.activation(
                                    out=ptb[:, 0:n * 128], in_=psc[:, 0:n * 128], func=AF.Exp), reads=[pscr], writes=[ptr])
                                for j, (kb, ty) in enumerate(kts):
                                    S.op("pe", lambda e, pob=pob, ptb=ptb, j=j, n=n, ti=sub * nblk + kb, vh=vh: e.matmul(
                                        out=pob[0:65, 0:128], lhsT=vh[:, ti, ab, :], rhs=ptb[:, j * 128:(j + 1) * 128],
                                        start=(j == 0), stop=(j == n - 1)), reads=[vhr, ptr], writes=[pobr])
                                S.op("dve", lambda e, pob=pob, hs=hs, qsl=qsl: e.tensor_tensor(
                                    out=accO[0:65, hs, qsl], in0=accO[0:65, hs, qsl], in1=pob[0:65, 0:128], op=ALU.add),
                                    reads=[pobr, "accO%d" % hs], writes=["accO%d" % hs])
                                yield

                    pp_banks[0] = [0, 1]
                    for _ in proj_gen(0):
                        pass
                    NPROJ, NATT = 24, 32
                    for p in range(6):
                        pg_ = proj_gen(p + 1) if p + 1 < 6 else None
                        done, step = 0, 0
                        for hd in (2 * p, 2 * p + 1):
                            for _ in attn_gen(hd):
                                step += 1
                                if pg_ is not None:
                                    want = (step * NPROJ + NATT - 1) // NATT
                                    while done < want:
                                        if next(pg_, "end") == "end":
                                            pg_ = None
                                            break
                                        done += 1
                        if pg_ is not None:
                            for _ in pg_:
                                pass
                    S.barrier()
                    ph2.close()
                    alloc_T()
                    pp_banks[0] = [0, 1, 2, 3, 4, 5]
                    rec = sb("rec", [128, 512], F32, ph)
                    for hs in range(4):
                        for tb in range(4):
                            tsl = slice(tb * 512, (tb + 1) * 512)
                            pp, pr = next_pp()
                            S.op("pe", lambda e, pp=pp, hs=hs, tsl=tsl: e.matmul(
                                out=pp[0:64, :], lhsT=sel65[0:65, :], rhs=accO[0:65, hs, tsl], start=True, stop=True),
                                reads=["cf", "accO%d" % hs], writes=[pr])
                            S.op("act", lambda e, pp=pp: e.activation(out=rec[0:64, :], in_=pp[0:64, :], func=AF.Ln),
                                 reads=[pr], writes=["rec"])
                            S.op("act", lambda e: e.activation(out=rec[0:64, :], in_=rec[0:64, :], func=AF.Exp, scale=-1.0),
                                 reads=["rec"], writes=["rec"])
                            S.op("dve", lambda e, hs=hs, tsl=tsl: e.tensor_tensor(
                                out=oT[0:64, hs, tsl], in0=accO[0:64, hs, tsl], in1=rec[0:64, :], op=ALU.mult),
                                reads=["rec", "accO%d" % hs], writes=["oT"])
                    S.barrier()
                mixedT = sb("mixedT", [128, 4, S_], BF16, mx)
                with ExitStack() as ph:
                    wu = sb("wu", [128, 8, 512], BF16, ph)
                    pw = sb("pw", [128, 4, 128], BF16, ph)
                    U2 = [sb("U%d" % i, [128, S_ + 32], F32, ph) for i in range(2)]
                    T1 = sb("T1", [128, S_ + 32], F32, ph)
                    T2 = sb("T2", [128, S_ + 32], F32, ph)
                    pooled = sb("pooled", [128, S_], BF16, ph)
                    S.dma("pool", lambda e: e.dma_start(
                        out=wu[:], in_=w_in_d[l][:, 2304:2816].rearrange("(c p) n -> p c n", p=128)), writes=["wu"])
                    S.dma("pool", lambda e: e.dma_start(
                        out=pw[:], in_=pool_w_d[l].rearrange("g c e -> c g e")), writes=["pw"])
                    for i in range(2):
                        S.op("pool", lambda e, i=i: e.memset(U2[i][:], 0.0), writes=["U%d" % i])

                    def uproj(g):
                        Ug, Ur = U2[g % 2], "U%d" % (g % 2)
                        for tb in range(4):
                            pp, pr = next_pp()
                            for c in range(8):
                                S.op("pe", lambda e, pp=pp, c=c, tb=tb, g=g: e.matmul(
                                    out=pp[:], lhsT=wu[:, c, g * 128:(g + 1) * 128], rhs=xnT[:, c, tb * 512:(tb + 1) * 512],
                                    start=(c == 0), stop=(c == 7)), reads=["wu"] + xall[4 * tb:4 * tb + 4], writes=[pr])
                            S.op("act", lambda e, pp=pp, tb=tb, Ug=Ug: e.copy(out=Ug[:, 16 + tb * 512:16 + (tb + 1) * 512], in_=pp[:]),
                                 reads=[pr], writes=[Ur])

                    uproj(0)
                    for g in range(4):
                        w = POOLW[g]
                        U, Ur = U2[g % 2], "U%d" % (g % 2)
                        if g + 1 < 4:
                            uproj(g + 1)
                        S.op("dve", lambda e, U=U: e.tensor_tensor(out=T1[:, 8:2072], in0=U[:, 7:2071], in1=U[:, 8:2072], op=ALU.add),
                             reads=[Ur], writes=["T1"])
                        cur, curr, oth, othr = T1, "T1", T2, "T2"
                        lo, hi, s = 8, 2072, 1
                        for lvl in range(g):
                            lo += 2 * s if lvl > 0 else 2
                            nlo, nhi = (10, 2070) if lvl == 0 else ((12, 2068) if lvl == 1 else (16, 2064))
                            S.op("dve", lambda e, cur=cur, oth=oth, s=s, nlo=nlo, nhi=nhi: e.tensor_tensor(
                                out=oth[:, nlo:nhi], in0=cur[:, nlo - s:nhi - s], in1=cur[:, nlo + s:nhi + s], op=ALU.add),
                                reads=[curr], writes=[othr])
                            cur, curr, oth, othr = oth, othr, cur, curr
                            s *= 2
                        S.op("dve", lambda e, cur=cur, oth=oth, w=w: e.tensor_scalar(
                            out=oth[:, 16:2064], in0=cur[:, 16:2064], scalar1=1.0 / w, scalar2=None, op0=ALU.mult),
                            reads=[curr], writes=[othr])
                        S.op("dve", lambda e, oth=oth, g=g: e.tensor_tensor(
                            out=oth[:, 16:24], in0=oth[:, 16:24], in1=cf[:, C_EDGE + g * 16:C_EDGE + g * 16 + 8], op=ALU.mult),
                            reads=["cf", othr], writes=[othr])
                        S.op("dve", lambda e, oth=oth, g=g: e.tensor_tensor(
                            out=oth[:, 2056:2064], in0=oth[:, 2056:2064], in1=cf[:, C_EDGE + g * 16 + 8:C_EDGE + g * 16 + 16],
                            op=ALU.mult), reads=["cf", othr], writes=[othr])
                        S.op("dve", lambda e, oth=oth, U=U: e.tensor_tensor(
                            out=pooled[:], in0=oth[:, 16:2064], in1=U[:, 16:2064], op=ALU.subtract),
                            reads=[othr, Ur], writes=["pooled"])
                        for tb in range(4):
                            pp, pr = next_pp()
                            S.op("pe", lambda e, pp=pp, g=g, tb=tb: e.matmul(
                                out=pp[:], lhsT=pw[:, g, :], rhs=pooled[:, tb * 512:(tb + 1) * 512], start=True, stop=True),
                                reads=["pw", "pooled"], writes=[pr])
                            S.op("act", lambda e, pp=pp, g=g, tb=tb: e.activation(
                                out=mixedT[:, g, tb * 512:(tb + 1) * 512], in_=pp[:], func=AF.Copy,
                                scale=pscale[:, l * 4 + g:l * 4 + g + 1]), reads=[pr, "pscale"], writes=["mixedT"])
                    S.barrier()
                with ExitStack() as ph:
                    wpa = sb("wpa", [128, 4, D], BF16, ph)
                    wpp = sb("wpp", [128, 4, D], BF16, ph)
                    wo = sb("wo", [128, 8, D], BF16, ph)
                    wgt = [sb("wgt%d" % i, [128, 8, 2, 128], BF16, ph) for i in range(2)]
                    g0 = sb("g0", [128, 512], F32, ph)
                    g1 = sb("g1", [128, 512], F32, ph)
                    t1 = sb("t1", [128, 512], F32, ph)
                    t2 = sb("t2", [128, 512], F32, ph)
                    yT = sb("yT", [128, 8, 512], BF16, ph)
                    S.dma("pool", lambda e: e.dma_start(
                        out=wpa[0:64, :, :], in_=w_pa_d[l].rearrange("(h p) n -> p h n", p=64)), writes=["wpa"])
                    S.dma("pool", lambda e: e.dma_start(
                        out=wpp[:], in_=w_pp_d[l].rearrange("(g p) n -> p g n", p=128)), writes=["wpp"])
                    S.dma("pool", lambda e: e.dma_start(
                        out=wo[:], in_=w_out_d[l].rearrange("(c p) n -> p c n", p=128)), writes=["wo"])
                    wgates = w_in_d[l][:, 2816:4864].rearrange("(c p) (t n) -> p c t n", p=128, t=2)
                    it = 0
                    for tb in range(4):
                        tsl = slice(tb * 512, (tb + 1) * 512)
                        xr = xall[4 * tb:4 * tb + 4]
                        for dc in range(8):
                            wg_ = wgt[it % 2]
                            wgr = "wgt%d" % (it % 2)
                            it += 1
                            for t2_ in range(2):
                                S.dma("pool", lambda e, wg_=wg_, dc=dc, t2_=t2_: e.dma_start(
                                    out=wg_[:, :, t2_, :], in_=wgates[:, :, t2_, dc * 128:(dc + 1) * 128]), writes=[wgr])
                            pc, pcr = next_pp()
                            for c in range(8):
                                S.op("pe", lambda e, pc=pc, c=c, wg_=wg_: e.matmul(
                                    out=pc[:], lhsT=wg_[:, c, 0, :], rhs=xnT[:, c, tsl], start=(c == 0), stop=(c == 7)),
                                    reads=[wgr] + xr, writes=[pcr])
                            pd, pdr = next_pp()
                            for c in range(8):
                                S.op("pe", lambda e, pd=pd, c=c, wg_=wg_: e.matmul(
                                    out=pd[:], lhsT=wg_[:, c, 1, :], rhs=xnT[:, c, tsl], start=(c == 0), stop=(c == 7)),
                                    reads=[wgr] + xr, writes=[pdr])
                            pa, par = next_pp()
                            for hs in range(4):
                                S.op("pe", lambda e, pa=pa, hs=hs, dc=dc: e.matmul(
                                    out=pa[:], lhsT=wpa[0:64, hs, dc * 128:(dc + 1) * 128], rhs=oT[0:64, hs, tsl],
                                    start=(hs == 0), stop=(hs == 3)), reads=["wpa", "oT"], writes=[par])
                            pb, pbr = next_pp()
                            for g in range(4):
                                S.op("pe", lambda e, pb=pb, g=g, dc=dc: e.matmul(
                                    out=pb[:], lhsT=wpp[:, g, dc * 128:(dc + 1) * 128], rhs=mixedT[:, g, tsl],
                                    start=(g == 0), stop=(g == 3)), reads=["wpp", "mixedT"], writes=[pbr])
                            S.op("act", lambda e, pc=pc: e.activation(out=g0[:], in_=pc[:], func=AF.Sigmoid),
                                 reads=[pcr], writes=["g0"])
                            S.op("act", lambda e, pd=pd: e.activation(out=g1[:], in_=pd[:], func=AF.Sigmoid),
                                 reads=[pdr], writes=["g1"])
                            S.op("dve", lambda e, pa=pa: e.tensor_tensor(out=t1[:], in0=g0[:], in1=pa[:], op=ALU.mult),
                                 reads=["g0", par], writes=["t1"])
                            S.op("dve", lambda e, pb=pb: e.tensor_tensor(out=t2[:], in0=g1[:], in1=pb[:], op=ALU.mult),
                                 reads=["g1", pbr], writes=["t2"])
                            S.op("dve", lambda e, dc=dc: e.tensor_tensor(out=yT[:, dc, :], in0=t1[:], in1=t2[:], op=ALU.add),
                                 reads=["t1", "t2"], writes=["yT"])
                        for ti in range(4):
                            tt = tb * 4 + ti
                            for nh in range(2):
                                pp, pr = next_pp()
                                for c in range(8):
                                    S.op("pe", lambda e, pp=pp, c=c, ti=ti, nh=nh: e.matmul(
                                        out=pp[:], lhsT=yT[:, c, ti * 128:(ti + 1) * 128], rhs=wo[:, c, nh * 512:(nh + 1) * 512],
                                        start=(c == 0), stop=(c == 7)), reads=["yT", "wo"], writes=[pr])
                                S.op("dve", lambda e, pp=pp, tt=tt, nh=nh: e.tensor_tensor(
                                    out=h[:, tt, nh * 512:(nh + 1) * 512], in0=h[:, tt, nh * 512:(nh + 1) * 512], in1=pp[:],
                                    op=ALU.add), reads=[pr, "h%d" % tt], writes=["h%d" % tt])
                    S.barrier()

        def moe(l):
            rms_stats(2 * l + 1)
            with ExitStack() as ph:
                desti = sb("desti", [128, NT, 2], I32, ph)
                cwt = sb("cwt", [128, NT, 2], F32, ph)
                NSLOT, NPRE = 8, 4
                wslot = [sb("ws%d" % i, [128, 4096], BF16, ph) for i in range(NPRE)]

                def wview(m):
                    t = wslot[m % NSLOT]
                    if m % 3 == 2:
                        return t[:].rearrange("p (c n) -> p c n", c=4)
                    return t[:].rearrange("p (c n) -> p c n", c=8)

                def issue_w(m):
                    if m >= 64 * 3:
                        return
                    ex_, j_ = m // 3, m % 3
                    if j_ == 2:
                        src = wd_d[l, ex_].rearrange("(c p) n -> p c n", p=128)
                        dst = wview(m)
                    else:
                        src = (wg_d, wu_d)[j_][l, ex_].rearrange("(c p r) n -> p c (r n)", p=128, r=2)
                        dst = wslot[m % NSLOT][:].rearrange("p (c n) -> p c n", c=4)
                    S.dma("pool", lambda e, src=src, dst=dst: e.dma_start(out=dst, in_=src), writes=["ws%d" % (m % NSLOT)])

                for m_ in range(NPRE):
                    issue_w(m_)
                rp = ExitStack()
                T = NT
                destf = sb("destf", [128, T, 2], F32, rp)
                wr = sb("wr", [128, 8, 72], F32, rp)
                xf2 = [sb("xf%d" % i, [128, D], F32, rp) for i in range(2)]
                xfT2 = [sb("xfT%d" % i, [128, 8, 128], F32, rp) for i in range(2)]
                lgA = sb("lgA", [128, T, 72], F32, rp)
                ohgA = sb("ohgA", [128, T, 8], F32, rp)
                t8A = sb("t8A", [128, T, 8], F32, rp)
                emA = sb("emA", [128, T, 64], F32, rp)
                em2A = sb("em2A", [128, T, 64], F32, rp)
                oh1A = sb("oh1A", [128, T, 64], F32, rp)
                oh2A = sb("oh2A", [128, T, 64], F32, rp)
                ohEA = sb("ohEA", [128, T, 64], F32, rp)
                t64A = sb("t64A", [128, T, 64], F32, rp)
                csA = sb("csA", [128, T, 64], F32, rp)
                rankA = sb("rankA", [128, T, 64], F32, rp)
                smA = sb("smA", [128, 16, T], F32, rp)
                iota64 = cf[:, C_IOTA:C_IOTA + 64]
                xtk = lambda tt, c0=0, n=1024: xnT[:, tt // 2, (tt % 2) * 1024 + c0:(tt % 2) * 1024 + c0 + n]
                S.dma("sp", lambda e: e.dma_start(out=wr[:], in_=wr_d[l].rearrange("(c p) n -> p c n", p=128)), writes=["wr"])
                S.op("pool", lambda e: e.memset(csA[:, 0, :], 0.0), writes=["csA"])
                def r_stage_a(tt):
                    xf = xf2[tt % 2]
                    xfr = "xf%d" % (tt % 2)
                    norm_tile(tt, xf[:], xfr)
                    S.op("pool", lambda e, tt=tt, xf=xf: e.tensor_copy(out=xtk(tt), in_=xf[:]), reads=[xfr], writes=["xtok%d" % tt])
                    xfT = xfT2[tt % 2]
                    for half in range(2):
                        bk = 2 + 2 * (tt % 2) + half
                        bname = "B%d" % bk
                        for c4 in range(4):
                            c = half * 4 + c4
                            S.op("pe", lambda e, c=c, c4=c4, bk=bk, xf=xf: e.transpose(
                                out=B[bk][:, c4 * 128:(c4 + 1) * 128], in_=xf[:, c * 128:(c + 1) * 128], identity=ident),
                                reads=[xfr, "cf"], writes=[bname])
                        xres = "xfT%d_%d" % (tt % 2, half)
                        if half == 0:
                            S.op("act", lambda e, bk=bk, xfT=xfT: e.copy(
                                out=xfT[:, 0:4, :], in_=B[bk][:].rearrange("p (a b) -> p a b", a=4)), reads=[bname], writes=[xres])
                        else:
                            S.op("dve", lambda e, bk=bk, xfT=xfT: e.tensor_copy(
                                out=xfT[:, 4:8, :], in_=B[bk][:].rearrange("p (a b) -> p a b", a=4)), reads=[bname], writes=[xres])

                def r_stage_b(tt):
                    xfT = xfT2[tt % 2]
                    pp, pr = next_pp()
                    for c in range(8):
                        S.op("pe", lambda e, c=c, pp=pp, xfT=xfT: e.matmul(out=pp[:, 0:72], lhsT=xfT[:, c, :], rhs=wr[:, c, :],
                                                                           start=(c == 0), stop=(c == 7)),
                             reads=["xfT%d_0" % (tt % 2), "xfT%d_1" % (tt % 2), "wr"], writes=[pr])
                    S.op("dve", lambda e, tt=tt, pp=pp: e.tensor_copy(out=lgA[:, tt, :], in_=pp[:, 0:72]), reads=[pr], writes=["lgA"])

                pp_banks[0] = [0, 1]
                for tt in range(NT):
                    r_stage_a(tt)
                    if tt > 0:
                        r_stage_b(tt - 1)
                r_stage_b(NT - 1)
                pp_banks[0] = [0, 1, 2, 3, 4, 5]
                sA = lambda j: smA[:, j, :]
                bc = lambda ap2, n: ap2.unsqueeze(2).to_broadcast([128, T, n])
                V = lambda fn, reads, writes: S.op("dve", fn, reads=reads, writes=writes)
                V(lambda e: e.tensor_reduce(out=sA(0), in_=lgA[:, :, 0:8], axis=AX.X, op=ALU.max), ["lgA"], ["smA"])
                V(lambda e: e.tensor_tensor(out=ohgA[:], in0=lgA[:, :, 0:8], in1=bc(sA(0), 8), op=ALU.is_equal),
                  ["lgA", "smA"], ["ohgA"])
                V(lambda e: e.tensor_tensor(out=t8A[:], in0=lgA[:, :, 0:8], in1=bc(sA(0), 8), op=ALU.subtract),
                  ["lgA", "smA"], ["t8A"])
                S.op("act", lambda e: e.activation(out=t8A[:], in_=t8A[:], func=AF.Exp), reads=["t8A"], writes=["t8A"])
                V(lambda e: e.tensor_reduce(out=sA(2), in_=t8A[:], axis=AX.X, op=ALU.add), ["t8A"], ["smA"])
                V(lambda e: e.reciprocal(out=sA(3), in_=sA(2)), ["smA"], ["smA"])
                V(lambda e: e.tensor_scalar(out=t8A[:], in0=ohgA[:], scalar1=-1.0, scalar2=1e9, op0=ALU.add, op1=ALU.mult),
                  ["ohgA", "t8A"], ["t8A"])
                V(lambda e: e.tensor_tensor(out=emA[:].rearrange("p t (g e) -> p t g e", g=8),
                                            in0=lgA[:, :, 8:72].rearrange("p t (g e) -> p t g e", g=8),
                                            in1=t8A[:].unsqueeze(3).to_broadcast([128, T, 8, 8]), op=ALU.add),
                  ["lgA", "t8A"], ["emA"])
                V(lambda e: e.tensor_reduce(out=sA(4), in_=emA[:], axis=AX.X, op=ALU.max), ["emA"], ["smA"])
                V(lambda e: e.tensor_tensor(out=oh1A[:], in0=emA[:], in1=bc(sA(4), 64), op=ALU.is_equal), ["emA", "smA"], ["oh1A"])
                V(lambda e: e.scalar_tensor_tensor(out=em2A[:], in0=oh1A[:], scalar=-1e9, in1=emA[:], op0=ALU.mult, op1=ALU.add),
                  ["oh1A", "emA"], ["em2A"])
                V(lambda e: e.tensor_reduce(out=sA(5), in_=em2A[:], axis=AX.X, op=ALU.max), ["em2A"], ["smA"])
                V(lambda e: e.tensor_tensor(out=oh2A[:], in0=em2A[:], in1=bc(sA(5), 64), op=ALU.is_equal), ["em2A", "smA"], ["oh2A"])
                V(lambda e: e.tensor_tensor(out=sA(6), in0=sA(5), in1=sA(4), op=ALU.subtract), ["smA"], ["smA"])
                S.op("act", lambda e: e.activation(out=sA(7), in_=sA(6), func=AF.Exp), reads=["smA"], writes=["smA"])
                V(lambda e: e.tensor_scalar(out=sA(8), in0=sA(7), scalar1=1.0, scalar2=None, op0=ALU.add), ["smA"], ["smA"])
                V(lambda e: e.reciprocal(out=sA(9), in_=sA(8)), ["smA"], ["smA"])
                V(lambda e: e.tensor_tensor(out=sA(10), in0=sA(9), in1=sA(3), op=ALU.mult), ["smA"], ["smA"])
                V(lambda e: e.tensor_tensor(out=sA(11), in0=sA(10), in1=sA(7), op=ALU.mult), ["smA"], ["smA"])
                V(lambda e: e.tensor_tensor(out=ohEA[:], in0=oh1A[:], in1=oh2A[:], op=ALU.add), ["oh1A", "oh2A"], ["ohEA"])
                for tt in range(1, NT):
                    V(lambda e, tt=tt: e.tensor_tensor(out=csA[:, tt, :], in0=csA[:, tt - 1, :], in1=ohEA[:, tt - 1, :], op=ALU.add),
                      ["csA", "ohEA"], ["csA"])
                for half in range(2):
                    pk, pkr = next_pp()
                    for t8 in range(8):
                        tt = half * 8 + t8
                        S.op("pe", lambda e, tt=tt, t8=t8, pk=pk: e.matmul(out=pk[:, t8 * 64:(t8 + 1) * 64], lhsT=tri,
                                                                            rhs=ohEA[:, tt, :], start=True, stop=False),
                             reads=["cf", "ohEA"], writes=[pkr])
                        S.op("pe", lambda e, tt=tt, t8=t8, pk=pk: e.matmul(out=pk[:, t8 * 64:(t8 + 1) * 64], lhsT=ones,
                                                                            rhs=csA[:, tt, :], start=False, stop=True),
                             reads=["cf", "csA"], writes=[pkr])
                    S.op("act", lambda e, half=half, pk=pk: e.copy(out=rankA[:, half * 8:(half + 1) * 8, :],
                                                                   in_=pk[:].rearrange("p (a c) -> p a c", c=64)),
                         reads=[pkr], writes=["rankA"])
                for k, ohk, ohr in ((0, oh1A, "oh1A"), (1, oh2A, "oh2A")):
                    V(lambda e, ohk=ohk: e.tensor_tensor(out=t64A[:], in0=ohk[:], in1=rankA[:], op=ALU.mult),
                      ["rankA", ohr, "t64A"], ["t64A"])
                    V(lambda e, k=k: e.tensor_reduce(out=sA(12 + k), in_=t64A[:], axis=AX.X, op=ALU.add), ["t64A"], ["smA"])
                    V(lambda e, ohk=ohk: e.tensor_tensor(out=t64A[:], in0=ohk[:], in1=iota64.unsqueeze(1).to_broadcast([128, T, 64]),
                                                         op=ALU.mult), ["cf", ohr, "t64A"], ["t64A"])
                    V(lambda e, k=k: e.tensor_reduce(out=sA(14 + k), in_=t64A[:], axis=AX.X, op=ALU.add), ["t64A"], ["smA"])
                    V(lambda e, k=k: e.scalar_tensor_tensor(out=destf[:, :, k], in0=sA(14 + k), scalar=float(CAPE), in1=sA(12 + k),
                                                            op0=ALU.mult, op1=ALU.add), ["smA"], ["destf"])
                    V(lambda e, k=k: e.tensor_scalar(out=sA(1), in0=sA(12 + k), scalar1=float(CAPE), scalar2=1e6,
                                                     op0=ALU.is_ge, op1=ALU.mult), ["smA"], ["smA"])
                    V(lambda e, k=k: e.tensor_tensor(out=destf[:, :, k], in0=destf[:, :, k], in1=sA(1), op=ALU.add),
                      ["smA", "destf"], ["destf"])
                    V(lambda e, k=k: e.tensor_scalar(out=sA(1), in0=sA(12 + k), scalar1=float(CAPE), scalar2=None, op0=ALU.is_lt),
                      ["smA"], ["smA"])
                    V(lambda e, k=k: e.tensor_tensor(out=cwt[:, :, k], in0=sA(10 + k), in1=sA(1), op=ALU.mult),
                      ["smA"], ["cwt"])
                S.op("dve", lambda e: e.tensor_copy(out=desti[:], in_=destf[:]), reads=["destf"], writes=["desti"])
                S.barrier()
                rp.close()
                xgs = []
                for tt in range(NT):
                    for k in range(2):
                        nm = "xgs%d" % (tt * 2 + k)
                        xgs.append(nm)
                        S.dma("pool", lambda e, tt=tt, k=k: e.indirect_dma_start(
                            out=xg_d, out_offset=bass.IndirectOffsetOnAxis(ap=desti[:, tt, k:k + 1], axis=0),
                            in_=xtk(tt), in_offset=None, bounds_check=bc_reg, oob_is_err=False),
                            reads=["xtok%d" % tt, "desti", "xg_init"], writes=[nm])
                xe = [sb("xe%d" % i, [128, D], BF16, ph) for i in range(2)]
                xeT = [sb("xeT%d" % i, [128, 8, 128], BF16, ph) for i in range(2)]
                wslot += [sb("ws%d" % i, [128, 4096], BF16, ph) for i in range(NPRE, NSLOT)]
                for m_ in range(NPRE, NSLOT):
                    issue_w(m_)
                sil = [sb("sil%d" % i, [128, 512], F32, ph) for i in range(2)]
                hw = [sb("hw%d" % i, [128, 512], BF16, ph) for i in range(2)]
                ye = [sb("ye%d" % i, [128, D], BF16, ph) for i in range(2)]
                gy = [sb("gy%d" % i, [128, D], BF16, ph) for i in range(4)]
                xnb2 = [sb("xnbm%d" % i, [128, D], BF16, ph) for i in range(2)] if l == 0 else None
                for i in range(4):
                    S.op("pool", lambda e, i=i: e.memset(gy[i][:], 0.0), writes=["gy%d" % i])
                ygs = []
                def stage_a(ex):
                    k = ex % 2
                    wg_v, wu_v = wview(3 * ex), wview(3 * ex + 1)
                    wg_r, wu_r = ["ws%d" % ((3 * ex + j_) % NSLOT) for j_ in range(2)]
                    S.dma("sp", lambda e, k=k, ex=ex: e.dma_start(out=xe[k][:], in_=xg_d[ex * CAPE:(ex + 1) * CAPE, :]),
                          reads=xgs, writes=["xe%d" % k])
                    for c in range(8):
                        S.op("pe", lambda e, c=c, k=k: e.transpose(out=Tb[k][:, c, :], in_=xe[k][:, (c // 2) * 256 + (c % 2):(c // 2) * 256 + 256:2],
                                                                   identity=identb[:]),
                             reads=["xe%d" % k, "identb"], writes=["T%d" % k])
                    S.op("act", lambda e, k=k: e.copy(out=xeT[k][:], in_=Tb[k][:]), reads=["T%d" % k], writes=["xeT%d" % k])
                    pg, pgr = next_pq()
                    for fc in range(4):
                        for c in range(8):
                            S.op("pe", lambda e, pg=pg, c=c, fc=fc, k=k: e.matmul(
                                out=pg[:, fc * 128:(fc + 1) * 128], lhsT=wg_v[:, c, fc * 128:(fc + 1) * 128], rhs=xeT[k][:, c, :],
                                start=(c == 0), stop=(c == 7)), reads=[wg_r, "xeT%d" % k], writes=[pgr])
                    issue_w(3 * ex + NSLOT)
                    pu, pur = next_pq()
                    for fc in range(4):
                        for c in range(8):
                            S.op("pe", lambda e, pu=pu, c=c, fc=fc, k=k: e.matmul(
                                out=pu[:, fc * 128:(fc + 1) * 128], lhsT=wu_v[:, c, fc * 128:(fc + 1) * 128], rhs=xeT[k][:, c, :],
                                start=(c == 0), stop=(c == 7)), reads=[wu_r, "xeT%d" % k], writes=[pur])
                    issue_w(3 * ex + 1 + NSLOT)
                    S.op("act", lambda e, pg=pg, k=k: e.activation(out=sil[k][:], in_=pg[:], func=AF.Silu),
                         reads=[pgr], writes=["sil%d" % k])
                    S.op("dve", lambda e, pu=pu, k=k: e.tensor_tensor(out=hw[k][:], in0=sil[k][:], in1=pu[:], op=ALU.mult),
                         reads=["sil%d" % k, pur], writes=["hw%d" % k])

                def stage_b(ex):
                    k = ex % 2
                    wd_v = wview(3 * ex + 2)
                    wd_r = "ws%d" % ((3 * ex + 2) % NSLOT)
                    for nh in range(2):
                        pp, pr = next_pq()
                        for fc in range(4):
                            S.op("pe", lambda e, pp=pp, fc=fc, nh=nh, k=k: e.matmul(
                                out=pp[:], lhsT=hw[k][:, fc * 128:(fc + 1) * 128], rhs=wd_v[:, fc, nh * 512:(nh + 1) * 512],
                                start=(fc == 0), stop=(fc == 3)), reads=["hw%d" % k, wd_r], writes=[pr])
                        if nh == 0:
                            S.op("act", lambda e, pp=pp, k=k: e.copy(out=ye[k][:, 0:512], in_=pp[:]), reads=[pr], writes=["ye%d" % k])
                        else:
                            S.op("dve", lambda e, pp=pp, k=k: e.tensor_copy(out=ye[k][:, 512:1024], in_=pp[:]),
                                 reads=[pr], writes=["ye%d" % k])
                    issue_w(3 * ex + 2 + NSLOT)
                    nm = "ygs%d" % ex
                    ygs.append(nm)
                    S.dma("sp", lambda e, k=k, ex=ex: e.dma_start(out=yg_d[ex * CAPE:(ex + 1) * CAPE, :], in_=ye[k][:]),
                          reads=["ye%d" % k], writes=[nm])

                for ex in range(64):
                    stage_a(ex)
                    if ex > 0:
                        stage_b(ex - 1)
                stage_b(63)
                S.dma("sp", lambda e: e.dma_start(out=gv[:], in_=gvec_d[:, 2 if l == 0 else 4, :]), writes=["gv"])
                gi_box = [0]

                def c_stage1(tt):
                    for k in range(2):
                        j = gi_box[0] % 4
                        gi_box[0] += 1
                        S.dma("pool", lambda e, tt=tt, k=k, j=j: e.indirect_dma_start(
                            out=gy[j][:, :], out_offset=None, in_=yg_d,
                            in_offset=bass.IndirectOffsetOnAxis(ap=desti[:, tt, k:k + 1], axis=0),
                            bounds_check=bc_reg, oob_is_err=False),
                            reads=ygs + ["desti"], writes=["gy%d" % j])
                        S.op("dve", lambda e, tt=tt, k=k, j=j: e.scalar_tensor_tensor(
                            out=h[:, tt, :], in0=gy[j][:], scalar=cwt[:, tt, k:k + 1], in1=h[:, tt, :],
                            op0=ALU.mult, op1=ALU.add), reads=["gy%d" % j, "cwt", "h%d" % tt], writes=["h%d" % tt])
                    rms_a(tt)

                def c_stage2(tt):
                    rr = rms_b(tt)
                    if l == 0:
                        xb, xr = xnb2[tt % 2], "xnbm%d" % (tt % 2)
                        norm_tile(tt, xb[:], xr, rres=rr)
                        Tt, Tr = Tb[tt % 2], "T%d" % (tt % 2)
                        for c in range(8):
                            S.op("pe", lambda e, c=c, xb=xb, Tt=Tt: e.transpose(out=Tt[:, c, :], in_=xb[:, c * 128:(c + 1) * 128],
                                                                                identity=identb[:]),
                                 reads=[xr, "identb"], writes=[Tr])
                        if tt % 2 == 0:
                            S.op("act", lambda e, tt=tt, Tt=Tt: e.copy(out=xnT[:, :, tt * 128:(tt + 1) * 128], in_=Tt[:]),
                                 reads=[Tr], writes=["xnT%d" % tt])
                        else:
                            S.op("dve", lambda e, tt=tt, Tt=Tt: e.tensor_copy(out=xnT[:, :, tt * 128:(tt + 1) * 128], in_=Tt[:]),
                                 reads=[Tr], writes=["xnT%d" % tt])
                    else:
                        for hf in range(2):
                            csl = slice(hf * 512, (hf + 1) * 512)
                            S.op("dve", lambda e, tt=tt, hf=hf, csl=csl: e.scalar_tensor_tensor(
                                out=sil[hf][:], in0=h[:, tt, csl], scalar=rstd[:, tt:tt + 1], in1=gv[:, csl],
                                op0=ALU.mult, op1=ALU.mult), reads=["h%d" % tt, rr, "gv"], writes=["sil%d" % hf])
                            S.dma("sp", lambda e, tt=tt, hf=hf, csl=csl: e.dma_start(out=out_d[tt * 128:(tt + 1) * 128, csl], in_=sil[hf][:]),
                                  reads=["sil%d" % hf])

                for tt in range(NT):
                    c_stage1(tt)
                    if tt > 0:
                        c_stage2(tt - 1)
                c_stage2(NT - 1)
                S.barrier()

        def dump_h(dst):
            for tt in range(NT):
                S.dma("sp", lambda e, tt=tt: e.dma_start(out=dst[tt * 128:(tt + 1) * 128, :], in_=h[:, tt, :]),
                      reads=["h%d" % tt])

        stages = [(0, "mix"), (0, "moe"), (1, "mix"), (1, "moe")]
        for si, (l, kind) in enumerate(stages):
            if kind == "mix":
                mixer(l)
            else:
                moe(l)
            if dbg and dbg == si + 1:
                dump_h(dbg_d)
        S.finish()
    return nc


def _t5_bucket(rel):
    nb = 16
    ret = np.where(rel > 0, nb, 0)
    n = np.abs(rel)
    max_exact = 8
    nf = np.maximum(n, max_exact).astype(np.float32)
    large = max_exact + (np.log(nf / np.float32(max_exact)) / np.float32(math.log(1024 / max_exact))
                         * np.float32(nb - max_exact)).astype(np.int32)
    large = np.minimum(large, nb - 1)
    return ret + np.where(n < max_exact, n, large)


def _host_consts(rel_bias):
    kk = np.arange(128)[:, None]
    qq = np.arange(128)[None, :]
    biasT = np.zeros((128, 28, 128), np.float32)
    for hd in range(12):
        dil = GROUPS[hd // 4][0]
        for ty in range(3):
            if hd >= 8 and ty != 1:
                continue
            rel = kk - qq + (ty - 1) * 128
            b = rel_bias[_t5_bucket(rel * dil), hd]
            biasT[:, bias_index(hd, ty), :] = np.where(np.abs(rel) <= 64, b, np.float32(NEGM))
    cf = np.zeros((128, C_N), np.float32)
    cf[:, C_ID:C_ID + 128] = np.eye(128, dtype=np.float32)
    cf[:, C_TRI:C_TRI + 128] = (kk < qq).astype(np.float32)
    cf[:, C_ONE:C_ONE + 128] = 1.0
    cf[:, C_IOTA:C_IOTA + CAPG] = np.arange(CAPG, dtype=np.float32)[None, :]
    for g, w in enumerate(POOLW):
        e = np.ones(16, np.float32)
        for i in range(8):
            cnt = min(i + w - w // 2, S_) - max(i - w // 2, 0)
            e[i] = w / cnt
            ii = S_ - 8 + i
            cnt = min(ii + w - w // 2, S_) - max(ii - w // 2, 0)
            e[8 + i] = w / cnt
        cf[:, C_EDGE + g * 16:C_EDGE + g * 16 + 16] = e[None, :]
    cf[64, C_SEL:C_SEL + 64] = 1.0
    return biasT, cf


_NC_CACHE = {}


def kernel(x, rel_bias, norm_mix_g, w_in, pool_w, pool_scale, w_proj_attn, w_proj_pool, w_out, norm_ffn_g,
           w_router_group, w_router_expert, w_gate_e, w_up_e, w_down_e, norm_final_g, _dbg=0, _cores=8):
    f = lambda a: np.ascontiguousarray(np.asarray(a, dtype=np.float32))
    x = f(x)
    rel_bias = f(rel_bias)
    biasT, cf = _host_consts(rel_bias)
    gvec = np.stack([f(norm_mix_g)[0], f(norm_ffn_g)[0], f(norm_mix_g)[1], f(norm_ffn_g)[1], f(norm_final_g)], 0)
    gvec = np.ascontiguousarray(np.broadcast_to(gvec[None], (128, 5, D)))
    pscale = np.ascontiguousarray(f(pool_scale).reshape(2, 4, 128).transpose(2, 0, 1).reshape(128, 8))
    wre = f(w_router_expert).transpose(0, 2, 1, 3).reshape(2, D, 64)
    wr = np.ascontiguousarray(np.concatenate([f(w_router_group), wre], axis=2))
    shared = {
        "w_in": f(w_in), "pool_w": f(pool_w), "w_proj_attn": f(w_proj_attn), "w_proj_pool": f(w_proj_pool),
        "w_out": f(w_out), "wr": wr, "w_gate_e": f(w_gate_e), "w_up_e": f(w_up_e), "w_down_e": f(w_down_e),
        "gvec": gvec, "pscale": pscale, "biasT": biasT, "cf32": cf,
    }
    key = _dbg
    if key not in _NC_CACHE:
        _NC_CACHE[key] = build(_dbg)
    nc = _NC_CACHE[key]
    in_maps = []
    for b in range(_cores):
        m = dict(shared)
        m["x"] = np.ascontiguousarray(x[b])
        in_maps.append(m)
    res = run_bass_kernel_spmd(nc, in_maps, core_ids=list(range(_cores)))
    out = np.stack([res.results[b]["out"] for b in range(_cores)], 0).astype(np.float32)
    if _dbg:
        return out, np.stack([res.results[b]["dbg"] for b in range(_cores)], 0)
    return out
```

```python
import math
from contextlib import ExitStack

import numpy as np
import concourse.bass as bass
import concourse.mybir as mybir
from concourse.bass_utils import run_bass_kernel_spmd

F32 = mybir.dt.float32
BF16 = mybir.dt.bfloat16
I32 = mybir.dt.int32
ALU = mybir.AluOpType
AF = mybir.ActivationFunctionType
AX = mybir.AxisListType

S_ = 2048
D = 1024
NT = 16
INW = 4864
CAPG = 384
CAPE = 128
NEGM = -30000.0
GROUPS = ((1, 0), (4, 1), (16, 2))
POOLW = (2, 4, 8, 16)
C_ID, C_TRI, C_ONE, C_IOTA, C_EDGE, C_SEL = 0, 128, 256, 384, 768, 832
C_N = 896


class Sched:
    ENG = ["pe", "act", "dve", "pool", "sp"]
    RING = 12

    def __init__(self, nc, es):
        self.nc = nc
        self.obj = {"pe": nc.tensor, "act": nc.scalar, "dve": nc.vector, "pool": nc.gpsimd, "sp": nc.sync}
        self.sem = {e: es.enter_context(nc.semaphore("s_" + e)) for e in self.ENG}
        self.cnt = {e: 0 for e in self.ENG}
        self.seen = {x: {} for x in self.ENG}
        self.lastw = {}
        self.reads = {}
        self.ring = {}
        self.ring_i = {}
        for q in ("sp", "pool"):
            self.ring[q] = []
            self.ring_i[q] = 0
            for i in range(self.RING):
                name = "q_%s_%d" % (q, i)
                self.sem[name] = es.enter_context(nc.semaphore(name))
                self.cnt[name] = 0
                self.ring[q].append(name)

    def _dep(self, x, e, ts, raw=False):
        if e == x and x in ("pe", "sp"):
            return
        if self.seen[x].get(e, 0) < ts:
            self.seen[x][e] = ts
            self.obj[x].wait_ge(self.sem[e], ts)

    def _deps(self, x, reads, writes):
        for r in reads:
            w = self.lastw.get(r)
            if w is not None:
                self._dep(x, w[0], w[1], raw=True)
        for r in writes:
            w = self.lastw.get(r)
            if w is not None:
                self._dep(x, *w)
            rd = self.reads.get(r)
            if rd:
                for e, ts in rd.items():
                    self._dep(x, e, ts)

    def _mark(self, who, ts, reads, writes):
        for r in reads:
            self.reads.setdefault(r, {})[who] = ts
        for r in writes:
            self.lastw[r] = (who, ts)
            self.reads[r] = {}

    def op(self, x, fn, reads=(), writes=()):
        self._deps(x, reads, writes)
        self.cnt[x] += 1
        ts = self.cnt[x]
        fn(self.obj[x]).then_inc(self.sem[x], 1)
        self._mark(x, ts, reads, writes)

    def dma(self, q, fn, reads=(), writes=()):
        self._deps(q, reads, writes)
        i = self.ring_i[q]
        self.ring_i[q] = (i + 1) % self.RING
        name = self.ring[q][i]
        if self.cnt[name] > 0:
            self._dep(q, name, self.cnt[name])
        self.cnt[name] += 16
        ts = self.cnt[name]
        fn(self.obj[q]).then_inc(self.sem[name], 16)
        self._mark(name, ts, reads, writes)

    def barrier(self):
        for x in self.ENG:
            for e in self.cnt:
                if self.cnt[e] > 0:
                    self._dep(x, e, self.cnt[e])

    def finish(self):
        for name in self.cnt:
            if name.startswith("q_") and self.cnt[name] > 0:
                self._dep("sp", name, self.cnt[name])
        for e in self.ENG:
            if self.cnt[e] > 0:
                self._dep("sp", e, self.cnt[e])


def bias_index(h, ty):
    return h * 3 + ty if h < 8 else 24 + (h - 8)


def tok_slice(dil, sub, blk):
    st = sub + dil * 128 * blk
    return slice(st, st + dil * 127 + 1, dil)


def build(dbg=0):
    nc = bass.Bass("TRN2", target_bir_lowering=False)
    dt = lambda name, shape, d=F32, kind="ExternalInput": nc.dram_tensor(name, shape, d, kind=kind).ap()
    x_d = dt("x", [S_, D])
    w_in_d = dt("w_in", [2, D, INW])
    pool_w_d = dt("pool_w", [2, 4, 128, 128])
    w_pa_d = dt("w_proj_attn", [2, 256, D])
    w_pp_d = dt("w_proj_pool", [2, 512, D])
    w_out_d = dt("w_out", [2, D, D])
    wr_d = dt("wr", [2, D, 72])
    wg_d = dt("w_gate_e", [2, 64, D, 512])
    wu_d = dt("w_up_e", [2, 64, D, 512])
    wd_d = dt("w_down_e", [2, 64, 512, D])
    gvec_d = dt("gvec", [128, 5, D])
    pscale_d = dt("pscale", [128, 8])
    bias_d = dt("biasT", [128, 28, 128])
    cf_d = dt("cf32", [128, C_N])
    xg_d = dt("xg_scr", [64 * CAPE, D], BF16, kind="Internal")
    yg_d = dt("yg_scr", [64 * CAPE, D], BF16, kind="Internal")
    out_d = dt("out", [S_, D], kind="ExternalOutput")
    dbg_d = dt("dbg", [S_, D], kind="ExternalOutput") if dbg else None

    with ExitStack() as es:
        uid = [0]

        def sb(name, shape, d=F32, st=es):
            uid[0] += 1
            return st.enter_context(nc.sbuf_tensor("sb%d_%s" % (uid[0], name), shape, d))

        psum = lambda name, shape, d=F32: es.enter_context(nc.psum_tensor("ps_" + name, shape, d))
        S = Sched(nc, es)
        h = sb("h", [128, NT, D])
        xnT = sb("xnT", [128, 8, S_], BF16)
        biasT = sb("biasT", [128, 28, 128], BF16)
        cf = sb("cf", [128, C_N])
        gv = sb("gv", [128, D])
        pscale = sb("pscale", [128, 8])
        identb = sb("identb", [128, 128], BF16)
        ssq = sb("ssq", [128, NT])
        rstd = sb("rstd", [128, NT])
        junk = sb("junk", [128, D], BF16)
        B = [psum("B%d" % i, [128, 512]) for i in range(6)]
        tgen = [0]
        tstack = [ExitStack()]
        Tb = [None, None]

        def alloc_T():
            tgen[0] += 1
            tstack[0] = ExitStack()
            for i in range(2):
                Tb[i] = tstack[0].enter_context(nc.psum_tensor("ps_T%d_%d" % (i, tgen[0]), [128, 8, 128], BF16))

        alloc_T()
        es.callback(lambda: tstack[0].close())
        ident = cf[:, C_ID:C_ID + 128]
        tri = cf[:, C_TRI:C_TRI + 128]
        ones = cf[:, C_ONE:C_ONE + 128]
        iota = cf[:, C_IOTA:C_IOTA + CAPG]
        sel65 = cf[:, C_SEL:C_SEL + 64]

        S.dma("sp", lambda e: e.dma_start(out=cf[:], in_=cf_d), writes=["cf"])
        S.dma("pool", lambda e: e.dma_start(out=biasT[:], in_=bias_d), writes=["biasT"])
        S.dma("sp", lambda e: e.dma_start(out=pscale[:], in_=pscale_d), writes=["pscale"])
        for tt in range(NT):
            S.dma("sp", lambda e, tt=tt: e.dma_start(out=h[:, tt, :], in_=x_d[tt * 128:(tt + 1) * 128, :]),
                  writes=["h%d" % tt])
        S.op("dve", lambda e: e.tensor_copy(out=identb[:], in_=ident), reads=["cf"], writes=["identb"])

        pp_i = [0]
        pp_banks = [[0, 1, 2, 3, 4, 5]]

        pp_extra = {}

        def next_pp():
            lst = pp_banks[0]
            pp_i[0] = (pp_i[0] + 1) % len(lst)
            i = lst[pp_i[0]]
            if i >= 6:
                return pp_extra[i]
            return B[i], "B%d" % i

        bc_reg = nc.gpsimd.alloc_register("bc_reg")
        nc.gpsimd.reg_mov(bc_reg, 64 * CAPE - 1)
        pq_i = [0]
        pq_list = [(B[i], "B%d" % i) for i in range(6)]

        def next_pq():
            i = pq_i[0]
            pq_i[0] = (i + 1) % 6
            return pq_list[i]

        def rms_stats(gidx):
            S.dma("sp", lambda e: e.dma_start(out=gv[:], in_=gvec_d[:, gidx, :]), writes=["gv"])
            for tt in range(NT):
                S.op("act", lambda e, tt=tt: e.activation(out=junk[:], in_=h[:, tt, :], func=AF.Square,
                                                          accum_out=ssq[:, tt:tt + 1]),
                     reads=["h%d" % tt], writes=["junk", "ssq"])
            S.op("dve", lambda e: e.tensor_scalar(out=rstd[:], in0=ssq[:], scalar1=1.0 / D, scalar2=1e-6,
                                                  op0=ALU.mult, op1=ALU.add), reads=["ssq"], writes=["rstd"])
            S.op("act", lambda e: e.activation(out=rstd[:], in_=rstd[:], func=AF.Sqrt), reads=["rstd"], writes=["rstd"])
            S.op("dve", lambda e: e.reciprocal(out=rstd[:], in_=rstd[:]), reads=["rstd"], writes=["rstd"])

        def norm_tile(tt, out_ap, out_res, rres="rstd"):
            S.op("dve", lambda e: e.scalar_tensor_tensor(out=out_ap, in0=h[:, tt, :], scalar=rstd[:, tt:tt + 1],
                                                         in1=gv[:], op0=ALU.mult, op1=ALU.mult),
                 reads=["h%d" % tt, rres, "gv"], writes=[out_res])

        def rms_tile(tt):
            rms_a(tt)
            return rms_b(tt)

        def rms_a(tt):
            rr = "rstd_t%d" % tt
            S.op("act", lambda e: e.activation(out=junk[:], in_=h[:, tt, :], func=AF.Square, accum_out=ssq[:, tt:tt + 1]),
                 reads=["h%d" % tt], writes=["junk", "ssq_t%d" % tt])
            S.op("dve", lambda e: e.tensor_scalar(out=rstd[:, tt:tt + 1], in0=ssq[:, tt:tt + 1], scalar1=1.0 / D, scalar2=1e-6,
                                                  op0=ALU.mult, op1=ALU.add), reads=["ssq_t%d" % tt], writes=[rr])

        def rms_b(tt):
            rr = "rstd_t%d" % tt
            S.op("act", lambda e: e.activation(out=rstd[:, tt:tt + 1], in_=rstd[:, tt:tt + 1], func=AF.Ln), reads=[rr], writes=[rr])
            S.op("act", lambda e: e.activation(out=rstd[:, tt:tt + 1], in_=rstd[:, tt:tt + 1], func=AF.Exp, scale=-0.5),
                 reads=[rr], writes=[rr])
            return rr

        def mixer(l):
            m0 = ExitStack()
            if l == 0:
                rms_stats(2 * l)
                xnb = [sb("xnb%d" % i, [128, D], BF16, m0) for i in range(2)]
            for tt in (range(NT) if l == 0 else ()):
                xb = xnb[tt % 2]
                xr = "xnb%d" % (tt % 2)
                norm_tile(tt, xb[:], xr)
                Tt, Tr = Tb[tt % 2], "T%d" % (tt % 2)
                for c in range(8):
                    S.op("pe", lambda e, c=c, xb=xb, Tt=Tt: e.transpose(out=Tt[:, c, :], in_=xb[:, c * 128:(c + 1) * 128],
                                                                        identity=identb[:]),
                         reads=[xr, "identb"], writes=[Tr])
                if tt % 2 == 0:
                    S.op("act", lambda e, tt=tt, Tt=Tt: e.copy(out=xnT[:, :, tt * 128:(tt + 1) * 128], in_=Tt[:]),
                         reads=[Tr], writes=["xnT%d" % tt])
                else:
                    S.op("dve", lambda e, tt=tt, Tt=Tt: e.tensor_copy(out=xnT[:, :, tt * 128:(tt + 1) * 128], in_=Tt[:]),
                         reads=[Tr], writes=["xnT%d" % tt])
            xall = ["xnT%d" % t for t in range(NT)]
            S.barrier()
            m0.close()
            if l == 0:
                S.op("pool", lambda e: e.memset(junk[:], 0.0), writes=["junk"])
                for a_ in range(64 * CAPE // 128):
                    S.dma("sp", lambda e, a_=a_: e.dma_start(out=xg_d[a_ * 128:(a_ + 1) * 128, :], in_=junk[:]),
                          reads=["junk"], writes=["xg_init"])
            with ExitStack() as mx:
                oT = sb("oT", [128, 4, S_], BF16, mx)
                with ExitStack() as ph:
                    accO = sb("accO", [128, 4, S_], F32, ph)
                    ph2 = ExitStack()
                    wh = [sb("wh%d" % i, [128, 8, 3, 128], BF16, ph2) for i in range(2)]
                    qA0 = [sb("qA0_%d" % i, [128, S_], BF16, ph2) for i in range(2)]
                    q0B = [sb("q0B_%d" % i, [128, S_], BF16, ph2) for i in range(2)]
                    kAB = [sb("kAB%d" % i, [128, S_], BF16, ph2) for i in range(2)]
                    vhp = [sb("vhp%d" % i, [128, NT, 2, 65], BF16, ph2) for i in range(2)]
                    pt = [sb("pt%d" % i, [128, 384], BF16, ph2) for i in range(3)]
                    tstack[0].close()
                    Bx = [ph2.enter_context(nc.psum_tensor("ps_Bx%d_%d" % (i, tgen[0]), [128, 512], F32)) for i in range(2)]
                    sc_banks = [(B[2], "B2"), (B[3], "B3")]
                    pv_banks = [(B[4], "B4"), (B[5], "B5")]
                    pp_extra[6], pp_extra[7] = (Bx[0], "Bx0"), (Bx[1], "Bx1")
                    wqkv = w_in_d[l][:, 0:2304].rearrange("(c p) (t n) -> p c t n", p=128, t=3)
                    wh_loaded = set()

                    def load_wh(p):
                        if p in wh_loaded:
                            return
                        wh_loaded.add(p)
                        for t3 in range(3):
                            S.dma("pool", lambda e, p=p, t3=t3: e.dma_start(
                                out=wh[p % 2][:, :, t3, :], in_=wqkv[:, :, t3, p * 128:(p + 1) * 128]), writes=["wh%d" % (p % 2)])

                    load_wh(0)
                    for i in range(2):
                        S.op("pool", lambda e, i=i: e.memset(qA0[i][:], 0.0), writes=["qA0_%d" % i])
                        S.op("pool", lambda e, i=i: e.memset(q0B[i][:], 0.0), writes=["q0B_%d" % i])
                    for i in range(2):
                        S.op("pool", lambda e, i=i: e.memset(vhp[i][:], 1.0), writes=["vhp%d" % i])
                    S.op("pool", lambda e: e.memset(accO[:], 0.0), writes=["accO%d" % i for i in range(4)])
                    rot = [0]

                    def proj_gen(p):
                        gi = p // 2
                        dil = GROUPS[gi][0]
                        nsub = dil
                        nblk = 16 // dil
                        b_ = p % 2
                        w_ = wh[b_]
                        wr_ = "wh%d" % b_
                        load_wh(p)
                        for tb in range(4):
                            tsl = slice(tb * 512, (tb + 1) * 512)
                            pp, pr = next_pp()
                            for c in range(8):
                                S.op("pe", lambda e, pp=pp, c=c, tsl=tsl, w_=w_: e.matmul(
                                    out=pp[:], lhsT=w_[:, c, 0, :], rhs=xnT[:, c, tsl], start=(c == 0), stop=(c == 7)),
                                    reads=[wr_] + xall[4 * tb:4 * tb + 4], writes=[pr])
                            S.op("act", lambda e, pp=pp, tsl=tsl: e.activation(
                                out=qA0[b_][0:64, tsl], in_=pp[0:64, :], func=AF.Copy, scale=0.125), reads=[pr], writes=["qA0_%d" % b_])
                            S.op("act", lambda e, pp=pp, tsl=tsl: e.activation(
                                out=q0B[b_][64:128, tsl], in_=pp[64:128, :], func=AF.Copy, scale=0.125), reads=[pr], writes=["q0B_%d" % b_])
                            yield
                        for tb in range(4):
                            tsl = slice(tb * 512, (tb + 1) * 512)
                            pp, pr = next_pp()
                            for c in range(8):
                                S.op("pe", lambda e, pp=pp, c=c, tsl=tsl, w_=w_: e.matmul(
                                    out=pp[:], lhsT=w_[:, c, 1, :], rhs=xnT[:, c, tsl], start=(c == 0), stop=(c == 7)),
                                    reads=[wr_] + xall[4 * tb:4 * tb + 4], writes=[pr])
                            S.op("dve", lambda e, pp=pp, tsl=tsl: e.tensor_copy(out=kAB[b_][:, tsl], in_=pp[:]),
                                 reads=[pr], writes=["kAB%d" % b_])
                            yield
                        for sub in range(nsub):
                            for blk in range(nblk):
                                ti = sub * nblk + blk
                                tsl = tok_slice(dil, sub, blk)
                                pp, pr = next_pp()
                                for c in range(8):
                                    S.op("pe", lambda e, pp=pp, c=c, tsl=tsl, w_=w_: e.matmul(
                                        out=pp[:, 0:128], lhsT=xnT[:, c, tsl], rhs=w_[:, c, 2, :],
                                        start=(c == 0), stop=(c == 7)), reads=[wr_] + xall, writes=[pr])
                                S.op("dve", lambda e, pp=pp, ti=ti: e.tensor_copy(
                                    out=vhp[b_][:, ti, :, 0:64], in_=pp[:, 0:128].rearrange("p (a b) -> p a b", a=2)),
                                    reads=[pr], writes=["vhp%d" % b_])
                                yield

                    def attn_gen(hd):
                        gi = hd // 4
                        hs = hd % 4
                        dil = GROUPS[gi][0]
                        nsub = dil
                        nblk = 16 // dil
                        b_ = (hd // 2) % 2
                        ab = hd % 2
                        qT = (qA0, q0B)[ab][b_]
                        qTr = ("qA0_%d", "q0B_%d")[ab] % b_
                        kT, kTr = kAB[b_], "kAB%d" % b_
                        vh, vhr = vhp[b_], "vhp%d" % b_
                        for sub in range(nsub):
                            for blk in range(nblk):
                                qsl = tok_slice(dil, sub, blk)
                                kts = [(blk + dk, dk + 1) for dk in (-1, 0, 1) if 0 <= blk + dk < nblk]
                                n = len(kts)
                                b0 = bias_index(hd, kts[0][1])
                                ri = rot[0] % 2
                                rot[0] += 1
                                psc, pscr = sc_banks[ri]
                                pob, pobr = pv_banks[ri]
                                ptb, ptr = pt[ri], "pt%d" % ri
                                S.op("pe", lambda e, psc=psc, n=n, b0=b0: e.matmul(
                                    out=psc[:, 0:n * 128], lhsT=identb[:], rhs=biasT[:, b0:b0 + n, :].rearrange("p a b -> p (a b)"),
                                    start=True, stop=False), reads=["identb", "biasT"], writes=[pscr])
                                for j, (kb, ty) in enumerate(kts):
                                    ksl = tok_slice(dil, sub, kb)
                                    S.op("pe", lambda e, psc=psc, j=j, n=n, ksl=ksl, qsl=qsl, qT=qT, kT=kT: e.matmul(
                                        out=psc[:, j * 128:(j + 1) * 128], lhsT=kT[:, ksl], rhs=qT[:, qsl],
                                        start=False, stop=(j == n - 1)), reads=[qTr, kTr], writes=[pscr])
                                S.op("act", lambda e, psc=psc, ptb=ptb, n=n: e.activation(
                                    out=ptb[:, 0:n * 128], in_=psc[:, 0:n * 128], func=AF.Exp), reads=[pscr], writes=[ptr])
                                for j, (kb, ty) in enumerate(kts):
                                    S.op("pe", lambda e, pob=pob, ptb=ptb, j=j, n=n, ti=sub * nblk + kb, vh=vh: e.matmul(
                                        out=pob[0:65, 0:128], lhsT=vh[:, ti, ab, :], rhs=ptb[:, j * 128:(j + 1) * 128],
                                        start=(j == 0), stop=(j == n - 1)), reads=[vhr, ptr], writes=[pobr])
                                S.op("dve", lambda e, pob=pob, hs=hs, qsl=qsl: e.tensor_tensor(
                                    out=accO[0:65, hs, qsl], in0=accO[0:65, hs, qsl], in1=pob[0:65, 0:128], op=ALU.add),
                                    reads=[pobr, "accO%d" % hs], writes=["accO%d" % hs])
                                yield

                    pp_banks[0] = [0, 1, 6, 7]
                    for _ in proj_gen(0):
                        pass
                    NPROJ, NATT = 24, 32
                    for p in range(6):
                        pg_ = proj_gen(p + 1) if p + 1 < 6 else None
                        done, step = 0, 0
                        for hd in (2 * p, 2 * p + 1):
                            for _ in attn_gen(hd):
                                step += 1
                                if pg_ is not None:
                                    want = (step * NPROJ + NATT - 1) // NATT
                                    while done < want:
                                        if next(pg_, "end") == "end":
                                            pg_ = None
                                            break
                                        done += 1
                        if pg_ is not None:
                            for _ in pg_:
                                pass
                    S.barrier()
                    ph2.close()
                    alloc_T()
                    pp_banks[0] = [0, 1, 2, 3, 4, 5]
                    rec = sb("rec", [128, 512], F32, ph)
                    for hs in range(4):
                        for tb in range(4):
                            tsl = slice(tb * 512, (tb + 1) * 512)
                            pp, pr = next_pp()
                            S.op("pe", lambda e, pp=pp, hs=hs, tsl=tsl: e.matmul(
                                out=pp[0:64, :], lhsT=sel65[0:65, :], rhs=accO[0:65, hs, tsl], start=True, stop=True),
                                reads=["cf", "accO%d" % hs], writes=[pr])
                            S.op("act", lambda e, pp=pp: e.activation(out=rec[0:64, :], in_=pp[0:64, :], func=AF.Ln),
                                 reads=[pr], writes=["rec"])
                            S.op("act", lambda e: e.activation(out=rec[0:64, :], in_=rec[0:64, :], func=AF.Exp, scale=-1.0),
                                 reads=["rec"], writes=["rec"])
                            S.op("dve", lambda e, hs=hs, tsl=tsl: e.tensor_tensor(
                                out=oT[0:64, hs, tsl], in0=accO[0:64, hs, tsl], in1=rec[0:64, :], op=ALU.mult),
                                reads=["rec", "accO%d" % hs], writes=["oT"])
                    S.barrier()
                mixedT = sb("mixedT", [128, 4, S_], BF16, mx)
                with ExitStack() as ph:
                    wu = sb("wu", [128, 8, 512], BF16, ph)
                    pw = sb("pw", [128, 4, 128], BF16, ph)
                    U2 = [sb("U%d" % i, [128, S_ + 32], F32, ph) for i in range(2)]
                    T1 = sb("T1", [128, S_ + 32], F32, ph)
                    T2 = sb("T2", [128, S_ + 32], F32, ph)
                    pooled = sb("pooled", [128, S_], BF16, ph)
                    S.dma("pool", lambda e: e.dma_start(
                        out=wu[:], in_=w_in_d[l][:, 2304:2816].rearrange("(c p) n -> p c n", p=128)), writes=["wu"])
                    S.dma("pool", lambda e: e.dma_start(
                        out=pw[:], in_=pool_w_d[l].rearrange("g c e -> c g e")), writes=["pw"])
                    for i in range(2):
                        S.op("pool", lambda e, i=i: e.memset(U2[i][:], 0.0), writes=["U%d" % i])

                    def uproj(g):
                        Ug, Ur = U2[g % 2], "U%d" % (g % 2)
                        for tb in range(4):
                            pp, pr = next_pp()
                            for c in range(8):
                                S.op("pe", lambda e, pp=pp, c=c, tb=tb, g=g: e.matmul(
                                    out=pp[:], lhsT=wu[:, c, g * 128:(g + 1) * 128], rhs=xnT[:, c, tb * 512:(tb + 1) * 512],
                                    start=(c == 0), stop=(c == 7)), reads=["wu"] + xall[4 * tb:4 * tb + 4], writes=[pr])
                            S.op("act", lambda e, pp=pp, tb=tb, Ug=Ug: e.copy(out=Ug[:, 16 + tb * 512:16 + (tb + 1) * 512], in_=pp[:]),
                                 reads=[pr], writes=[Ur])

                    uproj(0)
                    for g in range(4):
                        w = POOLW[g]
                        U, Ur = U2[g % 2], "U%d" % (g % 2)
                        if g + 1 < 4:
                            uproj(g + 1)
                        S.op("dve", lambda e, U=U: e.tensor_tensor(out=T1[:, 8:2072], in0=U[:, 7:2071], in1=U[:, 8:2072], op=ALU.add),
                             reads=[Ur], writes=["T1"])
                        cur, curr, oth, othr = T1, "T1", T2, "T2"
                        lo, hi, s = 8, 2072, 1
                        for lvl in range(g):
                            lo += 2 * s if lvl > 0 else 2
                            nlo, nhi = (10, 2070) if lvl == 0 else ((12, 2068) if lvl == 1 else (16, 2064))
                            S.op("dve", lambda e, cur=cur, oth=oth, s=s, nlo=nlo, nhi=nhi: e.tensor_tensor(
                                out=oth[:, nlo:nhi], in0=cur[:, nlo - s:nhi - s], in1=cur[:, nlo + s:nhi + s], op=ALU.add),
                                reads=[curr], writes=[othr])
                            cur, curr, oth, othr = oth, othr, cur, curr
                            s *= 2
                        S.op("dve", lambda e, cur=cur, oth=oth, w=w: e.tensor_scalar(
                            out=oth[:, 16:2064], in0=cur[:, 16:2064], scalar1=1.0 / w, scalar2=None, op0=ALU.mult),
                            reads=[curr], writes=[othr])
                        S.op("dve", lambda e, oth=oth, g=g: e.tensor_tensor(
                            out=oth[:, 16:24], in0=oth[:, 16:24], in1=cf[:, C_EDGE + g * 16:C_EDGE + g * 16 + 8], op=ALU.mult),
                            reads=["cf", othr], writes=[othr])
                        S.op("dve", lambda e, oth=oth, g=g: e.tensor_tensor(
                            out=oth[:, 2056:2064], in0=oth[:, 2056:2064], in1=cf[:, C_EDGE + g * 16 + 8:C_EDGE + g * 16 + 16],
                            op=ALU.mult), reads=["cf", othr], writes=[othr])
                        S.op("dve", lambda e, oth=oth, U=U: e.tensor_tensor(
                            out=pooled[:], in0=oth[:, 16:2064], in1=U[:, 16:2064], op=ALU.subtract),
                            reads=[othr, Ur], writes=["pooled"])
                        for tb in range(4):
                            pp, pr = next_pp()
                            S.op("pe", lambda e, pp=pp, g=g, tb=tb: e.matmul(
                                out=pp[:], lhsT=pw[:, g, :], rhs=pooled[:, tb * 512:(tb + 1) * 512], start=True, stop=True),
                                reads=["pw", "pooled"], writes=[pr])
                            S.op("act", lambda e, pp=pp, g=g, tb=tb: e.activation(
                                out=mixedT[:, g, tb * 512:(tb + 1) * 512], in_=pp[:], func=AF.Copy,
                                scale=pscale[:, l * 4 + g:l * 4 + g + 1]), reads=[pr, "pscale"], writes=["mixedT"])
                    S.barrier()
                with ExitStack() as ph:
                    wpa = sb("wpa", [128, 4, D], BF16, ph)
                    wpp = sb("wpp", [128, 4, D], BF16, ph)
                    wo = sb("wo", [128, 8, D], BF16, ph)
                    wgt = [sb("wgt%d" % i, [128, 8, 2, 128], BF16, ph) for i in range(2)]
                    g0 = sb("g0", [128, 512], F32, ph)
                    g1 = sb("g1", [128, 512], F32, ph)
                    t1 = sb("t1", [128, 512], F32, ph)
                    t2 = sb("t2", [128, 512], F32, ph)
                    yT = sb("yT", [128, 8, 512], BF16, ph)
                    S.dma("pool", lambda e: e.dma_start(
                        out=wpa[0:64, :, :], in_=w_pa_d[l].rearrange("(h p) n -> p h n", p=64)), writes=["wpa"])
                    S.dma("pool", lambda e: e.dma_start(
                        out=wpp[:], in_=w_pp_d[l].rearrange("(g p) n -> p g n", p=128)), writes=["wpp"])
                    S.dma("pool", lambda e: e.dma_start(
                        out=wo[:], in_=w_out_d[l].rearrange("(c p) n -> p c n", p=128)), writes=["wo"])
                    wgates = w_in_d[l][:, 2816:4864].rearrange("(c p) (t n) -> p c t n", p=128, t=2)
                    it = 0
                    for tb in range(4):
                        tsl = slice(tb * 512, (tb + 1) * 512)
                        xr = xall[4 * tb:4 * tb + 4]
                        for dc in range(8):
                            wg_ = wgt[it % 2]
                            wgr = "wgt%d" % (it % 2)
                            it += 1
                            for t2_ in range(2):
                                S.dma("pool", lambda e, wg_=wg_, dc=dc, t2_=t2_: e.dma_start(
                                    out=wg_[:, :, t2_, :], in_=wgates[:, :, t2_, dc * 128:(dc + 1) * 128]), writes=[wgr])
                            pc, pcr = next_pp()
                            for c in range(8):
                                S.op("pe", lambda e, pc=pc, c=c, wg_=wg_: e.matmul(
                                    out=pc[:], lhsT=wg_[:, c, 0, :], rhs=xnT[:, c, tsl], start=(c == 0), stop=(c == 7)),
                                    reads=[wgr] + xr, writes=[pcr])
                            pd, pdr = next_pp()
                            for c in range(8):
                                S.op("pe", lambda e, pd=pd, c=c, wg_=wg_: e.matmul(
                                    out=pd[:], lhsT=wg_[:, c, 1, :], rhs=xnT[:, c, tsl], start=(c == 0), stop=(c == 7)),
                                    reads=[wgr] + xr, writes=[pdr])
                            pa, par = next_pp()
                            for hs in range(4):
                                S.op("pe", lambda e, pa=pa, hs=hs, dc=dc: e.matmul(
                                    out=pa[:], lhsT=wpa[0:64, hs, dc * 128:(dc + 1) * 128], rhs=oT[0:64, hs, tsl],
                                    start=(hs == 0), stop=(hs == 3)), reads=["wpa", "oT"], writes=[par])
                            pb, pbr = next_pp()
                            for g in range(4):
                                S.op("pe", lambda e, pb=pb, g=g, dc=dc: e.matmul(
                                    out=pb[:], lhsT=wpp[:, g, dc * 128:(dc + 1) * 128], rhs=mixedT[:, g, tsl],
                                    start=(g == 0), stop=(g == 3)), reads=["wpp", "mixedT"], writes=[pbr])
                            S.op("act", lambda e, pc=pc: e.activation(out=g0[:], in_=pc[:], func=AF.Sigmoid),
                                 reads=[pcr], writes=["g0"])
                            S.op("act", lambda e, pd=pd: e.activation(out=g1[:], in_=pd[:], func=AF.Sigmoid),
                                 reads=[pdr], writes=["g1"])
                            S.op("dve", lambda e, pa=pa: e.tensor_tensor(out=t1[:], in0=g0[:], in1=pa[:], op=ALU.mult),
                                 reads=["g0", par], writes=["t1"])
                            S.op("dve", lambda e, pb=pb: e.tensor_tensor(out=t2[:], in0=g1[:], in1=pb[:], op=ALU.mult),
                                 reads=["g1", pbr], writes=["t2"])
                            S.op("dve", lambda e, dc=dc: e.tensor_tensor(out=yT[:, dc, :], in0=t1[:], in1=t2[:], op=ALU.add),
                                 reads=["t1", "t2"], writes=["yT"])
                        for ti in range(4):
                            tt = tb * 4 + ti
                            for nh in range(2):
                                pp, pr = next_pp()
                                for c in range(8):
                                    S.op("pe", lambda e, pp=pp, c=c, ti=ti, nh=nh: e.matmul(
                                        out=pp[:], lhsT=yT[:, c, ti * 128:(ti + 1) * 128], rhs=wo[:, c, nh * 512:(nh + 1) * 512],
                                        start=(c == 0), stop=(c == 7)), reads=["yT", "wo"], writes=[pr])
                                S.op("dve", lambda e, pp=pp, tt=tt, nh=nh: e.tensor_tensor(
                                    out=h[:, tt, nh * 512:(nh + 1) * 512], in0=h[:, tt, nh * 512:(nh + 1) * 512], in1=pp[:],
                                    op=ALU.add), reads=[pr, "h%d" % tt], writes=["h%d" % tt])
                    S.barrier()

        def moe(l):
            rms_stats(2 * l + 1)
            with ExitStack() as ph:
                desti = sb("desti", [128, NT, 2], I32, ph)
                cwt = sb("cwt", [128, NT, 2], F32, ph)
                NSLOT, NPRE = 8, 4
                wslot = [sb("ws%d" % i, [128, 4096], BF16, ph) for i in range(NPRE)]

                def wview(m):
                    t = wslot[m % NSLOT]
                    if m % 3 == 2:
                        return t[:].rearrange("p (c n) -> p c n", c=4)
                    return t[:].rearrange("p (c n) -> p c n", c=8)

                def issue_w(m):
                    if m >= 64 * 3:
                        return
                    ex_, j_ = m // 3, m % 3
                    if j_ == 2:
                        src = wd_d[l, ex_].rearrange("(c p) n -> p c n", p=128)
                        dst = wview(m)
                    else:
                        src = (wg_d, wu_d)[j_][l, ex_].rearrange("(c p r) n -> p c (r n)", p=128, r=2)
                        dst = wslot[m % NSLOT][:].rearrange("p (c n) -> p c n", c=4)
                    S.dma("pool", lambda e, src=src, dst=dst: e.dma_start(out=dst, in_=src), writes=["ws%d" % (m % NSLOT)])

                for m_ in range(NPRE):
                    issue_w(m_)
                rp = ExitStack()
                T = NT
                destf = sb("destf", [128, T, 2], F32, rp)
                wr = sb("wr", [128, 8, 72], F32, rp)
                xf2 = [sb("xf%d" % i, [128, D], F32, rp) for i in range(2)]
                xfT2 = [sb("xfT%d" % i, [128, 8, 128], F32, rp) for i in range(2)]
                lgA = sb("lgA", [128, T, 72], F32, rp)
                ohgA = sb("ohgA", [128, T, 8], F32, rp)
                t8A = sb("t8A", [128, T, 8], F32, rp)
                emA = sb("emA", [128, T, 64], F32, rp)
                em2A = sb("em2A", [128, T, 64], F32, rp)
                oh1A = sb("oh1A", [128, T, 64], F32, rp)
                oh2A = sb("oh2A", [128, T, 64], F32, rp)
                ohEA = sb("ohEA", [128, T, 64], F32, rp)
                t64A = sb("t64A", [128, T, 64], F32, rp)
                csA = sb("csA", [128, T, 64], F32, rp)
                rankA = sb("rankA", [128, T, 64], F32, rp)
                smA = sb("smA", [128, 16, T], F32, rp)
                iota64 = cf[:, C_IOTA:C_IOTA + 64]
                xtk = lambda tt, c0=0, n=1024: xnT[:, tt // 2, (tt % 2) * 1024 + c0:(tt % 2) * 1024 + c0 + n]
                S.dma("sp", lambda e: e.dma_start(out=wr[:], in_=wr_d[l].rearrange("(c p) n -> p c n", p=128)), writes=["wr"])
                S.op("pool", lambda e: e.memset(csA[:, 0, :], 0.0), writes=["csA"])
                def r_stage_a(tt):
                    xf = xf2[tt % 2]
                    xfr = "xf%d" % (tt % 2)
                    norm_tile(tt, xf[:], xfr)
                    S.op("pool", lambda e, tt=tt, xf=xf: e.tensor_copy(out=xtk(tt), in_=xf[:]), reads=[xfr], writes=["xtok%d" % tt])
                    xfT = xfT2[tt % 2]
                    for half in range(2):
                        bk = 2 + 2 * (tt % 2) + half
                        bname = "B%d" % bk
                        for c4 in range(4):
                            c = half * 4 + c4
                            S.op("pe", lambda e, c=c, c4=c4, bk=bk, xf=xf: e.transpose(
                                out=B[bk][:, c4 * 128:(c4 + 1) * 128], in_=xf[:, c * 128:(c + 1) * 128], identity=ident),
                                reads=[xfr, "cf"], writes=[bname])
                        xres = "xfT%d_%d" % (tt % 2, half)
                        if half == 0:
                            S.op("act", lambda e, bk=bk, xfT=xfT: e.copy(
                                out=xfT[:, 0:4, :], in_=B[bk][:].rearrange("p (a b) -> p a b", a=4)), reads=[bname], writes=[xres])
                        else:
                            S.op("dve", lambda e, bk=bk, xfT=xfT: e.tensor_copy(
                                out=xfT[:, 4:8, :], in_=B[bk][:].rearrange("p (a b) -> p a b", a=4)), reads=[bname], writes=[xres])

                def r_stage_b(tt):
                    xfT = xfT2[tt % 2]
                    pp, pr = next_pp()
                    for c in range(8):
                        S.op("pe", lambda e, c=c, pp=pp, xfT=xfT: e.matmul(out=pp[:, 0:72], lhsT=xfT[:, c, :], rhs=wr[:, c, :],
                                                                           start=(c == 0), stop=(c == 7)),
                             reads=["xfT%d_0" % (tt % 2), "xfT%d_1" % (tt % 2), "wr"], writes=[pr])
                    S.op("dve", lambda e, tt=tt, pp=pp: e.tensor_copy(out=lgA[:, tt, :], in_=pp[:, 0:72]), reads=[pr], writes=["lgA"])

                pp_banks[0] = [0, 1]
                for tt in range(NT):
                    r_stage_a(tt)
                    if tt > 0:
                        r_stage_b(tt - 1)
                r_stage_b(NT - 1)
                pp_banks[0] = [0, 1, 2, 3, 4, 5]
                sA = lambda j: smA[:, j, :]
                bc = lambda ap2, n: ap2.unsqueeze(2).to_broadcast([128, T, n])
                V = lambda fn, reads, writes: S.op("dve", fn, reads=reads, writes=writes)
                V(lambda e: e.tensor_reduce(out=sA(0), in_=lgA[:, :, 0:8], axis=AX.X, op=ALU.max), ["lgA"], ["smA"])
                V(lambda e: e.tensor_tensor(out=ohgA[:], in0=lgA[:, :, 0:8], in1=bc(sA(0), 8), op=ALU.is_equal),
                  ["lgA", "smA"], ["ohgA"])
                V(lambda e: e.tensor_tensor(out=t8A[:], in0=lgA[:, :, 0:8], in1=bc(sA(0), 8), op=ALU.subtract),
                  ["lgA", "smA"], ["t8A"])
                S.op("act", lambda e: e.activation(out=t8A[:], in_=t8A[:], func=AF.Exp), reads=["t8A"], writes=["t8A"])
                V(lambda e: e.tensor_reduce(out=sA(2), in_=t8A[:], axis=AX.X, op=ALU.add), ["t8A"], ["smA"])
                V(lambda e: e.reciprocal(out=sA(3), in_=sA(2)), ["smA"], ["smA"])
                V(lambda e: e.tensor_scalar(out=t8A[:], in0=ohgA[:], scalar1=-1.0, scalar2=1e9, op0=ALU.add, op1=ALU.mult),
                  ["ohgA", "t8A"], ["t8A"])
                V(lambda e: e.tensor_tensor(out=emA[:].rearrange("p t (g e) -> p t g e", g=8),
                                            in0=lgA[:, :, 8:72].rearrange("p t (g e) -> p t g e", g=8),
                                            in1=t8A[:].unsqueeze(3).to_broadcast([128, T, 8, 8]), op=ALU.add),
                  ["lgA", "t8A"], ["emA"])
                V(lambda e: e.tensor_reduce(out=sA(4), in_=emA[:], axis=AX.X, op=ALU.max), ["emA"], ["smA"])
                V(lambda e: e.tensor_tensor(out=oh1A[:], in0=emA[:], in1=bc(sA(4), 64), op=ALU.is_equal), ["emA", "smA"], ["oh1A"])
                V(lambda e: e.scalar_tensor_tensor(out=em2A[:], in0=oh1A[:], scalar=-1e9, in1=emA[:], op0=ALU.mult, op1=ALU.add),
                  ["oh1A", "emA"], ["em2A"])
                V(lambda e: e.tensor_reduce(out=sA(5), in_=em2A[:], axis=AX.X, op=ALU.max), ["em2A"], ["smA"])
                V(lambda e: e.tensor_tensor(out=oh2A[:], in0=em2A[:], in1=bc(sA(5), 64), op=ALU.is_equal), ["em2A", "smA"], ["oh2A"])
                V(lambda e: e.tensor_tensor(out=sA(6), in0=sA(5), in1=sA(4), op=ALU.subtract), ["smA"], ["smA"])
                S.op("act", lambda e: e.activation(out=sA(7), in_=sA(6), func=AF.Exp), reads=["smA"], writes=["smA"])
                V(lambda e: e.tensor_scalar(out=sA(8), in0=sA(7), scalar1=1.0, scalar2=None, op0=ALU.add), ["smA"], ["smA"])
                V(lambda e: e.reciprocal(out=sA(9), in_=sA(8)), ["smA"], ["smA"])
                V(lambda e: e.tensor_tensor(out=sA(10), in0=sA(9), in1=sA(3), op=ALU.mult), ["smA"], ["smA"])
                V(lambda e: e.tensor_tensor(out=sA(11), in0=sA(10), in1=sA(7), op=ALU.mult), ["smA"], ["smA"])
                V(lambda e: e.tensor_tensor(out=ohEA[:], in0=oh1A[:], in1=oh2A[:], op=ALU.add), ["oh1A", "oh2A"], ["ohEA"])
                for tt in range(1, NT):
                    V(lambda e, tt=tt: e.tensor_tensor(out=csA[:, tt, :], in0=csA[:, tt - 1, :], in1=ohEA[:, tt - 1, :], op=ALU.add),
                      ["csA", "ohEA"], ["csA"])
                for half in range(2):
                    pk, pkr = next_pp()
                    for t8 in range(8):
                        tt = half * 8 + t8
                        S.op("pe", lambda e, tt=tt, t8=t8, pk=pk: e.matmul(out=pk[:, t8 * 64:(t8 + 1) * 64], lhsT=tri,
                                                                            rhs=ohEA[:, tt, :], start=True, stop=False),
                             reads=["cf", "ohEA"], writes=[pkr])
                        S.op("pe", lambda e, tt=tt, t8=t8, pk=pk: e.matmul(out=pk[:, t8 * 64:(t8 + 1) * 64], lhsT=ones,
                                                                            rhs=csA[:, tt, :], start=False, stop=True),
                             reads=["cf", "csA"], writes=[pkr])
                    S.op("act", lambda e, half=half, pk=pk: e.copy(out=rankA[:, half * 8:(half + 1) * 8, :],
                                                                   in_=pk[:].rearrange("p (a c) -> p a c", c=64)),
                         reads=[pkr], writes=["rankA"])
                for k, ohk, ohr in ((0, oh1A, "oh1A"), (1, oh2A, "oh2A")):
                    V(lambda e, ohk=ohk: e.tensor_tensor(out=t64A[:], in0=ohk[:], in1=rankA[:], op=ALU.mult),
                      ["rankA", ohr, "t64A"], ["t64A"])
                    V(lambda e, k=k: e.tensor_reduce(out=sA(12 + k), in_=t64A[:], axis=AX.X, op=ALU.add), ["t64A"], ["smA"])
                    V(lambda e, ohk=ohk: e.tensor_tensor(out=t64A[:], in0=ohk[:], in1=iota64.unsqueeze(1).to_broadcast([128, T, 64]),
                                                         op=ALU.mult), ["cf", ohr, "t64A"], ["t64A"])
                    V(lambda e, k=k: e.tensor_reduce(out=sA(14 + k), in_=t64A[:], axis=AX.X, op=ALU.add), ["t64A"], ["smA"])
                    V(lambda e, k=k: e.scalar_tensor_tensor(out=destf[:, :, k], in0=sA(14 + k), scalar=float(CAPE), in1=sA(12 + k),
                                                            op0=ALU.mult, op1=ALU.add), ["smA"], ["destf"])
                    V(lambda e, k=k: e.tensor_scalar(out=sA(1), in0=sA(12 + k), scalar1=float(CAPE), scalar2=1e6,
                                                     op0=ALU.is_ge, op1=ALU.mult), ["smA"], ["smA"])
                    V(lambda e, k=k: e.tensor_tensor(out=destf[:, :, k], in0=destf[:, :, k], in1=sA(1), op=ALU.add),
                      ["smA", "destf"], ["destf"])
                    V(lambda e, k=k: e.tensor_scalar(out=sA(1), in0=sA(12 + k), scalar1=float(CAPE), scalar2=None, op0=ALU.is_lt),
                      ["smA"], ["smA"])
                    V(lambda e, k=k: e.tensor_tensor(out=cwt[:, :, k], in0=sA(10 + k), in1=sA(1), op=ALU.mult),
                      ["smA"], ["cwt"])
                S.op("dve", lambda e: e.tensor_copy(out=desti[:], in_=destf[:]), reads=["destf"], writes=["desti"])
                S.barrier()
                rp.close()
                xgs = []
                for tt in range(NT):
                    for k in range(2):
                        nm = "xgs%d" % (tt * 2 + k)
                        xgs.append(nm)
                        S.dma("pool", lambda e, tt=tt, k=k: e.indirect_dma_start(
                            out=xg_d, out_offset=bass.IndirectOffsetOnAxis(ap=desti[:, tt, k:k + 1], axis=0),
                            in_=xtk(tt), in_offset=None, bounds_check=bc_reg, oob_is_err=False),
                            reads=["xtok%d" % tt, "desti", "xg_init"], writes=[nm])
                xe = [sb("xe%d" % i, [128, D], BF16, ph) for i in range(2)]
                xeT = [sb("xeT%d" % i, [128, 8, 128], BF16, ph) for i in range(2)]
                wslot += [sb("ws%d" % i, [128, 4096], BF16, ph) for i in range(NPRE, NSLOT)]
                for m_ in range(NPRE, NSLOT):
                    issue_w(m_)
                sil = [sb("sil%d" % i, [128, 512], F32, ph) for i in range(2)]
                hw = [sb("hw%d" % i, [128, 512], BF16, ph) for i in range(2)]
                ye = [sb("ye%d" % i, [128, D], BF16, ph) for i in range(2)]
                gy = [sb("gy%d" % i, [128, D], BF16, ph) for i in range(4)]
                xnb2 = [sb("xnbm%d" % i, [128, D], BF16, ph) for i in range(2)] if l == 0 else None
                for i in range(4):
                    S.op("pool", lambda e, i=i: e.memset(gy[i][:], 0.0), writes=["gy%d" % i])
                ygs = []
                def stage_a(ex):
                    k = ex % 2
                    wg_v, wu_v = wview(3 * ex), wview(3 * ex + 1)
                    wg_r, wu_r = ["ws%d" % ((3 * ex + j_) % NSLOT) for j_ in range(2)]
                    S.dma("sp", lambda e, k=k, ex=ex: e.dma_start(out=xe[k][:], in_=xg_d[ex * CAPE:(ex + 1) * CAPE, :]),
                          reads=xgs, writes=["xe%d" % k])
                    for c in range(8):
                        S.op("pe", lambda e, c=c, k=k: e.transpose(out=Tb[k][:, c, :], in_=xe[k][:, (c // 2) * 256 + (c % 2):(c // 2) * 256 + 256:2],
                                                                   identity=identb[:]),
                             reads=["xe%d" % k, "identb"], writes=["T%d" % k])
                    S.op("act", lambda e, k=k: e.copy(out=xeT[k][:], in_=Tb[k][:]), reads=["T%d" % k], writes=["xeT%d" % k])
                    pg, pgr = next_pq()
                    for fc in range(4):
                        for c in range(8):
                            S.op("pe", lambda e, pg=pg, c=c, fc=fc, k=k: e.matmul(
                                out=pg[:, fc * 128:(fc + 1) * 128], lhsT=wg_v[:, c, fc * 128:(fc + 1) * 128], rhs=xeT[k][:, c, :],
                                start=(c == 0), stop=(c == 7)), reads=[wg_r, "xeT%d" % k], writes=[pgr])
                    issue_w(3 * ex + NSLOT)
                    pu, pur = next_pq()
                    for fc in range(4):
                        for c in range(8):
                            S.op("pe", lambda e, pu=pu, c=c, fc=fc, k=k: e.matmul(
                                out=pu[:, fc * 128:(fc + 1) * 128], lhsT=wu_v[:, c, fc * 128:(fc + 1) * 128], rhs=xeT[k][:, c, :],
                                start=(c == 0), stop=(c == 7)), reads=[wu_r, "xeT%d" % k], writes=[pur])
                    issue_w(3 * ex + 1 + NSLOT)
                    S.op("act", lambda e, pg=pg, k=k: e.activation(out=sil[k][:], in_=pg[:], func=AF.Silu),
                         reads=[pgr], writes=["sil%d" % k])
                    S.op("dve", lambda e, pu=pu, k=k: e.tensor_tensor(out=hw[k][:], in0=sil[k][:], in1=pu[:], op=ALU.mult),
                         reads=["sil%d" % k, pur], writes=["hw%d" % k])

                def stage_b(ex):
                    k = ex % 2
                    wd_v = wview(3 * ex + 2)
                    wd_r = "ws%d" % ((3 * ex + 2) % NSLOT)
                    for nh in range(2):
                        pp, pr = next_pq()
                        for fc in range(4):
                            S.op("pe", lambda e, pp=pp, fc=fc, nh=nh, k=k: e.matmul(
                                out=pp[:], lhsT=hw[k][:, fc * 128:(fc + 1) * 128], rhs=wd_v[:, fc, nh * 512:(nh + 1) * 512],
                                start=(fc == 0), stop=(fc == 3)), reads=["hw%d" % k, wd_r], writes=[pr])
                        if nh == 0:
                            S.op("act", lambda e, pp=pp, k=k: e.copy(out=ye[k][:, 0:512], in_=pp[:]), reads=[pr], writes=["ye%d" % k])
                        else:
                            S.op("dve", lambda e, pp=pp, k=k: e.tensor_copy(out=ye[k][:, 512:1024], in_=pp[:]),
                                 reads=[pr], writes=["ye%d" % k])
                    issue_w(3 * ex + 2 + NSLOT)
                    nm = "ygs%d" % ex
                    ygs.append(nm)
                    S.dma("sp", lambda e, k=k, ex=ex: e.dma_start(out=yg_d[ex * CAPE:(ex + 1) * CAPE, :], in_=ye[k][:]),
                          reads=["ye%d" % k], writes=[nm])

                for ex in range(64):
                    stage_a(ex)
                    if ex > 0:
                        stage_b(ex - 1)
                stage_b(63)
                S.dma("sp", lambda e: e.dma_start(out=gv[:], in_=gvec_d[:, 2 if l == 0 else 4, :]), writes=["gv"])
                gi_box = [0]

                def c_stage1(tt):
                    for k in range(2):
                        j = gi_box[0] % 4
                        gi_box[0] += 1
                        S.dma("pool", lambda e, tt=tt, k=k, j=j: e.indirect_dma_start(
                            out=gy[j][:, :], out_offset=None, in_=yg_d,
                            in_offset=bass.IndirectOffsetOnAxis(ap=desti[:, tt, k:k + 1], axis=0),
                            bounds_check=bc_reg, oob_is_err=False),
                            reads=ygs + ["desti"], writes=["gy%d" % j])
                        S.op("dve", lambda e, tt=tt, k=k, j=j: e.scalar_tensor_tensor(
                            out=h[:, tt, :], in0=gy[j][:], scalar=cwt[:, tt, k:k + 1], in1=h[:, tt, :],
                            op0=ALU.mult, op1=ALU.add), reads=["gy%d" % j, "cwt", "h%d" % tt], writes=["h%d" % tt])
                    rms_a(tt)

                def c_stage2(tt):
                    rr = rms_b(tt)
                    if l == 0:
                        xb, xr = xnb2[tt % 2], "xnbm%d" % (tt % 2)
                        norm_tile(tt, xb[:], xr, rres=rr)
                        Tt, Tr = Tb[tt % 2], "T%d" % (tt % 2)
                        for c in range(8):
                            S.op("pe", lambda e, c=c, xb=xb, Tt=Tt: e.transpose(out=Tt[:, c, :], in_=xb[:, c * 128:(c + 1) * 128],
                                                                                identity=identb[:]),
                                 reads=[xr, "identb"], writes=[Tr])
                        if tt % 2 == 0:
                            S.op("act", lambda e, tt=tt, Tt=Tt: e.copy(out=xnT[:, :, tt * 128:(tt + 1) * 128], in_=Tt[:]),
                                 reads=[Tr], writes=["xnT%d" % tt])
                        else:
                            S.op("dve", lambda e, tt=tt, Tt=Tt: e.tensor_copy(out=xnT[:, :, tt * 128:(tt + 1) * 128], in_=Tt[:]),
                                 reads=[Tr], writes=["xnT%d" % tt])
                    else:
                        for hf in range(2):
                            csl = slice(hf * 512, (hf + 1) * 512)
                            S.op("dve", lambda e, tt=tt, hf=hf, csl=csl: e.scalar_tensor_tensor(
                                out=sil[hf][:], in0=h[:, tt, csl], scalar=rstd[:, tt:tt + 1], in1=gv[:, csl],
                                op0=ALU.mult, op1=ALU.mult), reads=["h%d" % tt, rr, "gv"], writes=["sil%d" % hf])
                            S.dma("sp", lambda e, tt=tt, hf=hf, csl=csl: e.dma_start(out=out_d[tt * 128:(tt + 1) * 128, csl], in_=sil[hf][:]),
                                  reads=["sil%d" % hf])

                for tt in range(NT):
                    c_stage1(tt)
                    if tt > 0:
                        c_stage2(tt - 1)
                c_stage2(NT - 1)
                S.barrier()

        def dump_h(dst):
            for tt in range(NT):
                S.dma("sp", lambda e, tt=tt: e.dma_start(out=dst[tt * 128:(tt + 1) * 128, :], in_=h[:, tt, :]),
                      reads=["h%d" % tt])

        stages = [(0, "mix"), (0, "moe"), (1, "mix"), (1, "moe")]
        for si, (l, kind) in enumerate(stages):
            if kind == "mix":
                mixer(l)
            else:
                moe(l)
            if dbg and dbg == si + 1:
                dump_h(dbg_d)
        S.finish()
    return nc


def _t5_bucket(rel):
    nb = 16
    ret = np.where(rel > 0, nb, 0)
    n = np.abs(rel)
    max_exact = 8
    nf = np.maximum(n, max_exact).astype(np.float32)
    large = max_exact + (np.log(nf / np.float32(max_exact)) / np.float32(math.log(1024 / max_exact))
                         * np.float32(nb - max_exact)).astype(np.int32)
    large = np.minimum(large, nb - 1)
    return ret + np.where(n < max_exact, n, large)


def _host_consts(rel_bias):
    kk = np.arange(128)[:, None]
    qq = np.arange(128)[None, :]
    biasT = np.zeros((128, 28, 128), np.float32)
    for hd in range(12):
        dil = GROUPS[hd // 4][0]
        for ty in range(3):
            if hd >= 8 and ty != 1:
                continue
            rel = kk - qq + (ty - 1) * 128
            b = rel_bias[_t5_bucket(rel * dil), hd]
            biasT[:, bias_index(hd, ty), :] = np.where(np.abs(rel) <= 64, b, np.float32(NEGM))
    cf = np.zeros((128, C_N), np.float32)
    cf[:, C_ID:C_ID + 128] = np.eye(128, dtype=np.float32)
    cf[:, C_TRI:C_TRI + 128] = (kk < qq).astype(np.float32)
    cf[:, C_ONE:C_ONE + 128] = 1.0
    cf[:, C_IOTA:C_IOTA + CAPG] = np.arange(CAPG, dtype=np.float32)[None, :]
    for g, w in enumerate(POOLW):
        e = np.ones(16, np.float32)
        for i in range(8):
            cnt = min(i + w - w // 2, S_) - max(i - w // 2, 0)
            e[i] = w / cnt
            ii = S_ - 8 + i
            cnt = min(ii + w - w // 2, S_) - max(ii - w // 2, 0)
            e[8 + i] = w / cnt
        cf[:, C_EDGE + g * 16:C_EDGE + g * 16 + 16] = e[None, :]
    cf[64, C_SEL:C_SEL + 64] = 1.0
    return biasT, cf


_NC_CACHE = {}


def kernel(x, rel_bias, norm_mix_g, w_in, pool_w, pool_scale, w_proj_attn, w_proj_pool, w_out, norm_ffn_g,
           w_router_group, w_router_expert, w_gate_e, w_up_e, w_down_e, norm_final_g, _dbg=0, _cores=8):
    f = lambda a: np.ascontiguousarray(np.asarray(a, dtype=np.float32))
    x = f(x)
    rel_bias = f(rel_bias)
    biasT, cf = _host_consts(rel_bias)
    gvec = np.stack([f(norm_mix_g)[0], f(norm_ffn_g)[0], f(norm_mix_g)[1], f(norm_ffn_g)[1], f(norm_final_g)], 0)
    gvec = np.ascontiguousarray(np.broadcast_to(gvec[None], (128, 5, D)))
    pscale = np.ascontiguousarray(f(pool_scale).reshape(2, 4, 128).transpose(2, 0, 1).reshape(128, 8))
    wre = f(w_router_expert).transpose(0, 2, 1, 3).reshape(2, D, 64)
    wr = np.ascontiguousarray(np.concatenate([f(w_router_group), wre], axis=2))
    shared = {
        "w_in": f(w_in), "pool_w": f(pool_w), "w_proj_attn": f(w_proj_attn), "w_proj_pool": f(w_proj_pool),
        "w_out": f(w_out), "wr": wr, "w_gate_e": f(w_gate_e), "w_up_e": f(w_up_e), "w_down_e": f(w_down_e),
        "gvec": gvec, "pscale": pscale, "biasT": biasT, "cf32": cf,
    }
    key = _dbg
    if key not in _NC_CACHE:
        _NC_CACHE[key] = build(_dbg)
    nc = _NC_CACHE[key]
    in_maps = []
    for b in range(_cores):
        m = dict(shared)
        m["x"] = np.ascontiguousarray(x[b])
        in_maps.append(m)
    res = run_bass_kernel_spmd(nc, in_maps, core_ids=list(range(_cores)))
    out = np.stack([res.results[b]["out"] for b in range(_cores)], 0).astype(np.float32)
    if _dbg:
        return out, np.stack([res.results[b]["dbg"] for b in range(_cores)], 0)
    return out
```
